# Optimizing a Trainium2 kernel written in Bass

```python
import jax, jax.numpy as jnp
from jax import lax
import numpy as np

D_MODEL = 1024
BATCH = 8
SEQ = 4096
DEPTH = 1

HG_HEADS = 4
HG_DK = 128
HG_DV = 128
HG_QK_WIDTH = HG_HEADS * HG_DK
HG_WIDTH = HG_HEADS * HG_DV
HG_CHUNK = 64
ATT_GROUPS = ((128, 1), (512, 4), (2048, 16))
ATT_HEADS_PER_GROUP = 4
ATT_HEAD_DIM = 64
ATT_HEADS = ATT_HEADS_PER_GROUP * len(ATT_GROUPS)
ATT_WIDTH = ATT_HEADS * ATT_HEAD_DIM
ATT_OUT_WIDTH = ATT_HEADS_PER_GROUP * ATT_HEAD_DIM
IN_SIZES = (HG_QK_WIDTH, HG_QK_WIDTH, HG_WIDTH, HG_WIDTH, ATT_WIDTH, ATT_WIDTH, ATT_WIDTH, D_MODEL, D_MODEL)
IN_COLS = sum(IN_SIZES)
IN_SPLIT_IDX = tuple(int(v) for v in np.cumsum(IN_SIZES)[:-1])
N_EXPERTS = 256
TOP_K = 8
D_EXPERT = 256
D_SHARED = 256
ROUTE_SCALE = 2.5
MOE_BLOCK = 128
RMS_EPS = 1e-6

kernel_name = 'hybrid_hgrn2_dilated_attn_moe_block'


def rms_norm(x, g):
    xf = x.astype(jnp.float32)
    y = xf * lax.rsqrt(jnp.mean(xf * xf, axis=-1, keepdims=True) + RMS_EPS)
    return (y * g.astype(jnp.float32)).astype(x.dtype)


def swiglu(x, wg, wu, wd):
    return (jax.nn.silu(x @ wg) * (x @ wu)) @ wd


def hgrn2_chunked(q, f_logit, v, lb):
    B, S, H, DK = q.shape
    DV = v.shape[-1]
    C = HG_CHUNK
    NC = S // C
    f = lb + (1.0 - lb) * jax.nn.sigmoid(f_logit.astype(jnp.float32))
    log_f = jnp.log(f)
    k = 1.0 - f

    def chunks(t):
        return t.astype(jnp.float32).reshape(B, NC, C, H, t.shape[-1]).transpose(1, 0, 3, 2, 4)

    causal = jnp.tril(jnp.ones((C, C), dtype=bool))

    def step(state, inp):
        qc, kc, vc, lfc = inp
        b = jnp.cumsum(lfc, axis=2)
        o_inter = jnp.einsum('bhtk,bhkv->bhtv', qc * jnp.exp(b), state)
        diff = b[:, :, :, None, :] - b[:, :, None, :, :]
        decay = jnp.exp(jnp.where(causal[:, :, None], diff, -jnp.inf))
        scores = jnp.einsum('bhtk,bhsk,bhtsk->bhts', qc, kc, decay)
        o_intra = jnp.einsum('bhts,bhsv->bhtv', scores, vc)
        b_end = b[:, :, -1:, :]
        state = jnp.exp(b_end[:, :, 0, :])[..., None] * state + jnp.einsum('bhsk,bhsv->bhkv', kc * jnp.exp(b_end - b), vc)
        return state, o_inter + o_intra

    s0 = jnp.zeros((B, H, DK, DV), jnp.float32)
    _, o = lax.scan(step, s0, (chunks(q), chunks(k), chunks(v), chunks(log_f)))
    return o.transpose(1, 0, 3, 2, 4).reshape(B, S, H, DV)


def dilated_window_group(q, k, v, window, dilation):
    B, S, H, E = q.shape
    nk = window // dilation
    L = S // dilation
    nb = -(-L // nk)
    Lp = nb * nk

    def to_blocks(t):
        t = t.astype(jnp.float32).reshape(B, L, dilation, H, E).transpose(0, 2, 1, 3, 4)
        t = jnp.pad(t, ((0, 0), (0, 0), (0, Lp - L), (0, 0), (0, 0)))
        return t.reshape(B, dilation, nb, nk, H, E)

    def with_prev(t):
        prev = jnp.pad(t[:, :, :-1], ((0, 0), (0, 0), (1, 0), (0, 0), (0, 0), (0, 0)))
        return jnp.concatenate([prev, t], axis=3)

    qb = to_blocks(q)
    kk = with_prev(to_blocks(k))
    vv = with_prev(to_blocks(v))
    s = jnp.einsum('brnqhe,brnkhe->brnhqk', qb, kk) * (E ** -0.5)
    i = jnp.arange(nk)[None, :, None]
    j = jnp.arange(2 * nk)[None, None, :]
    n = jnp.arange(nb)[:, None, None]
    valid = (j >= i) & (j <= i + nk) & (n * nk + j - nk >= 0)
    s = jnp.where(valid[:, None], s, -jnp.inf)
    m = jnp.max(s, axis=-1, keepdims=True)
    p = jnp.exp(s - m)
    l = jnp.sum(p, axis=-1, keepdims=True)
    o = jnp.einsum('brnhqk,brnkhe->brnqhe', p, vv) / jnp.swapaxes(l, 3, 4)
    lse = jnp.swapaxes((m + jnp.log(l))[..., 0], 3, 4)
    o = o.reshape(B, dilation, Lp, H, E)[:, :, :L].transpose(0, 2, 1, 3, 4).reshape(B, S, H, E)
    lse = lse.reshape(B, dilation, Lp, H)[:, :, :L].transpose(0, 2, 1, 3).reshape(B, S, H)
    return o, lse


def dilated_attention(q, k, v):
    B, S, _, E = q.shape
    outs, lses = [], []
    for g, (window, dilation) in enumerate(ATT_GROUPS):
        hs = slice(g * ATT_HEADS_PER_GROUP, (g + 1) * ATT_HEADS_PER_GROUP)
        o, lse = dilated_window_group(q[:, :, hs], k[:, :, hs], v[:, :, hs], window, dilation)
        outs.append(o)
        lses.append(lse)
    w = jax.nn.softmax(jnp.stack(lses, axis=0), axis=0)
    y = jnp.sum(w[..., None] * jnp.stack(outs, axis=0), axis=0)
    return y.reshape(B, S, ATT_OUT_WIDTH).astype(q.dtype)


def routed_experts(h, w_router, router_bias, w_gate, w_up, w_down):
    T, D = h.shape
    scores = jax.nn.sigmoid((h @ w_router).astype(jnp.float32))
    _, idx = lax.top_k(scores + router_bias.astype(jnp.float32), TOP_K)
    sel = jnp.take_along_axis(scores, idx, axis=-1)
    gates = (sel / jnp.sum(sel, axis=-1, keepdims=True) * ROUTE_SCALE).astype(h.dtype)
    flat_e = idx.reshape(-1).astype(jnp.int32)
    flat_tok = jnp.repeat(jnp.arange(T, dtype=jnp.int32), TOP_K)
    flat_w = gates.reshape(-1)
    order = jnp.argsort(flat_e)
    e_s, tok_s, w_s = flat_e[order], flat_tok[order], flat_w[order]
    counts = jnp.bincount(flat_e, length=N_EXPERTS).astype(jnp.int32)
    starts = jnp.cumsum(counts) - counts
    padded = (counts + MOE_BLOCK - 1) // MOE_BLOCK * MOE_BLOCK
    pend = jnp.cumsum(padded)
    pstart = pend - padded
    dest = pstart[e_s] + jnp.arange(T * TOP_K, dtype=jnp.int32) - starts[e_s]
    n_blocks = -(-(T * TOP_K) // MOE_BLOCK) + N_EXPERTS
    n_slots = n_blocks * MOE_BLOCK
    slot_tok = jnp.full((n_slots,), T, jnp.int32).at[dest].set(tok_s)
    slot_w = jnp.zeros((n_slots,), h.dtype).at[dest].set(w_s)
    blk_e = jnp.minimum(jnp.searchsorted(pend, jnp.arange(n_blocks, dtype=jnp.int32) * MOE_BLOCK, side='right'), N_EXPERTS - 1)
    h_pad = jnp.concatenate([h, jnp.zeros((1, D), h.dtype)], axis=0)

    def block_step(acc, blk):
        tok, w, e = blk
        yb = swiglu(h_pad[tok], w_gate[e], w_up[e], w_down[e]) * w[:, None]
        return acc.at[tok].add(yb), None

    acc, _ = lax.scan(block_step, jnp.zeros((T + 1, D), h.dtype),
                      (slot_tok.reshape(n_blocks, MOE_BLOCK), slot_w.reshape(n_blocks, MOE_BLOCK), blk_e))
    return acc[:T]


def setup_inputs(seed: int = 0) -> dict:
    key = jax.random.key(seed)
    ks = jax.random.split(key, 24)
    D = D_MODEL

    def nrm(k, shape, scale):
        return jax.random.normal(k, shape, jnp.float32) * scale

    lb_offset = jnp.where(jnp.arange(DEPTH + 1) == 0, -2.0, 0.0).astype(jnp.float32)[:, None]
    return {
        'x': nrm(ks[0], (BATCH, SEQ, D), 1.0),
        'c': nrm(ks[1], (BATCH, D), 1.0),
        'ada_w': nrm(ks[2], (DEPTH, D, 6 * D), 0.5 * D ** -0.5),
        'ada_b': nrm(ks[3], (DEPTH, 6 * D), 0.02),
        'norm1_g': 1.0 + nrm(ks[4], (DEPTH, D), 0.02),
        'w_in': nrm(ks[5], (DEPTH, D, IN_COLS), D ** -0.5),
        'lb_logits': nrm(ks[6], (DEPTH + 1, HG_QK_WIDTH), 0.1) + lb_offset,
        'hg_norm_g': 1.0 + nrm(ks[7], (DEPTH, HG_WIDTH), 0.02),
        'w_branch_a': nrm(ks[8], (DEPTH, HG_WIDTH, D), HG_WIDTH ** -0.5),
        'w_branch_b': nrm(ks[9], (DEPTH, ATT_OUT_WIDTH, D), ATT_OUT_WIDTH ** -0.5),
        'w_out': nrm(ks[10], (DEPTH, D, D), D ** -0.5),
        'norm2_g': 1.0 + nrm(ks[11], (DEPTH, D), 0.02),
        'w_router': nrm(ks[12], (DEPTH, D, N_EXPERTS), D ** -0.5),
        'router_bias': nrm(ks[13], (DEPTH, N_EXPERTS), 0.01),
        'w_exp_gate': nrm(ks[14], (DEPTH, N_EXPERTS, D, D_EXPERT), D ** -0.5),
        'w_exp_up': nrm(ks[15], (DEPTH, N_EXPERTS, D, D_EXPERT), D ** -0.5),
        'w_exp_down': nrm(ks[16], (DEPTH, N_EXPERTS, D_EXPERT, D), D_EXPERT ** -0.5),
        'w_sh_gate': nrm(ks[17], (DEPTH, D, D_SHARED), D ** -0.5),
        'w_sh_up': nrm(ks[18], (DEPTH, D, D_SHARED), D ** -0.5),
        'w_sh_down': nrm(ks[19], (DEPTH, D_SHARED, D), D_SHARED ** -0.5),
        'final_g': 1.0 + nrm(ks[20], (D,), 0.02),
    }


def reference(x, c, ada_w, ada_b, norm1_g, w_in, lb_logits, hg_norm_g, w_branch_a, w_branch_b, w_out,
              norm2_g, w_router, router_bias, w_exp_gate, w_exp_up, w_exp_down, w_sh_gate, w_sh_up, w_sh_down,
              final_g):
    B, S, D = x.shape
    lb_table = jnp.cumsum(jax.nn.softmax(lb_logits.astype(jnp.float32), axis=0), axis=0)
    for l in range(DEPTH):
        mod = jax.nn.silu(c) @ ada_w[l] + ada_b[l]
        shift1, scale1, gate1, shift2, scale2, gate2 = jnp.split(mod[:, None, :], 6, axis=-1)

        h = rms_norm(x, norm1_g[l]) * (1.0 + scale1) + shift1
        hq, hf, hi, hg, aq, ak, av, ga, gb = jnp.split(h @ w_in[l], IN_SPLIT_IDX, axis=-1)
        o_a = hgrn2_chunked(hq.reshape(B, S, HG_HEADS, HG_DK), hf.reshape(B, S, HG_HEADS, HG_DK),
                            hi.reshape(B, S, HG_HEADS, HG_DV), lb_table[l].reshape(HG_HEADS, HG_DK))
        y_a = rms_norm(o_a.astype(x.dtype), hg_norm_g[l].reshape(HG_HEADS, HG_DV)).reshape(B, S, HG_WIDTH) * jax.nn.silu(hg)
        y_b = dilated_attention(aq.reshape(B, S, ATT_HEADS, ATT_HEAD_DIM), ak.reshape(B, S, ATT_HEADS, ATT_HEAD_DIM),
                                av.reshape(B, S, ATT_HEADS, ATT_HEAD_DIM))
        merged = jax.nn.sigmoid(ga) * (y_a @ w_branch_a[l]) + jax.nn.sigmoid(gb) * (y_b @ w_branch_b[l])
        x = x + gate1 * (merged @ w_out[l])

        h2 = (rms_norm(x, norm2_g[l]) * (1.0 + scale2) + shift2).reshape(B * S, D)
        y = routed_experts(h2, w_router[l], router_bias[l], w_exp_gate[l], w_exp_up[l], w_exp_down[l]) \
            + swiglu(h2, w_sh_gate[l], w_sh_up[l], w_sh_down[l])
        x = x + gate2 * y.reshape(B, S, D)
    return rms_norm(x, final_g)
```

```python
import os
from contextlib import ExitStack
import numpy as np
import concourse.bass as bass
import concourse.mybir as mybir
from concourse.bass_utils import run_bass_kernel_spmd

F32 = mybir.dt.float32
BF16 = mybir.dt.bfloat16
I32 = mybir.dt.int32
AF = mybir.ActivationFunctionType
ALU = mybir.AluOpType
AX = mybir.AxisListType

D = 1024
SEQ = 4096
NT = SEQ // 128
EPS = 1e-6
NBLK = 512


class Sched:
    ENGS = ("pe", "act", "dve", "pool", "sp")
    DMA_RING = {"sp": 12, "pool": 12, "act": 6}

    def __init__(self, nc):
        self.nc = nc
        self.ops = []

    def op(self, eng, fn, reads=(), writes=(), dma=False, extra=(), nobar=False):
        self.ops.append(dict(eng=eng, fn=fn, reads=tuple(reads), writes=tuple(writes), dma=dma, extra=tuple(extra), nobar=nobar))
        return len(self.ops) - 1

    def pe(self, fn, reads=(), writes=()):
        return self.op("pe", fn, reads, writes)

    def act(self, fn, reads=(), writes=()):
        return self.op("act", fn, reads, writes)

    def dve(self, fn, reads=(), writes=()):
        return self.op("dve", fn, reads, writes)

    def pool(self, fn, reads=(), writes=()):
        return self.op("pool", fn, reads, writes)

    def dma(self, eng, fn, reads=(), writes=(), nobar=False):
        return self.op(eng, fn, reads, writes, dma=True, nobar=nobar)

    def barrier(self):
        n = len(self.ops)
        lastc = {}
        dmas = []
        for i, o in enumerate(self.ops):
            if o["dma"]:
                if not o["nobar"]:
                    dmas.append(i)
            else:
                lastc[o["eng"]] = i
        start = getattr(self, "_bar_from", 0)
        ex = [i for i in dmas if i >= start] + list(lastc.values())
        for e in self.ENGS:
            self.op(e, lambda eng: eng.nop(), extra=ex)
        self._bar_from = len(self.ops)

    def emit(self, stack):
        nc = self.nc
        ops = self.ops
        n = len(ops)
        last_w, readers, deps = {}, {}, [None] * n
        for i, o in enumerate(ops):
            d = set(o["extra"])
            for b in o["reads"]:
                w = last_w.get(b)
                if w is not None:
                    d.add(w)
            for b in o["writes"]:
                w = last_w.get(b)
                if w is not None:
                    d.add(w)
                d.update(readers.get(b, ()))
            d.discard(i)
            for b in o["reads"]:
                readers.setdefault(b, []).append(i)
            for b in o["writes"]:
                last_w[b] = i
                readers[b] = []
            deps[i] = d
        signal = [False] * n
        for i, o in enumerate(ops):
            for j in deps[i]:
                pj = ops[j]
                if pj["dma"]:
                    continue
                if pj["eng"] != o["eng"] or pj["eng"] != "pe":
                    signal[j] = True
        seq = [0] * n
        cnt = {e: 0 for e in self.ENGS}
        dcnt = {e: 0 for e in self.ENGS}
        dnum = [0] * n
        for i, o in enumerate(ops):
            if o["dma"]:
                dnum[i] = dcnt[o["eng"]]
                dcnt[o["eng"]] += 1
            elif signal[i]:
                cnt[o["eng"]] += 1
                seq[i] = cnt[o["eng"]]
        esem = {e: stack.enter_context(nc.semaphore("S_" + e)) for e in self.ENGS}
        dsem = {}
        for e, r in self.DMA_RING.items():
            if dcnt[e] > 0:
                dsem[e] = [stack.enter_context(nc.semaphore("D_%s%d" % (e, k))) for k in range(r)]
        self.stats = dict(n_ops=n, signals=dict(cnt), dmas=dict(dcnt))
        per_eng = {e: [i for i, o in enumerate(ops) if o["eng"] == e] for e in self.ENGS}
        RING = self.DMA_RING

        def run_engine(e, eng):
            known = {x: 0 for x in self.ENGS}
            dknown = {}
            for i in per_eng[e]:
                o = ops[i]
                wc, wd = {}, {}
                for j in deps[i]:
                    pj = ops[j]
                    if pj["dma"]:
                        r = RING[pj["eng"]]
                        key = (pj["eng"], dnum[j] % r)
                        v = 16 * (dnum[j] // r + 1)
                        if dknown.get(key, 0) < v:
                            wd[key] = max(wd.get(key, 0), v)
                    else:
                        if pj["eng"] == e and e == "pe":
                            continue
                        if known[pj["eng"]] < seq[j]:
                            wc[pj["eng"]] = max(wc.get(pj["eng"], 0), seq[j])
                if o["dma"]:
                    r = RING[e]
                    if dnum[i] >= r:
                        key = (e, dnum[i] % r)
                        v = 16 * (dnum[i] // r)
                        if dknown.get(key, 0) < v:
                            wd[key] = max(wd.get(key, 0), v)
                for x, v in wc.items():
                    eng.wait_ge(esem[x], v)
                    known[x] = v
                for key, v in wd.items():
                    eng.wait_ge(dsem[key[0]][key[1]], v)
                    dknown[key] = v
                ins = o["fn"](eng)
                if o["dma"]:
                    ins.then_inc(dsem[e][dnum[i] % RING[e]], 16)
                elif signal[i]:
                    ins.then_inc(esem[e], 1)
            if dcnt[e] > 0:
                r = RING[e]
                for k in range(min(r, dcnt[e])):
                    last = ((dcnt[e] - 1 - k) // r) * r + k
                    v = 16 * (last // r + 1)
                    if dknown.get((e, k), 0) < v:
                        eng.wait_ge(dsem[e][k], v)

        with nc.Block() as block:

            @block.tensor
            def _(eng):
                run_engine("pe", eng)

            @block.scalar
            def _(eng):
                run_engine("act", eng)

            @block.vector
            def _(eng):
                run_engine("dve", eng)

            @block.gpsimd
            def _(eng):
                run_engine("pool", eng)

            @block.sync
            def _(eng):
                run_engine("sp", eng)


class Arena:
    def __init__(self, nc, limit=228 * 1024):
        self.nc, self.off, self.limit, self.n = nc, 17 * 1024, limit, 0

    def alloc(self, name, shape, dt):
        esz = 4 if dt in (F32, I32) else 2
        nbytes = int(np.prod(shape[1:])) * esz
        self.off = (self.off + 31) // 32 * 32
        self.n += 1
        t = self.nc.alloc_sbuf_tensor_at("%s_%d" % (name, self.n), list(shape), dt, offset=self.off)
        self.off += nbytes
        assert self.off <= self.limit, ("SBUF overflow", name, self.off)
        return t

    def mark(self):
        return self.off

    def release(self, m):
        self.off = m


def build(stage=99, debug=False):
    nc = bass.Bass("TRN2", target_bir_lowering=False)
    kin = "ExternalInput"

    def din(name, shape):
        return nc.dram_tensor(name, list(shape), F32, kind=kin).ap()

    x = din("x", [SEQ, D])
    c = din("c", [D])
    ada_w = din("ada_w", [D, 6 * D])
    ada_b = din("ada_b", [6 * D])
    norm1_g = din("norm1_g", [D])
    w_in = din("w_in", [D, 6400])
    lb_logits = din("lb_logits", [2, 512])
    hg_norm_g = din("hg_norm_g", [512])
    w_branch_a = din("w_branch_a", [512, D])
    w_branch_b = din("w_branch_b", [256, D])
    w_out = din("w_out", [D, D])
    norm2_g = din("norm2_g", [D])
    w_router = din("w_router", [D, 256])
    router_bias = din("router_bias", [256])
    w_exp_all = din("w_exp_all", [256 * 128, 6144])
    w_sh_gate = din("w_sh_gate", [D, 256])
    w_sh_up = din("w_sh_up", [D, 256])
    w_sh_down = din("w_sh_down", [256, D])
    final_g = din("final_g", [D])
    out = nc.dram_tensor("out", [SEQ, D], F32, kind="ExternalOutput").ap()
    skind = "ExternalOutput" if debug else "Internal"
    mod_d = nc.dram_tensor("mod_d", [48, 128], F32, kind=skind).ap()
    yaT_d = nc.dram_tensor("yaT_d", [4, 128, SEQ], BF16, kind=skind).ap()
    ybT_d = nc.dram_tensor("ybT_d", [2, 128, SEQ], BF16, kind=skind).ap()
    x2p_d = nc.dram_tensor("x2p_d", [SEQ, D], F32, kind=skind).ap()
    h2_d = nc.dram_tensor("h2_d", [SEQ, D], BF16, kind=skind).ap()
    xs_d = nc.dram_tensor("xs_d", [NBLK * 128, D], BF16, kind="Internal").ap()
    ys_d = nc.dram_tensor("ys_d", [NBLK * 128, D], BF16, kind="Internal").ap()
    blk_d = nc.dram_tensor("blk_d", [2, NBLK], I32, kind=skind).ap()
    w16_d = [nc.dram_tensor("w16_d%d" % i, [128 * 128, 6144], BF16, kind="Internal").ap() for i in range(2)]
    dbg_hT = nc.dram_tensor("dbg_hT", [8, 128, SEQ], BF16, kind="ExternalOutput").ap() if debug else None
    dbg_d8 = nc.dram_tensor("dbg_d8", [128, NT * 8], I32, kind="ExternalOutput").ap() if debug else None
    dbg_w8 = nc.dram_tensor("dbg_w8", [128, NT * 8], F32, kind="ExternalOutput").ap() if debug else None

    st = ExitStack()
    with st:
        st.enter_context(nc.allow_low_precision("bf16 matmul operands, fp32 accumulation"))
        st.enter_context(nc.allow_non_contiguous_dma("small strided parameter loads"))
        S = Sched(nc)
        A = Arena(nc)
        PS = [nc.alloc_psum_tensor("ps%d" % i, [128, 512], F32) for i in range(8)]
        CV = {"n": 0}

        def convert_experts(k):
            for _ in range(k):
                e_ = CV["n"]
                if e_ >= 256:
                    return
                CV["n"] += 1
                S.dma("pool", lambda e, e_=e_: e.dma_start(out=w16_d[e_ // 128][(e_ % 128) * 128:(e_ % 128 + 1) * 128, :], in_=w_exp_all[e_ * 128:(e_ + 1) * 128, :]),
                      writes=["w16_%d" % e_], nobar=True)
        PSB = [p[:].bitcast(BF16) for p in PS]

        ident_f = A.alloc("ident_f", [128, 128], F32)
        ident_b = A.alloc("ident_b", [128, 128], BF16)
        io_i = A.alloc("io_i", [128, 128], I32)
        io_f = A.alloc("io_f", [128, 128], F32)
        S.pool(lambda e: e.iota(io_i[:], pattern=[[1, 128]], base=0, channel_multiplier=-1), writes=["io_i"])
        S.dve(lambda e: e.tensor_copy(out=io_f[:], in_=io_i[:]), reads=["io_i"], writes=["io_f"])
        S.dve(lambda e: e.tensor_single_scalar(out=ident_f[:], in_=io_f[:], scalar=0.0, op=ALU.is_equal), reads=["io_f"], writes=["ident_f"])
        S.dve(lambda e: e.tensor_copy(out=ident_b[:], in_=ident_f[:]), reads=["ident_f"], writes=["ident_b"])
        ones_b = A.alloc("ones_b", [128, 128], BF16)
        S.dve(lambda e: e.memset(ones_b[:], 1.0), writes=["ones_b"])
        modT = A.alloc("modT", [128, 48], F32)
        gate1_bc = A.alloc("gate1_bc", [128, D], F32)
        gate2_bc = A.alloc("gate2_bc", [128, D], F32)
        a_bc = A.alloc("a_bc", [128, D], F32)
        sh_bc = A.alloc("sh_bc", [128, D], F32)
        hT = A.alloc("hT", [128, 8, SEQ], BF16)
        base_mark = A.mark()

        def HTn(i):
            return "hTt%d" % i

        m0 = A.mark()
        sc = A.alloc("sc", [128, 8], F32)
        crow = A.alloc("crow", [8, 128], F32)
        adab = A.alloc("adab", [48, 128], F32)
        S.dma("sp", lambda e: e.dma_start(out=crow[:], in_=c.rearrange("(k p) -> k p", p=128)), writes=["crow"])
        S.dma("sp", lambda e: e.dma_start(out=adab[:], in_=ada_b.rearrange("(k p) -> k p", p=128)), writes=["adab"])
        S.pe(lambda e: e.transpose(out=PS[1][:, 0:8], in_=crow[:, :], identity=ident_f[0:8, 0:8]), reads=["crow", "ident_f"], writes=["ps1"])
        S.act(lambda e: e.activation(out=sc[:], in_=PS[1][:, 0:8], func=AF.Silu), reads=["ps1"], writes=["sc"])
        awb = [A.alloc("awb%d" % i, [128, 8, 1024], F32) for i in range(2)]
        for pc in range(6):
            buf = awb[pc % 2]
            bn = "awb%d" % (pc % 2)
            S.dma("sp", lambda e, buf=buf, pc=pc: e.dma_start(
                out=buf[:], in_=ada_w[:, pc * 1024:(pc + 1) * 1024].rearrange("(k p) c -> p k c", p=128)), writes=[bn])

            def f(e, buf=buf, pc=pc):
                ins = None
                for j in range(8):
                    jc = pc * 8 + j
                    for k in range(8):
                        ins = e.matmul(PS[0][:, jc:jc + 1], lhsT=buf[:, k, j * 128:(j + 1) * 128], rhs=sc[:, k:k + 1],
                                       start=(k == 0), stop=(k == 7))
                return ins
            S.pe(f, reads=[bn, "sc"], writes=["ps0"])
        S.dve(lambda e: e.tensor_copy(out=modT[:], in_=PS[0][:, 0:48]), reads=["ps0"], writes=["modT"])
        S.pe(lambda e: e.transpose(out=PS[1][0:48, 0:128], in_=modT[:, 0:48], identity=ident_f[:]), reads=["modT", "ident_f"], writes=["ps1"])
        modrow = A.alloc("modrow", [48, 128], F32)
        S.dve(lambda e: e.tensor_tensor(out=modrow[:], in0=PS[1][0:48, 0:128], in1=adab[:], op=ALU.add), reads=["ps1", "adab"], writes=["modrow"])
        S.dma("sp", lambda e: e.dma_start(out=mod_d, in_=modrow[:]), reads=["modrow"], writes=["mod_d"])

        def mod_bc(dst, name, j0):
            src = mod_d[j0:j0 + 8, :].rearrange("a b -> (a b)").partition_broadcast(128)
            S.dma("sp", lambda e: e.dma_start(out=dst[:], in_=src), reads=["mod_d"], writes=[name])

        mod_bc(gate1_bc, "gate1_bc", 16)
        mod_bc(gate2_bc, "gate2_bc", 40)

        def load_norm_consts(gvec, j_shift, j_scale):
            mod_bc(sh_bc, "sh_bc", j_shift)
            mod_bc(a_bc, "a_bc", j_scale)
            gtmp = A.alloc("gtmp", [128, D], F32)
            S.dma("sp", lambda e: e.dma_start(out=gtmp[:], in_=gvec.partition_broadcast(128)), writes=["gtmp"])
            S.dve(lambda e: e.scalar_tensor_tensor(out=a_bc[:], in0=a_bc[:], scalar=1.0, in1=gtmp[:], op0=ALU.add, op1=ALU.mult),
                  reads=["a_bc", "gtmp"], writes=["a_bc"])

        def norm_mod_T(xt, xname, hb, hbname, ss, ssname, junk, junkname):
            S.act(lambda e: e.activation(out=junk[:], in_=xt[:], func=AF.Square, accum_out=ss[:, 0:1]),
                  reads=[xname], writes=[junkname, ssname])
            S.dve(lambda e: e.tensor_scalar(out=ss[:, 1:2], in0=ss[:, 0:1], scalar1=1.0 / D, scalar2=EPS, op0=ALU.mult, op1=ALU.add),
                  reads=[ssname], writes=[ssname + "b"])
            S.act(lambda e: e.activation(out=ss[:, 3:4], in_=ss[:, 1:2], func=AF.Sqrt),
                  reads=[ssname + "b"], writes=[ssname + "d"])
            S.dve(lambda e: e.reciprocal(out=ss[:, 2:3], in_=ss[:, 3:4]),
                  reads=[ssname + "d"], writes=[ssname + "c"])
            S.dve(lambda e: e.scalar_tensor_tensor(out=junk[:], in0=xt[:], scalar=ss[:, 2:3], in1=a_bc[:], op0=ALU.mult, op1=ALU.mult),
                  reads=[xname, ssname + "c", "a_bc", junkname], writes=[junkname])
            S.dve(lambda e: e.tensor_tensor(out=hb[:], in0=junk[:], in1=sh_bc[:], op=ALU.add),
                  reads=[junkname, "sh_bc"], writes=[hbname])

        if stage <= 0:
            zt = A.alloc("zt", [128, D], F32)
            S.dve(lambda e: e.memset(zt[:], 0.0), writes=["zt"])
            S.dma("sp", lambda e: e.dma_start(out=out[0:128, :], in_=zt[:]), reads=["zt"])
            S.emit(st)
            return nc, S
        load_norm_consts(norm1_g, 0, 8)
        xts = [A.alloc("xt%d" % i, [128, D], F32) for i in range(3)]
        junks = [A.alloc("junk%d" % i, [128, D], F32) for i in range(2)]
        hbs = [A.alloc("hb%d" % i, [128, D], BF16) for i in range(2)]
        ssq1 = A.alloc("ssq1", [128, NT], F32)
        rstd1 = A.alloc("rstd1", [128, NT], F32)
        for i in range(NT):
            xt, xn_ = xts[i % 3], "xt%d" % (i % 3)
            S.dma("sp", lambda e, xt=xt, i=i: e.dma_start(out=xt[:], in_=x[i * 128:(i + 1) * 128, :]), writes=[xn_])
            S.act(lambda e, xt=xt, i=i: e.activation(out=junks[i % 2][:], in_=xt[:], func=AF.Square, accum_out=ssq1[:, i:i + 1]),
                  reads=[xn_], writes=["junk%d" % (i % 2), "ssq1_%d" % i])
            convert_experts(1)
        S.dve(lambda e: e.tensor_scalar(out=rstd1[:], in0=ssq1[:], scalar1=1.0 / D, scalar2=EPS, op0=ALU.mult, op1=ALU.add), reads=["ssq1_%d" % i for i in range(NT)], writes=["rstd1a"])
        S.act(lambda e: e.activation(out=ssq1[:], in_=rstd1[:], func=AF.Sqrt), reads=["rstd1a"], writes=["ssq1b"])
        S.dve(lambda e: e.reciprocal(out=rstd1[:], in_=ssq1[:]), reads=["ssq1b", "rstd1a"], writes=["rstd1"])
        for i in range(NT):
            xt, xn_ = xts[i % 3], "xt%d" % (i % 3)
            S.dma("sp", lambda e, xt=xt, i=i: e.dma_start(out=xt[:], in_=x[i * 128:(i + 1) * 128, :]), writes=[xn_])
            j = i % 2
            S.dve(lambda e, xt=xt, i=i, j=j: e.scalar_tensor_tensor(out=junks[j][:], in0=xt[:], scalar=rstd1[:, i:i + 1], in1=a_bc[:], op0=ALU.mult, op1=ALU.mult),
                  reads=[xn_, "rstd1", "a_bc"], writes=["junk%d" % j])
            S.dve(lambda e, j=j: e.tensor_tensor(out=hbs[j][:], in0=junks[j][:], in1=sh_bc[:], op=ALU.add), reads=["junk%d" % j, "sh_bc"], writes=["hb%d" % j])
            pb = 2 + (i % 2)

            def f(e, i=i, j=j, pb=pb):
                ins = None
                for kc in range(8):
                    ins = e.transpose(out=PSB[pb][:, kc * 128:(kc + 1) * 128], in_=hbs[j][:, kc * 128:(kc + 1) * 128], identity=ident_b[:])
                return ins
            S.pe(f, reads=["hb%d" % j, "ident_b"], writes=["ps%d" % pb])
            S.act(lambda e, i=i, pb=pb: e.activation(out=hT[:, :, i * 128:(i + 1) * 128], in_=PSB[pb][:, 0:1024].rearrange("p (k t) -> p k t", k=8), func=AF.Copy),
                  reads=["ps%d" % pb], writes=["hTt%d" % i])
        A.release(m0)
        if os.environ.get('KBAR', '1') == '1':
            S.barrier()
        if debug:
            for kc in range(8):
                S.dma("sp", lambda e, kc=kc: e.dma_start(out=dbg_hT[kc], in_=hT[:, kc, :]), reads=["hTt%d" % i for i in range(NT)])
        if stage <= 1:
            zt = A.alloc("zt", [128, D], F32)
            S.dve(lambda e: e.memset(zt[:], 0.0), writes=["zt"])
            S.dma("sp", lambda e: e.dma_start(out=out[0:128, :], in_=zt[:]), reads=["zt"])
            S.emit(st)
            return nc, S

        m2 = A.mark()
        wA = A.alloc("wA", [128, 8, 2048], BF16)
        S.dma("pool", lambda e: e.dma_start(out=wA[:], in_=w_in[:, 0:2048].rearrange("(k p) c -> p k c", p=128)), writes=["wA"])
        lbt = A.alloc("lbt", [128, 2, 4], F32)
        lb = A.alloc("lb", [128, 4], F32)
        oml = A.alloc("oml", [128, 4], F32)
        for r_ in range(2):
            S.dma("sp", lambda e, r_=r_: e.dma_start(out=lbt[:, r_, :], in_=lb_logits[r_].rearrange("(h k) -> k h", k=128)), writes=["lbt"])
        S.dve(lambda e: e.tensor_tensor(out=lb[:], in0=lbt[:, 0, :], in1=lbt[:, 1, :], op=ALU.subtract), reads=["lbt"], writes=["lb"])
        S.act(lambda e: e.activation(out=lb[:], in_=lb[:], func=AF.Sigmoid), reads=["lb"], writes=["lb"])
        S.dve(lambda e: e.tensor_scalar(out=oml[:], in0=lb[:], scalar1=-1.0, scalar2=1.0, op0=ALU.mult, op1=ALU.add), reads=["lb"], writes=["oml"])
        gn_bc = A.alloc("gn_bc", [128, 512], F32)
        S.dma("sp", lambda e: e.dma_start(out=gn_bc[:], in_=hg_norm_g.partition_broadcast(128)), writes=["gn_bc"])
        rmask = A.alloc("rmask", [128, 8, 64], F32)
        S.dve(lambda e: e.memset(rmask[:], 1.0), writes=["rmask"])
        S.dve(lambda e: e.memset(rmask[:, :, 0:1], 0.0), reads=["rmask"], writes=["rmask"])
        hmask = A.alloc("hmask", [128, 128], F32)
        S.dve(lambda e: e.tensor_single_scalar(out=hmask[:], in_=io_f[:], scalar=0.0, op=ALU.is_ge), reads=["io_f"], writes=["hmask"])
        S.dve(lambda e: e.memset(hmask[0:64, 64:128], 0.0), reads=["hmask"], writes=["hmask"])
        Sst = A.alloc("Sst", [128, 4, 128], F32)
        S.dve(lambda e: e.memset(Sst[:], 0.0), writes=["Sst"])
        sbf = [A.alloc("sbf%d" % i, [128, 512], BF16) for i in range(2)]
        sig = A.alloc("sig", [128, 512], F32)
        ff = A.alloc("ff", [128, 512], F32)
        logf = A.alloc("logf", [128, 512], F32)
        bb = A.alloc("bb", [128, 8, 64], F32)
        eb = A.alloc("eb", [128, 512], F32)
        enb = A.alloc("enb", [128, 512], F32)
        kk = A.alloc("kk", [128, 512], F32)
        dd = A.alloc("dd", [128, 8, 64], F32)
        ed = A.alloc("ed", [128, 512], F32)
        dec = A.alloc("dec", [128, 8], F32)
        qe = A.alloc("qe", [128, 512], BF16)
        ke = A.alloc("ke", [128, 512], BF16)
        kendT = A.alloc("kendT", [128, 512], BF16)
        kend_tm = A.alloc("kend_tm", [128, 512], BF16)
        v_sb = A.alloc("v_sb", [128, 512], BF16)
        ATb = A.alloc("ATb", [128, 4, 128], BF16)
        sg = A.alloc("sg", [128, 512], F32)
        t1 = A.alloc("t1", [128, 4, 128], F32)
        t2 = A.alloc("t2", [128, 512], F32)
        ya = A.alloc("ya", [128, 512], BF16)
        yaT_sb = A.alloc("yaT_sb", [128, 4, 128], BF16)
        ssq = A.alloc("ssq", [128, 12], F32)
        junkh = A.alloc("junkh", [128, 128], F32)
        bbf = bb[:].rearrange("p a b -> p (a b)")
        ddf = dd[:].rearrange("p a b -> p (a b)")
        rmf = rmask[:].rearrange("p a b -> p (a b)")
        sig2 = [sig, A.alloc("sigB", [128, 512], F32)]
        v_sb2 = [v_sb, A.alloc("v_sbB", [128, 512], BF16)]
        sg2 = [sg, A.alloc("sgB", [128, 512], F32)]
        qsb2 = [A.alloc("qsbA", [128, 512], F32), A.alloc("qsbB", [128, 512], F32)]

        def hg_proj(i):
            tok = slice(i * 128, (i + 1) * 128)
            hr = [HTn(i)]
            def fproj(e):
                ins = None
                for h in range(4):
                    for kc in range(8):
                        ins = e.matmul(PS[0][:, h * 128:(h + 1) * 128], lhsT=wA[:, kc, h * 128:(h + 1) * 128], rhs=hT[:, kc, tok], start=(kc == 0), stop=(kc == 7))
                for h in range(4):
                    for kc in range(8):
                        ins = e.matmul(PS[1][:, h * 128:(h + 1) * 128], lhsT=wA[:, kc, 512 + h * 128:512 + (h + 1) * 128], rhs=hT[:, kc, tok], start=(kc == 0), stop=(kc == 7))
                for kc in range(8):
                    ins = e.matmul(PS[2][:, :], lhsT=hT[:, kc, tok], rhs=wA[:, kc, 1024:1536], start=(kc == 0), stop=(kc == 7))
                for kc in range(8):
                    ins = e.matmul(PS[3][:, :], lhsT=hT[:, kc, tok], rhs=wA[:, kc, 1536:2048], start=(kc == 0), stop=(kc == 7))
                return ins
            S.pe(fproj, reads=hr + ["wA"], writes=["ps0", "ps1", "ps2", "ps3"])
            S.act(lambda e: e.activation(out=sig2[i % 2][:], in_=PS[1][:, :], func=AF.Sigmoid), reads=["ps1"], writes=["sig%d" % (i % 2)])
            S.act(lambda e: e.activation(out=v_sb2[i % 2][:], in_=PS[2][:, :], func=AF.Copy), reads=["ps2"], writes=["v_sb%d" % (i % 2)])
            S.act(lambda e: e.activation(out=sg2[i % 2][:], in_=PS[3][:, :], func=AF.Sigmoid), reads=["ps3"], writes=["sg%d" % (i % 2)])
            S.dve(lambda e: e.tensor_tensor(out=sg2[i % 2][:], in0=PS[3][:, :], in1=sg2[i % 2][:], op=ALU.mult), reads=["ps3", "sg%d" % (i % 2)], writes=["sg%d" % (i % 2)])
            S.act(lambda e: e.activation(out=qsb2[i % 2][:], in_=PS[0][:, :], func=AF.Copy), reads=["ps0"], writes=["qsb%d" % (i % 2)])

        def hg_main(i):
            tok = slice(i * 128, (i + 1) * 128)
            def faff(e):
                ins = None
                for h in range(4):
                    ins = e.tensor_scalar(out=ff[:, h * 128:(h + 1) * 128], in0=sig2[i % 2][:, h * 128:(h + 1) * 128], scalar1=oml[:, h:h + 1], scalar2=lb[:, h:h + 1], op0=ALU.mult, op1=ALU.add)
                return ins
            S.dve(faff, reads=["sig%d" % (i % 2), "oml", "lb"], writes=["ff"])
            S.act(lambda e: e.activation(out=logf[:], in_=ff[:], func=AF.Ln), reads=["ff"], writes=["logf"])
            S.pool(lambda e: e.tensor_scalar(out=kk[:], in0=ff[:], scalar1=-1.0, scalar2=1.0, op0=ALU.mult, op1=ALU.add), reads=["ff"], writes=["kk"])
            S.dve(lambda e: e.tensor_tensor_scan(out=bbf, data0=rmf, data1=logf[:], initial=0.0, op0=ALU.mult, op1=ALU.add), reads=["rmask", "logf"], writes=["bb"])
            S.act(lambda e: e.activation(out=eb[:], in_=bbf, func=AF.Exp), reads=["bb"], writes=["eb"])
            S.act(lambda e: e.activation(out=enb[:], in_=bbf, func=AF.Exp, scale=-1.0), reads=["bb"], writes=["enb"])
            S.act(lambda e: e.activation(out=dec[:], in_=bb[:, :, 63], func=AF.Exp), reads=["bb"], writes=["dec"])
            S.dve(lambda e: e.tensor_tensor(out=dd[:], in0=bb[:, :, 63:64].to_broadcast([128, 8, 64]), in1=bb[:], op=ALU.subtract), reads=["bb"], writes=["dd"])
            S.act(lambda e: e.activation(out=ed[:], in_=ddf, func=AF.Exp), reads=["dd"], writes=["ed"])
            S.dve(lambda e: e.tensor_tensor(out=qe[:], in0=qsb2[i % 2][:], in1=eb[:], op=ALU.mult), reads=["qsb%d" % (i % 2), "eb"], writes=["qe"])
            S.pool(lambda e: e.tensor_tensor(out=ke[:], in0=kk[:], in1=enb[:], op=ALU.mult), reads=["kk", "enb"], writes=["ke"])
            S.pool(lambda e: e.tensor_tensor(out=kendT[:], in0=kk[:], in1=ed[:], op=ALU.mult), reads=["kk", "ed"], writes=["kendT"])
            if i + 1 < NT:
                hg_proj(i + 1)

            def ftr(e):
                ins = None
                for h in range(4):
                    ins = e.transpose(out=PSB[7][:, h * 128:(h + 1) * 128], in_=kendT[:, h * 128:(h + 1) * 128], identity=ident_b[:])
                return ins
            S.pe(ftr, reads=["kendT", "ident_b"], writes=["ps7"])
            S.act(lambda e: e.activation(out=kend_tm[:], in_=PSB[7][:, 0:512], func=AF.Copy), reads=["ps7"], writes=["kend_tm"])

            def fat(e):
                ins = None
                for h in range(4):
                    ins = e.matmul(PS[4][:, h * 128:(h + 1) * 128], lhsT=ke[:, h * 128:(h + 1) * 128], rhs=qe[:, h * 128:(h + 1) * 128], start=True, stop=True)
                return ins
            S.pe(fat, reads=["ke", "qe"], writes=["ps4"])
            S.dve(lambda e: e.tensor_tensor(out=ATb[:], in0=PS[4][:, :].rearrange("p (h t) -> p h t", h=4), in1=hmask[:].unsqueeze(1).to_broadcast([128, 4, 128]), op=ALU.mult),
                  reads=["ps4", "hmask"], writes=["ATb"])
            for ci in range(2):
                r0 = 64 * ci
                S.act(lambda e, ci=ci: e.activation(out=sbf[ci][:], in_=Sst[:].rearrange("p h v -> p (h v)"), func=AF.Copy), reads=["Sst"], writes=["sbf%d" % ci])

                def fu(e, r0=r0):
                    ins = None
                    for h in range(4):
                        ins = e.matmul(PS[5][:, h * 128:(h + 1) * 128], lhsT=kend_tm[r0:r0 + 64, h * 128:(h + 1) * 128], rhs=v_sb2[i % 2][r0:r0 + 64, h * 128:(h + 1) * 128], start=True, stop=True)
                    return ins
                S.pe(fu, reads=["kend_tm", "v_sb%d" % (i % 2)], writes=["ps5"])
                S.dve(lambda e, ci=ci: e.tensor_tensor(out=Sst[:], in0=Sst[:], in1=dec[:].rearrange("p (h c) -> p h c", c=2)[:, :, ci:ci + 1].to_broadcast([128, 4, 128]), op=ALU.mult),
                      reads=["Sst", "dec"], writes=["Sst"])
                S.dve(lambda e: e.tensor_tensor(out=Sst[:].rearrange("p h v -> p (h v)"), in0=Sst[:].rearrange("p h v -> p (h v)"), in1=PS[5][:, :], op=ALU.add),
                      reads=["Sst", "ps5"], writes=["Sst"])

            def fo(e):
                ins = None
                for h in range(4):
                    hc = slice(h * 128, (h + 1) * 128)
                    e.matmul(PS[6][:, hc], lhsT=ATb[:, h, :], rhs=v_sb2[i % 2][:, hc], start=True, stop=False)
                    e.matmul(PS[6][0:64, hc], lhsT=qe[:, h * 128:h * 128 + 64], rhs=sbf[0][:, hc], start=False, stop=False)
                    ins = e.matmul(PS[6][64:128, hc], lhsT=qe[:, h * 128 + 64:h * 128 + 128], rhs=sbf[1][:, hc], start=False, stop=True)
                return ins
            S.pe(fo, reads=["ATb", "v_sb%d" % (i % 2), "qe", "sbf0", "sbf1"], writes=["ps6"])

            def fsq(e):
                ins = None
                for h in range(4):
                    ins = e.activation(out=junkh[:], in_=PS[6][:, h * 128:(h + 1) * 128], func=AF.Square, accum_out=ssq[:, h:h + 1])
                return ins
            S.act(fsq, reads=["ps6"], writes=["ssq", "junkh"])
            S.dve(lambda e: e.tensor_scalar(out=ssq[:, 4:8], in0=ssq[:, 0:4], scalar1=1.0 / 128, scalar2=EPS, op0=ALU.mult, op1=ALU.add), reads=["ssq"], writes=["ssqb"])
            S.act(lambda e: e.activation(out=ssq[:, 8:12], in_=ssq[:, 4:8], func=AF.Ln), reads=["ssqb"], writes=["ssqc"])
            S.act(lambda e: e.activation(out=ssq[:, 4:8], in_=ssq[:, 8:12], func=AF.Exp, scale=-0.5), reads=["ssqc", "ssqb"], writes=["ssqb"])
            S.dve(lambda e: e.tensor_tensor(out=t1[:], in0=PS[6][:, :].rearrange("p (h v) -> p h v", h=4), in1=ssq[:, 4:8].unsqueeze(2).to_broadcast([128, 4, 128]), op=ALU.mult),
                  reads=["ps6", "ssqb"], writes=["t1"])
            S.pool(lambda e: e.tensor_tensor(out=t2[:], in0=t1[:].rearrange("p h v -> p (h v)"), in1=sg2[i % 2][:], op=ALU.mult), reads=["t1", "sg%d" % (i % 2)], writes=["t2"])
            S.pool(lambda e: e.tensor_tensor(out=ya[:], in0=t2[:], in1=gn_bc[:], op=ALU.mult), reads=["t2", "gn_bc"], writes=["ya"])

            def fyt(e):
                ins = None
                for h in range(4):
                    ins = e.transpose(out=PSB[7][:, 512 + h * 128:512 + (h + 1) * 128], in_=ya[:, h * 128:(h + 1) * 128], identity=ident_b[:])
                return ins
            S.pe(fyt, reads=["ya", "ident_b"], writes=["ps7b"])
            S.act(lambda e: e.activation(out=yaT_sb[:].rearrange("p f t -> p (f t)"), in_=PSB[7][:, 512:1024], func=AF.Copy), reads=["ps7b"], writes=["yaT_sb"])
            S.dma("sp", lambda e, tok=tok: e.dma_start(out=yaT_d[:, :, tok].rearrange("f p t -> p f t"), in_=yaT_sb[:]), reads=["yaT_sb"], writes=["yaT_d"])
            convert_experts(3)
        hg_proj(0)
        for i in range(NT):
            hg_main(i)
        A.release(m2)
        S.barrier()
        if stage <= 2:
            zt = A.alloc("zt", [128, D], F32)
            S.dve(lambda e: e.memset(zt[:], 0.0), writes=["zt"])
            S.dma("sp", lambda e: e.dma_start(out=out[0:128, :], in_=zt[:]), reads=["zt"])
            S.emit(st)
            return nc, S

        m3 = A.mark()
        wB = A.alloc("wB", [128, 8, 2304], BF16)
        S.dma("pool", lambda e: e.dma_start(out=wB[:], in_=w_in[:, 2048:4352].rearrange("(k p) c -> p k c", p=128)), writes=["wB"])
        mk = A.alloc("mk", [128, 4, 128], BF16)
        mk0 = A.alloc("mk0", [128, 4, 128], BF16)
        S.dve(lambda e: e.tensor_single_scalar(out=mk[:, 0, :], in_=io_f[:], scalar=0.0, op=ALU.is_le), reads=["io_f"], writes=["mk"])
        S.dve(lambda e: e.tensor_single_scalar(out=mk[:, 1, :], in_=io_f[:], scalar=0.0, op=ALU.is_ge), reads=["io_f", "mk"], writes=["mk"])
        S.dve(lambda e: e.tensor_copy(out=mk[:, 2:4, :], in_=mk[:, 0:2, :]), reads=["mk"], writes=["mk"])
        S.dve(lambda e: e.tensor_copy(out=mk0[:], in_=mk[:]), reads=["mk"], writes=["mk0"])
        S.dve(lambda e: e.memset(mk0[:, 0, :], 0.0), reads=["mk0"], writes=["mk0"])
        S.dve(lambda e: e.memset(mk0[:, 2, :], 0.0), reads=["mk0"], writes=["mk0"])
        QT = A.alloc("QT", [128, 2, SEQ], BF16)
        S.dve(lambda e: e.memset(QT[64:128, 0, :], 0.0), writes=["QTz0"])
        S.dve(lambda e: e.memset(QT[0:64, 1, :], 0.0), writes=["QTz1"])
        KT = A.alloc("KT", [128, SEQ], BF16)
        Vb = A.alloc("Vb", [128, 32, 128], BF16)
        numT = A.alloc("numT", [128, SEQ], F32)
        denT = A.alloc("denT", [128, SEQ], F32)
        PT2 = [A.alloc("PT%d" % i, [128, 512], BF16) for i in range(2)]
        PTm2 = [A.alloc("PTm%d" % i, [128, 512], BF16) for i in range(2)]
        ybo = A.alloc("ybo", [128, SEQ], BF16)
        allh = [HTn(i) for i in range(NT)]
        for hp in range(2):
            for g, dil in enumerate((1, 4, 16)[:int(os.environ.get('KG', '3'))]):
                qc = 256 * g + 128 * hp
                kc0 = 768 + qc
                vc0 = 1536 + qc
                for tb in range(8):
                    tsl = slice(tb * 512, (tb + 1) * 512)

                    def fq(e, tsl=tsl, qc=qc, kc0=kc0):
                        ins = None
                        for kc in range(8):
                            ins = e.matmul(PS[0][:, :], lhsT=wB[:, kc, qc:qc + 128], rhs=hT[:, kc, tsl], start=(kc == 0), stop=(kc == 7))
                        for kc in range(8):
                            ins = e.matmul(PS[1][:, :], lhsT=wB[:, kc, kc0:kc0 + 128], rhs=hT[:, kc, tsl], start=(kc == 0), stop=(kc == 7))
                        return ins
                    S.pe(fq, reads=allh + ["wB"], writes=["ps0", "ps1"])
                    S.act(lambda e, tsl=tsl: e.activation(out=QT[0:64, 0, tsl], in_=PS[0][0:64, :], func=AF.Copy, scale=0.125), reads=["ps0", "QTz0"], writes=["QTa"])
                    S.act(lambda e, tsl=tsl: e.activation(out=QT[64:128, 1, tsl], in_=PS[0][64:128, :], func=AF.Copy, scale=0.125), reads=["ps0", "QTz1"], writes=["QTb"])
                    S.dve(lambda e, tsl=tsl: e.tensor_copy(out=KT[:, tsl], in_=PS[1][:, :]), reads=["ps1"], writes=["KT"])
                L = SEQ // dil
                nb = L // 128
                blocks = [(r, n_) for r in range(dil) for n_ in range(nb)]

                def tokslice(r, n_, dil=dil):
                    st_ = 128 * n_ * dil + r
                    return slice(st_, st_ + 127 * dil + 1, dil) if dil > 1 else slice(st_, st_ + 128)
                for b4 in range(8):
                    def fv(e, b4=b4, vc0=vc0, blocks=blocks, tokslice=tokslice):
                        ins = None
                        for q_ in range(4):
                            r, n_ = blocks[b4 * 4 + q_]
                            for kc in range(8):
                                ins = e.matmul(PS[2][:, q_ * 128:(q_ + 1) * 128], lhsT=hT[:, kc, tokslice(r, n_)], rhs=wB[:, kc, vc0:vc0 + 128], start=(kc == 0), stop=(kc == 7))
                        return ins
                    S.pe(fv, reads=allh + ["wB"], writes=["ps2"])
                    S.act(lambda e, b4=b4: e.activation(out=Vb[:, b4 * 4:(b4 + 1) * 4, :].rearrange("p a b -> p (a b)"), in_=PS[2][:, :], func=AF.Copy), reads=["ps2"], writes=["Vb"])
                def blk_params(bi):
                    r, n_ = blocks[bi]
                    qs = tokslice(r, n_)
                    ks = [tokslice(r, n_ - 1) if n_ > 0 else qs, qs]
                    vbi = [bi - 1 if n_ > 0 else bi, bi]
                    return n_, qs, ks, vbi

                def att_A(bi):
                    n_, qs, ks, vbi = blk_params(bi)
                    pb = 3 + (bi % 2)
                    p2 = bi % 2

                    def fs(e):
                        ins = None
                        for h in range(2):
                            for kb in range(2):
                                ins = e.matmul(PS[pb][:, (h * 2 + kb) * 128:(h * 2 + kb + 1) * 128], lhsT=KT[:, ks[kb]], rhs=QT[:, h, qs], start=True, stop=True)
                        return ins
                    S.pe(fs, reads=["QTa", "QTb", "KT"], writes=["ps%d" % pb])
                    S.act(lambda e: e.activation(out=PT2[p2][:], in_=PS[pb][:, :], func=AF.Exp), reads=["ps%d" % pb], writes=["PT%d" % p2])
                    mm_ = mk if n_ > 0 else mk0
                    S.dve(lambda e: e.tensor_tensor(out=PTm2[p2][:], in0=PT2[p2][:], in1=mm_[:].rearrange("p a b -> p (a b)"), op=ALU.mult), reads=["PT%d" % p2, "mk", "mk0"], writes=["PTm%d" % p2])

                def att_B(bi):
                    n_, qs, ks, vbi = blk_params(bi)
                    ob = 5 + (bi % 2)
                    p2 = bi % 2

                    def fpv(e):
                        ins = None
                        for h in range(2):
                            for kb in range(2):
                                ins = e.matmul(PS[ob][h * 64:(h + 1) * 64, 0:128], lhsT=Vb[:, vbi[kb], h * 64:(h + 1) * 64], rhs=PTm2[p2][:, (h * 2 + kb) * 128:(h * 2 + kb + 1) * 128], start=(kb == 0), stop=(kb == 1))
                        for h in range(2):
                            for kb in range(2):
                                ins = e.matmul(PS[ob][h * 64:(h + 1) * 64, 128:256], lhsT=ones_b[:, 0:64], rhs=PTm2[p2][:, (h * 2 + kb) * 128:(h * 2 + kb + 1) * 128], start=(kb == 0), stop=(kb == 1))
                        return ins
                    S.pe(fpv, reads=["Vb", "PTm%d" % p2, "ones_b"], writes=["ps%d" % ob])
                    if bi % 2 == 0:
                        convert_experts(1)
                    if g == 0:
                        S.act(lambda e: e.activation(out=numT[:, qs], in_=PS[ob][:, 0:128], func=AF.Copy), reads=["ps%d" % ob], writes=["numT"])
                        S.act(lambda e: e.activation(out=denT[:, qs], in_=PS[ob][:, 128:256], func=AF.Copy), reads=["ps%d" % ob], writes=["denT"])
                    else:
                        S.dve(lambda e: e.tensor_tensor(out=numT[:, qs], in0=PS[ob][:, 0:128], in1=numT[:, qs], op=ALU.add), reads=["ps%d" % ob, "numT"], writes=["numT"])
                        S.dve(lambda e: e.tensor_tensor(out=denT[:, qs], in0=PS[ob][:, 128:256], in1=denT[:, qs], op=ALU.add), reads=["ps%d" % ob, "denT"], writes=["denT"])

                nbk = min(len(blocks), int(os.environ.get('KNB', '9999')))
                for bi in range(nbk + 1):
                    if bi < nbk:
                        att_A(bi)
                    if bi >= 1:
                        att_B(bi - 1)
            S.dve(lambda e: e.reciprocal(out=denT[:], in_=denT[:]), reads=["denT"], writes=["denT"])
            S.dve(lambda e: e.tensor_tensor(out=ybo[:], in0=numT[:], in1=denT[:], op=ALU.mult), reads=["numT", "denT"], writes=["ybo"])
            S.dma("sp", lambda e, hp=hp: e.dma_start(out=ybT_d[hp], in_=ybo[:]), reads=["ybo"], writes=["ybT_d"])
        A.release(m3)
        S.barrier()
        if stage <= 3:
            zt = A.alloc("zt", [128, D], F32)
            S.dve(lambda e: e.memset(zt[:], 0.0), writes=["zt"])
            S.dma("sp", lambda e: e.dma_start(out=out[0:128, :], in_=zt[:]), reads=["zt"])
            S.emit(st)
            return nc, S

        m4 = A.mark()
        wC = A.alloc("wC", [128, 8, 2048], BF16)
        wa = A.alloc("wa", [128, 4, 1024], BF16)
        wb_ = A.alloc("wb_", [128, 2, 1024], BF16)
        wo = A.alloc("wo", [128, 8, 1024], BF16)
        S.dma("pool", lambda e: e.dma_start(out=wC[:], in_=w_in[:, 4352:6400].rearrange("(k p) c -> p k c", p=128)), writes=["wC"])
        S.dma("pool", lambda e: e.dma_start(out=wa[:], in_=w_branch_a.rearrange("(k p) c -> p k c", p=128)), writes=["wa"])
        S.dma("pool", lambda e: e.dma_start(out=wb_[:], in_=w_branch_b.rearrange("(k p) c -> p k c", p=128)), writes=["wb_"])
        S.dma("pool", lambda e: e.dma_start(out=wo[:], in_=w_out.rearrange("(k p) c -> p k c", p=128)), writes=["wo"])
        yaTb = [A.alloc("yaTb%d" % i, [128, 4, 512], BF16) for i in range(2)]
        ybTb = [A.alloc("ybTb%d" % i, [128, 2, 512], BF16) for i in range(2)]
        mergedT = A.alloc("mergedT", [128, 8, 512], BF16)
        sga = [A.alloc("sga%d" % i, [128, 512], F32) for i in range(2)]
        sgb = [A.alloc("sgb%d" % i, [128, 512], F32) for i in range(2)]
        m1 = [A.alloc("m1_%d" % i, [128, 512], F32) for i in range(2)]
        m2_ = [A.alloc("m2_%d" % i, [128, 512], F32) for i in range(2)]
        xt2 = [A.alloc("xt2_%d" % i, [128, D], F32) for i in range(2)]
        zt2 = [A.alloc("zt2_%d" % i, [128, D], F32) for i in range(2)]
        for tb in range(8):
            tsl = slice(tb * 512, (tb + 1) * 512)
            j = tb % 2
            S.dma("sp", lambda e, j=j, tsl=tsl: e.dma_start(out=yaTb[j][:], in_=yaT_d[:, :, tsl].rearrange("f p t -> p f t")), reads=["yaT_d"], writes=["yaTb%d" % j])
            S.dma("sp", lambda e, j=j, tsl=tsl: e.dma_start(out=ybTb[j][:], in_=ybT_d[:, :, tsl].rearrange("f p t -> p f t")), reads=["ybT_d"], writes=["ybTb%d" % j])
            hrd = [HTn(tb * 4 + q_) for q_ in range(4)]
            for cc in range(8):
                csl = slice(cc * 128, (cc + 1) * 128)
                pp = cc % 2
                b0 = 4 * pp

                def fm(e, j=j, tsl=tsl, csl=csl, cc=cc, b0=b0):
                    ins = None
                    for fc in range(4):
                        ins = e.matmul(PS[b0][:, :], lhsT=wa[:, fc, csl], rhs=yaTb[j][:, fc, :], start=(fc == 0), stop=(fc == 3))
                    for fc in range(2):
                        ins = e.matmul(PS[b0 + 1][:, :], lhsT=wb_[:, fc, csl], rhs=ybTb[j][:, fc, :], start=(fc == 0), stop=(fc == 1))
                    for kc in range(8):
                        ins = e.matmul(PS[b0 + 2][:, :], lhsT=wC[:, kc, csl], rhs=hT[:, kc, tsl], start=(kc == 0), stop=(kc == 7))
                    for kc in range(8):
                        ins = e.matmul(PS[b0 + 3][:, :], lhsT=wC[:, kc, 1024 + cc * 128:1024 + (cc + 1) * 128], rhs=hT[:, kc, tsl], start=(kc == 0), stop=(kc == 7))
                    return ins
                S.pe(fm, reads=hrd + ["wa", "wb_", "wC", "yaTb%d" % j, "ybTb%d" % j], writes=["ps%d" % (b0 + k_) for k_ in range(4)])
                S.act(lambda e, pp=pp, b0=b0: e.activation(out=sga[pp][:], in_=PS[b0 + 2][:, :], func=AF.Sigmoid), reads=["ps%d" % (b0 + 2)], writes=["sga%d" % pp])
                S.act(lambda e, pp=pp, b0=b0: e.activation(out=sgb[pp][:], in_=PS[b0 + 3][:, :], func=AF.Sigmoid), reads=["ps%d" % (b0 + 3)], writes=["sgb%d" % pp])
                S.dve(lambda e, pp=pp, b0=b0: e.tensor_tensor(out=m1[pp][:], in0=PS[b0][:, :], in1=sga[pp][:], op=ALU.mult), reads=["ps%d" % b0, "sga%d" % pp], writes=["m1_%d" % pp])
                S.dve(lambda e, pp=pp, b0=b0: e.tensor_tensor(out=m2_[pp][:], in0=PS[b0 + 1][:, :], in1=sgb[pp][:], op=ALU.mult), reads=["ps%d" % (b0 + 1), "sgb%d" % pp], writes=["m2_%d" % pp])
                S.pool(lambda e, cc=cc, pp=pp: e.tensor_tensor(out=mergedT[:, cc, :], in0=m1[pp][:], in1=m2_[pp][:], op=ALU.add), reads=["m1_%d" % pp, "m2_%d" % pp], writes=["mergedT%d" % cc])
                convert_experts(1)
            for q_ in range(4):
                i = tb * 4 + q_
                jj = i % 2
                S.dma("sp", lambda e, i=i, jj=jj: e.dma_start(out=xt2[jj][:], in_=x[i * 128:(i + 1) * 128, :]), writes=["xt2_%d" % jj])

                zb = 2 * (q_ % 2)

                def fz(e, q_=q_, zb=zb):
                    ins = None
                    for half in range(2):
                        for cc in range(8):
                            ins = e.matmul(PS[zb + half][:, :], lhsT=mergedT[:, cc, q_ * 128:(q_ + 1) * 128], rhs=wo[:, cc, half * 512:(half + 1) * 512], start=(cc == 0), stop=(cc == 7))
                    return ins
                S.pe(fz, reads=["mergedT%d" % cc for cc in range(8)] + ["wo"], writes=["ps%d" % zb, "ps%d" % (zb + 1)])
                for half in range(2):
                    hs = slice(half * 512, (half + 1) * 512)
                    S.dve(lambda e, jj=jj, half=half, hs=hs, zb=zb: e.tensor_tensor(out=zt2[jj][:, hs], in0=PS[zb + half][:, :], in1=gate1_bc[:, hs], op=ALU.mult),
                          reads=["ps%d" % (zb + half), "gate1_bc"], writes=["zt2_%d_%d" % (jj, half)])
                S.pool(lambda e, jj=jj: e.tensor_tensor(out=zt2[jj][:], in0=zt2[jj][:], in1=xt2[jj][:], op=ALU.add),
                       reads=["zt2_%d_0" % jj, "zt2_%d_1" % jj, "xt2_%d" % jj], writes=["zt2_%d_0" % jj, "zt2_%d_1" % jj])
                S.dma("sp", lambda e, i=i, jj=jj: e.dma_start(out=x2p_d[i * 128:(i + 1) * 128, :], in_=zt2[jj][:]), reads=["zt2_%d_0" % jj, "zt2_%d_1" % jj], writes=["x2p_d%d" % i])
        A.release(m4)
        S.barrier()
        A.release(base_mark)
        A.off -= 8 * SEQ * 2
        off_sb = (A.off + 31) // 32 * 32
        sb_all = A.alloc("sb_all", [128, NT, 256], F32)
        pos_all = A.alloc("pos_all", [128, NT, 256], F32)
        m8_all = A.alloc("m8_all", [128, NT, 8], F32)
        w8_all = A.alloc("w8_all", [128, NT, 8], F32)
        carry = A.alloc("carry", [128, 256], F32)
        route_mark = A.mark()
        m5 = A.mark()
        load_norm_consts(norm2_g, 24, 32)
        wr = A.alloc("wr", [128, 8, 256], BF16)
        wsg = A.alloc("wsg", [128, 8, 512], BF16)
        wsd = A.alloc("wsd", [128, 2, 1024], BF16)
        S.dma("pool", lambda e: e.dma_start(out=wr[:], in_=w_router.rearrange("(k p) c -> p k c", p=128)), writes=["wr"])
        S.dma("pool", lambda e: e.dma_start(out=wsg[:, :, 0:256], in_=w_sh_gate.rearrange("(k p) c -> p k c", p=128)), writes=["wsg_a"])
        S.dma("pool", lambda e: e.dma_start(out=wsg[:, :, 256:512], in_=w_sh_up.rearrange("(k p) c -> p k c", p=128)), writes=["wsg_b"])
        S.dma("pool", lambda e: e.dma_start(out=wsd[:], in_=w_sh_down.rearrange("(k p) c -> p k c", p=128)), writes=["wsd"])
        rb_bc = A.alloc("rb_bc", [128, 256], F32)
        S.dma("sp", lambda e: e.dma_start(out=rb_bc[:], in_=router_bias.partition_broadcast(128)), writes=["rb_bc"])
        tri_b = A.alloc("tri_b", [128, 128], BF16)
        S.dve(lambda e: e.tensor_single_scalar(out=tri_b[:], in_=io_f[:], scalar=0.0, op=ALU.is_gt), reads=["io_f"], writes=["tri_b"])
        S.dve(lambda e: e.memset(carry[:], 0.0), writes=["carry"])
        junk2 = [A.alloc("junk2_%d" % i, [128, D], F32) for i in range(2)]
        h2b = [A.alloc("h2b%d" % i, [128, D], BF16) for i in range(2)]
        ss2 = [A.alloc("ss2_%d" % i, [128, 4], F32) for i in range(2)]
        h2T = [A.alloc("h2T%d" % i, [128, 8, 128], BF16) for i in range(2)]
        scr = A.alloc("scr", [128, 256], F32)
        maskb = A.alloc("maskb", [128, 256], BF16)
        jk = A.alloc("jk", [128, 256], F32)
        s8 = A.alloc("s8", [128, 12], F32)
        sgl = A.alloc("sgl", [128, 256], F32)
        actT = A.alloc("actT", [128, 2, 128], BF16)
        yt = [A.alloc("yt%d" % i, [128, D], F32) for i in range(2)]
        NX1 = 3
        x1t = [A.alloc("x1u%d" % i, [128, D], F32) for i in range(NX1)]
        ssq_all = A.alloc("ssq_all", [128, NT], F32)
        rstd_all = A.alloc("rstd_all", [128, NT], F32)
        for i in range(NT):
            jx = i % NX1
            S.dma("sp", lambda e, i=i, jx=jx: e.dma_start(out=x1t[jx][:], in_=x2p_d[i * 128:(i + 1) * 128, :]), reads=["x2p_d%d" % i], writes=["x1u%d" % jx])
            S.act(lambda e, i=i, jx=jx: e.activation(out=junk2[i % 2][:], in_=x1t[jx][:], func=AF.Square, accum_out=ssq_all[:, i:i + 1]),
                  reads=["x1u%d" % jx], writes=["junk2_%d" % (i % 2), "ssq%d" % i])
        S.dve(lambda e: e.tensor_scalar(out=rstd_all[:], in0=ssq_all[:], scalar1=1.0 / D, scalar2=EPS, op0=ALU.mult, op1=ALU.add), reads=["ssq%d" % i for i in range(NT)], writes=["rstd_a"])
        S.act(lambda e: e.activation(out=ssq_all[:], in_=rstd_all[:], func=AF.Sqrt), reads=["rstd_a"], writes=["ssq_b"])
        S.dve(lambda e: e.reciprocal(out=rstd_all[:], in_=ssq_all[:]), reads=["ssq_b", "rstd_a"], writes=["rstd_all"])

        def stP(i):
            j = i % 2
            jx = i % NX1
            S.dma("sp", lambda e: e.dma_start(out=x1t[jx][:], in_=x2p_d[i * 128:(i + 1) * 128, :]), reads=["x2p_d%d" % i], writes=["x1u%d" % jx])
            S.dve(lambda e: e.scalar_tensor_tensor(out=junk2[j][:], in0=x1t[jx][:], scalar=rstd_all[:, i:i + 1], in1=a_bc[:], op0=ALU.mult, op1=ALU.mult),
                  reads=["x1u%d" % jx, "rstd_all", "a_bc"], writes=["junk2_%d" % j])
            S.dve(lambda e: e.tensor_tensor(out=h2b[j][:], in0=junk2[j][:], in1=sh_bc[:], op=ALU.add), reads=["junk2_%d" % j, "sh_bc"], writes=["h2b%d" % j])
            S.dma("sp", lambda e: e.dma_start(out=h2_d[i * 128:(i + 1) * 128, :], in_=h2b[j][:]), reads=["h2b%d" % j], writes=["h2_d%d" % i])

            def ft(e, j=j):
                ins = None
                for kc in range(8):
                    ins = e.transpose(out=PSB[0][:, kc * 128:(kc + 1) * 128], in_=h2b[j][:, kc * 128:(kc + 1) * 128], identity=ident_b[:])
                return ins
            S.pe(ft, reads=["h2b%d" % j, "ident_b"], writes=["ps0"])
            S.act(lambda e, j=j: e.activation(out=h2T[j][:].rearrange("p k t -> p (k t)"), in_=PSB[0][:, 0:1024], func=AF.Copy), reads=["ps0"], writes=["h2T%d" % j])

        def stQ(i):
            j = i % 2

            def frt(e, j=j):
                ins = None
                for kc in range(8):
                    ins = e.matmul(PS[1][:, 0:256], lhsT=h2T[j][:, kc, :], rhs=wr[:, kc, :], start=(kc == 0), stop=(kc == 7))
                return ins
            S.pe(frt, reads=["h2T%d" % j, "wr"], writes=["ps1"])
            S.act(lambda e: e.activation(out=scr[:], in_=PS[1][:, 0:256], func=AF.Sigmoid), reads=["ps1"], writes=["scr"])
            S.dve(lambda e, i=i: e.tensor_tensor(out=sb_all[:, i, :], in0=scr[:], in1=rb_bc[:], op=ALU.add), reads=["scr", "rb_bc"], writes=["sb_all%d" % i])
            S.dve(lambda e, i=i: e.max(out=m8_all[:, i, :], in_=sb_all[:, i, :]), reads=["sb_all%d" % i], writes=["m8_all%d" % i])
            S.dve(lambda e, i=i: e.tensor_scalar(out=maskb[:], in0=sb_all[:, i, :], scalar1=m8_all[:, i, 7:8], scalar2=None, op0=ALU.is_ge),
                  reads=["sb_all%d" % i, "m8_all%d" % i], writes=["maskb"])
            for k in range(8):
                S.dve(lambda e, i=i, k=k: e.scalar_tensor_tensor(out=jk[:], in0=sb_all[:, i, :], scalar=m8_all[:, i, k:k + 1], in1=scr[:], op0=ALU.is_equal, op1=ALU.mult, accum_out=s8[:, k:k + 1]),
                      reads=["sb_all%d" % i, "m8_all%d" % i, "scr"], writes=["s8_%d" % k])
            S.dve(lambda e: e.tensor_reduce(out=s8[:, 8:9], in_=s8[:, 0:8], axis=AX.X, op=ALU.add), reads=["s8_%d" % k for k in range(8)], writes=["s8s"])
            S.dve(lambda e: e.reciprocal(out=s8[:, 9:10], in_=s8[:, 8:9]), reads=["s8s"], writes=["s8r"])
            S.dve(lambda e, i=i: e.tensor_scalar(out=w8_all[:, i, :], in0=s8[:, 0:8], scalar1=s8[:, 9:10], scalar2=2.5, op0=ALU.mult, op1=ALU.mult),
                  reads=["s8r"] + ["s8_%d" % k for k in range(8)], writes=["w8_all%d" % i])

            def fsh(e, j=j):
                ins = None
                for m in range(4):
                    for kc in range(8):
                        ins = e.matmul(PS[3][:, m * 128:(m + 1) * 128], lhsT=wsg[:, kc, m * 128:(m + 1) * 128], rhs=h2T[j][:, kc, :], start=(kc == 0), stop=(kc == 7))
                return ins
            S.pe(fsh, reads=["h2T%d" % j, "wsg_a", "wsg_b"], writes=["ps3"])
            S.act(lambda e: e.activation(out=sgl[:], in_=PS[3][:, 0:256], func=AF.Sigmoid), reads=["ps3"], writes=["sgl"])
            S.dve(lambda e: e.tensor_tensor(out=sgl[:], in0=PS[3][:, 0:256], in1=sgl[:], op=ALU.mult), reads=["ps3", "sgl"], writes=["sgl"])
            S.dve(lambda e: e.tensor_tensor(out=actT[:].rearrange("p m t -> p (m t)"), in0=PS[3][:, 256:512], in1=sgl[:], op=ALU.mult), reads=["ps3", "sgl"], writes=["actT"])

            def frk(e):
                e.matmul(PS[2][:, 0:256], lhsT=tri_b[:], rhs=maskb[:], start=True, stop=True)
                return e.matmul(PS[2][:, 256:512], lhsT=ones_b[:], rhs=maskb[:], start=True, stop=True)
            S.pe(frk, reads=["tri_b", "ones_b", "maskb"], writes=["ps2"])
            S.dve(lambda e, i=i: e.tensor_tensor(out=pos_all[:, i, :], in0=PS[2][:, 0:256], in1=carry[:], op=ALU.add), reads=["ps2", "carry"], writes=["pos_all%d" % i])
            S.dve(lambda e: e.tensor_tensor(out=carry[:], in0=PS[2][:, 256:512], in1=carry[:], op=ALU.add), reads=["ps2", "carry"], writes=["carry"])

            def fsd(e):
                ins = None
                for half in range(2):
                    for m in range(2):
                        ins = e.matmul(PS[4 + half][:, :], lhsT=actT[:, m, :], rhs=wsd[:, m, half * 512:(half + 1) * 512], start=(m == 0), stop=(m == 1))
                return ins
            S.pe(fsd, reads=["actT", "wsd"], writes=["ps4", "ps5"])
            for half in range(2):
                hs = slice(half * 512, (half + 1) * 512)
                S.dve(lambda e, j=j, half=half, hs=hs: e.tensor_tensor(out=yt[j][:, hs], in0=PS[4 + half][:, :], in1=gate2_bc[:, hs], op=ALU.mult),
                      reads=["ps%d" % (4 + half), "gate2_bc"], writes=["yt%d_%d" % (j, half)])
            S.dve(lambda e, j=j, i=i: e.tensor_tensor(out=yt[j][:], in0=yt[j][:], in1=x1t[i % NX1][:], op=ALU.add),
                  reads=["yt%d_0" % j, "yt%d_1" % j, "x1u%d" % (i % NX1)], writes=["yt%d_0" % j, "yt%d_1" % j])
            S.dma("sp", lambda e, i=i, j=j: e.dma_start(out=x2p_d[i * 128:(i + 1) * 128, :], in_=yt[j][:]), reads=["yt%d_0" % j, "yt%d_1" % j], writes=["x2p_d%d" % i])

        stP(0)
        for i in range(NT):
            if i + 1 < NT:
                stP(i + 1)
            stQ(i)
        A.release(m5)
        S.barrier()
        if stage <= 4:
            zt = A.alloc("zt", [128, D], F32)
            S.dve(lambda e: e.memset(zt[:], 0.0), writes=["zt"])
            S.dma("sp", lambda e: e.dma_start(out=out[0:128, :], in_=zt[:]), reads=["zt"])
            S.emit(st)
            return nc, S

        PST = {}

        def bcreg(e):
            if "bc" not in PST:
                PST["bc"] = e.alloc_register("bc")
                e.reg_mov(PST["bc"], NBLK * 128 - 1)
            return PST["bc"]

        A.release(route_mark)
        cntp = A.alloc("cntp", [128, 256], F32)
        nbf = A.alloc("nbf", [128, 256], F32)
        nbi = A.alloc("nbi", [128, 256], I32)
        tmpa = A.alloc("tmpa", [128, 256], F32)
        tmpb = A.alloc("tmpb", [128, 256], F32)
        padded = A.alloc("padded", [128, 256], F32)
        pend = A.alloc("pend", [128, 256], F32)
        pstart = A.alloc("pstart", [128, 256], F32)
        ones256 = A.alloc("ones256", [128, 256], F32)
        S.dve(lambda e: e.memset(ones256[:], 1.0), writes=["ones256"])
        S.dve(lambda e: e.tensor_scalar(out=cntp[:], in0=carry[:], scalar1=63.5, scalar2=1.0 / 128, op0=ALU.add, op1=ALU.mult), reads=["carry"], writes=["cntp"])
        S.dve(lambda e: e.tensor_copy(out=nbi[:], in_=cntp[:]), reads=["cntp"], writes=["nbi"])
        S.dve(lambda e: e.tensor_copy(out=nbf[:], in_=nbi[:]), reads=["nbi"], writes=["nbf"])
        S.dve(lambda e: e.scalar_tensor_tensor(out=tmpa[:], in0=nbf[:], scalar=128.0, in1=carry[:], op0=ALU.mult, op1=ALU.is_lt), reads=["nbf", "carry"], writes=["tmpa"])
        S.dve(lambda e: e.tensor_scalar(out=tmpb[:], in0=nbf[:], scalar1=128.0, scalar2=-128.0, op0=ALU.mult, op1=ALU.add), reads=["nbf"], writes=["tmpb"])
        S.dve(lambda e: e.tensor_tensor(out=tmpb[:], in0=tmpb[:], in1=carry[:], op=ALU.is_ge), reads=["tmpb", "carry"], writes=["tmpb"])
        S.dve(lambda e: e.tensor_tensor(out=nbf[:], in0=nbf[:], in1=tmpa[:], op=ALU.add), reads=["nbf", "tmpa"], writes=["nbf"])
        S.dve(lambda e: e.tensor_tensor(out=nbf[:], in0=nbf[:], in1=tmpb[:], op=ALU.subtract), reads=["nbf", "tmpb"], writes=["nbf"])
        S.dve(lambda e: e.tensor_scalar(out=padded[:], in0=nbf[:], scalar1=128.0, scalar2=None, op0=ALU.mult), reads=["nbf"], writes=["padded"])
        S.dve(lambda e: e.tensor_tensor_scan(out=pend[:], data0=ones256[:], data1=padded[:], initial=0.0, op0=ALU.mult, op1=ALU.add), reads=["ones256", "padded"], writes=["pend"])
        S.dve(lambda e: e.tensor_tensor(out=pstart[:], in0=pend[:], in1=padded[:], op=ALU.subtract), reads=["pend", "padded"], writes=["pstart"])
        bvi = A.alloc("bvi", [128, 4], I32)
        bvf = A.alloc("bvf", [128, 4], F32)
        blkf = A.alloc("blkf", [128, 8], F32)
        blki = A.alloc("blki", [128, 8], I32)
        S.pool(lambda e: e.iota(bvi[:], pattern=[[128 * 128, 4]], base=0, channel_multiplier=128), writes=["bvi"])
        S.dve(lambda e: e.tensor_copy(out=bvf[:], in_=bvi[:]), reads=["bvi"], writes=["bvf"])
        for jb in range(4):
            S.dve(lambda e, jb=jb: e.tensor_scalar(out=tmpa[:], in0=pend[:], scalar1=bvf[:, jb:jb + 1], scalar2=0.0, op0=ALU.is_le, op1=ALU.add, accum_out=blkf[:, jb:jb + 1]),
                  reads=["pend", "bvf", "tmpa"], writes=["tmpa", "blkf%d" % jb])
        S.dve(lambda e: e.tensor_scalar(out=blkf[:, 0:4], in0=blkf[:, 0:4], scalar1=255.0, scalar2=None, op0=ALU.min), reads=["blkf%d" % jb for jb in range(4)], writes=["blkfa"])
        S.dve(lambda e: e.tensor_tensor(out=blkf[:, 4:8], in0=bvf[:], in1=pend[:, 255:256].to_broadcast([128, 4]), op=ALU.is_lt), reads=["bvf", "pend"], writes=["blkfb"])
        S.dve(lambda e: e.tensor_copy(out=blki[:], in_=blkf[:]), reads=["blkfa", "blkfb"], writes=["blki"])
        for r_ in range(2):
            S.dma("sp", lambda e, r_=r_: e.dma_start(out=blk_d[r_].rearrange("(j p) -> p j", p=128), in_=blki[:, r_ * 4:(r_ + 1) * 4]), reads=["blki"], writes=["blk_d%d" % r_])
        blkrow = A.alloc("blkrow", [1, 2 * NBLK], I32)
        S.dma("sp", lambda e: e.dma_start(out=blkrow[:], in_=blk_d.rearrange("r b -> (r b)").unsqueeze(0)), reads=["blk_d0", "blk_d1"], writes=["blkrow"])
        d8f = A.alloc("d8f", [128, 8], F32)
        d8i_all = A.alloc("d8i_all", [128, NT, 8], I32)
        keyt = A.alloc("keyt", [128, 256], F32)
        tmpk = [A.alloc("tmpk%d" % i, [128, 256], F32) for i in range(2)]
        h2g = [A.alloc("h2g%d" % i, [128, D], BF16) for i in range(3)]
        plan_mark = A.mark()
        for i in range(NT):
            j = i % 3
            S.dma("sp", lambda e, i=i, j=j: e.dma_start(out=h2g[j][:], in_=h2_d[i * 128:(i + 1) * 128, :]), reads=["h2_d%d" % i], writes=["h2g%d" % j])
            S.dve(lambda e, i=i: e.tensor_tensor(out=keyt[:], in0=pos_all[:, i, :], in1=pstart[:], op=ALU.add), reads=["pos_all%d" % i, "pstart"], writes=["keyt"])
            for k in range(8):
                tk = tmpk[k % 2]
                S.dve(lambda e, i=i, k=k, tk=tk: e.scalar_tensor_tensor(out=tk[:], in0=sb_all[:, i, :], scalar=m8_all[:, i, k:k + 1], in1=keyt[:], op0=ALU.is_equal, op1=ALU.mult),
                      reads=["sb_all%d" % i, "m8_all%d" % i, "keyt"], writes=["tmpk%d" % (k % 2)])
                S.dve(lambda e, k=k, tk=tk: e.tensor_reduce(out=d8f[:, k:k + 1], in_=tk[:], axis=AX.X, op=ALU.max), reads=["tmpk%d" % (k % 2)], writes=["d8f%d" % k])
            S.dve(lambda e, i=i: e.tensor_copy(out=d8i_all[:, i, :], in_=d8f[:]), reads=["d8f%d" % k for k in range(8)], writes=["d8i%d" % i])
            for k in range(8):
                S.dma("pool", lambda e, i=i, j=j, k=k: e.indirect_dma_start(
                    out=xs_d, out_offset=bass.IndirectOffsetOnAxis(ap=d8i_all[:, i, k:k + 1], axis=0),
                    in_=h2g[j][:], in_offset=None, bounds_check=bcreg(e), oob_is_err=False),
                    reads=["h2g%d" % j, "d8i%d" % i], writes=["xs_sc_%d_%d" % (i, k)])
        if debug:
            S.dma("sp", lambda e: e.dma_start(out=dbg_d8, in_=d8i_all[:].rearrange("p a b -> p (a b)")), reads=["d8i%d" % i for i in range(NT)])
            S.dma("sp", lambda e: e.dma_start(out=dbg_w8, in_=w8_all[:].rearrange("p a b -> p (a b)")), reads=["w8_all%d" % i for i in range(NT)])
        S.barrier()
        if stage <= 5:
            zt = A.alloc("zt", [128, D], F32)
            S.dve(lambda e: e.memset(zt[:], 0.0), writes=["zt"])
            S.dma("sp", lambda e: e.dma_start(out=out[0:128, :], in_=zt[:]), reads=["zt"])
            S.emit(st)
            return nc, S

        A.release(plan_mark)
        blkbc_i = A.alloc("blkbc_i", [128, 2 * NBLK], I32)
        blkbc_f = A.alloc("blkbc_f", [128, 2 * NBLK], F32)
        idxw_f = A.alloc("idxw_f", [128, NBLK], F32)
        idxw = A.alloc("idxw", [128, NBLK], I32)
        pcol_i = A.alloc("pcol_i", [128, 1], I32)
        pcol = A.alloc("pcol", [128, 1], F32)
        S.dma("sp", lambda e: e.dma_start(out=blkbc_i[:], in_=blk_d.rearrange("r b -> (r b)").partition_broadcast(128)), reads=["blk_d0", "blk_d1"], writes=["blkbc_i"])
        S.dve(lambda e: e.tensor_copy(out=blkbc_f[:], in_=blkbc_i[:]), reads=["blkbc_i"], writes=["blkbc_f"])
        S.pool(lambda e: e.iota(pcol_i[:], pattern=[[0, 1]], base=0, channel_multiplier=1), writes=["pcol_i"])
        S.dve(lambda e: e.tensor_copy(out=pcol[:], in_=pcol_i[:]), reads=["pcol_i"], writes=["pcol"])
        S.dve(lambda e: e.tensor_scalar(out=idxw_f[:], in0=blkbc_f[:, 0:NBLK], scalar1=128.0, scalar2=pcol[:, 0:1], op0=ALU.mult, op1=ALU.add), reads=["blkbc_f", "pcol"], writes=["idxw_f"])
        S.dve(lambda e: e.tensor_scalar(out=blkbc_f[:, NBLK:2 * NBLK], in0=blkbc_f[:, NBLK:2 * NBLK], scalar1=-1.0e6, scalar2=1.0e6, op0=ALU.mult, op1=ALU.add), reads=["blkbc_f"], writes=["blkbc_f"])
        S.dve(lambda e: e.tensor_tensor(out=idxw_f[:], in0=idxw_f[:], in1=blkbc_f[:, NBLK:2 * NBLK], op=ALU.add), reads=["idxw_f", "blkbc_f"], writes=["idxw_f"])
        idxw2 = A.alloc("idxw2", [128, NBLK], I32)
        gef = A.alloc("gef", [128, NBLK], F32)
        S.dve(lambda e: e.tensor_scalar(out=gef[:], in0=idxw_f[:], scalar1=16384.0, scalar2=1.0e6, op0=ALU.is_ge, op1=ALU.mult), reads=["idxw_f"], writes=["gef"])
        S.dve(lambda e: e.tensor_tensor(out=blkbc_f[:, 0:NBLK], in0=idxw_f[:], in1=gef[:], op=ALU.add), reads=["idxw_f", "gef", "blkbc_f"], writes=["blkbc_f"])
        S.dve(lambda e: e.tensor_copy(out=idxw[:], in_=blkbc_f[:, 0:NBLK]), reads=["blkbc_f"], writes=["idxw"])
        S.dve(lambda e: e.tensor_scalar(out=idxw_f[:], in0=idxw_f[:], scalar1=1.0e6 - 16384.0, scalar2=None, op0=ALU.add), reads=["idxw_f", "blkbc_f"], writes=["idxw_f"])
        S.dve(lambda e: e.tensor_tensor(out=idxw_f[:], in0=idxw_f[:], in1=gef[:], op=ALU.subtract), reads=["idxw_f", "gef"], writes=["idxw_f"])
        S.dve(lambda e: e.tensor_copy(out=idxw2[:], in_=idxw_f[:]), reads=["idxw_f"], writes=["idxw2"])
        convert_experts(256)
        NWB = 6
        NXB = 6
        PFD = NWB - 1
        A2 = Arena(nc)
        A2.off, A2.limit, A2.n = off_sb, off_sb + 2 * NT * 256 * 4, 5000
        wbuf = [A2.alloc("wbuf%d" % i, [128, 6144], BF16) for i in range(5)] + [A.alloc("wbuf%d" % i, [128, 6144], BF16) for i in range(5, NWB)]
        for i in range(NWB):
            S.pool(lambda e, i=i: e.memset(wbuf[i][:], 0.0), writes=["wbuf%dh0" % i, "wbuf%dh1" % i])
        Xb = [A.alloc("Xb%d" % i, [128, D], BF16) for i in range(NXB)]
        XT = [A.alloc("XT%d" % i, [128, 8, 128], BF16) for i in range(2)]
        Yb = [A.alloc("Yb%d" % i, [128, D], BF16) for i in range(2)]
        actE = [A.alloc("actE%d" % i, [128, 2, 128], BF16) for i in range(2)]
        sglE = [A.alloc("sglE%d" % i, [128, 256], F32) for i in range(2)]
        wreg = {}
        W16N = ["w16_%d" % e_ for e_ in range(256)]

        def wbound(e):
            if "r" not in wreg:
                wreg["r"] = e.alloc_register("wbnd")
                e.reg_mov(wreg["r"], 128 * 128 - 1)
            return wreg["r"]
        nblk_run = int(os.environ.get("KNBLK", str(NBLK)))

        def st_wload(b):
            s_ = b % NWB
            for hf, ix in ((0, idxw), (1, idxw2)):
                S.dma("pool", lambda e, hf=hf, ix=ix: e.indirect_dma_start(out=wbuf[s_][:], out_offset=None, in_=w16_d[hf],
                                                                         in_offset=bass.IndirectOffsetOnAxis(ap=ix[:, b:b + 1], axis=0), bounds_check=wbound(e), oob_is_err=False),
                      reads=["idxw", "idxw2"] + W16N, writes=["wbuf%dh%d" % (s_, hf)])

        def st_xload(b):
            jx = b % NXB
            if os.environ.get("KNOX", "0") == "1" and b > 8:
                return
            S.dma("sp", lambda e: e.dma_start(out=Xb[jx][:], in_=xs_d[b * 128:(b + 1) * 128, :]), writes=["Xb%d" % jx])

        def st_A(b):
            j, jx = b % 2, b % NXB

            def fxt(e):
                ins = None
                for kc in range(8):
                    ins = e.transpose(out=PSB[j][:, kc * 128:(kc + 1) * 128], in_=Xb[jx][:, kc * 128:(kc + 1) * 128], identity=ident_b[:])
                return ins
            S.pe(fxt, reads=["Xb%d" % jx, "ident_b"], writes=["ps%d" % j])
            S.act(lambda e: e.activation(out=XT[j][:].rearrange("p k t -> p (k t)"), in_=PSB[j][:, 0:1024], func=AF.Copy), reads=["ps%d" % j], writes=["XT%d" % j])

        def st_B(b):
            j, s_ = b % 2, b % NWB
            gb_ = 2 + j

            def fgu(e):
                ins = None
                for m in range(4):
                    base = (0 if m < 2 else 2048) + (m % 2) * 128
                    for kc in range(8):
                        ins = e.matmul(PS[gb_][:, m * 128:(m + 1) * 128], lhsT=wbuf[s_][:, base + kc * 256:base + kc * 256 + 128], rhs=XT[j][:, kc, :], start=(kc == 0), stop=(kc == 7))
                return ins
            S.pe(fgu, reads=["XT%d" % j, "wbuf%dh0" % s_, "wbuf%dh1" % s_], writes=["ps%d" % gb_])
            S.act(lambda e: e.activation(out=sglE[j][:], in_=PS[gb_][:, 0:256], func=AF.Silu), reads=["ps%d" % gb_], writes=["sglE%d" % j])
            S.dve(lambda e: e.tensor_tensor(out=actE[j][:].rearrange("p m t -> p (m t)"), in0=PS[gb_][:, 256:512], in1=sglE[j][:], op=ALU.mult), reads=["ps%d" % gb_, "sglE%d" % j], writes=["actE%d" % j])

        def st_C(b):
            j, s_ = b % 2, b % NWB
            yb_ = 4 + 2 * j

            def fy(e):
                ins = None
                for half in range(2):
                    for m in range(2):
                        ins = e.matmul(PS[yb_ + half][:, :], lhsT=actE[j][:, m, :], rhs=wbuf[s_][:, 4096 + m * 1024 + half * 512:4096 + m * 1024 + (half + 1) * 512], start=(m == 0), stop=(m == 1))
                return ins
            S.pe(fy, reads=["actE%d" % j, "wbuf%dh0" % s_, "wbuf%dh1" % s_], writes=["ps%d" % yb_, "ps%d" % (yb_ + 1)])
            S.act(lambda e: e.activation(out=Yb[j][:, 0:512], in_=PS[yb_][:, :], func=AF.Copy), reads=["ps%d" % yb_], writes=["Yb%d_0" % j])
            S.dve(lambda e: e.tensor_copy(out=Yb[j][:, 512:1024], in_=PS[yb_ + 1][:, :]), reads=["ps%d" % (yb_ + 1)], writes=["Yb%d_1" % j])
            if os.environ.get("KNOX", "0") == "1" and b > 8:
                return
            S.dma("sp", lambda e: e.dma_start(out=ys_d[b * 128:(b + 1) * 128, :], in_=Yb[j][:]), reads=["Yb%d_0" % j, "Yb%d_1" % j], writes=["ys_st_%d" % b])

        for b in range(min(PFD, nblk_run)):
            st_wload(b)
        for b in range(min(PFD, nblk_run)):
            st_xload(b)
        for t in range(-1, nblk_run + 1):
            if 0 <= t + 1 < nblk_run:
                st_A(t + 1)
            if 0 <= t < nblk_run:
                st_B(t)
            if 0 <= t - 1 < nblk_run:
                st_C(t - 1)
            if t >= 0 and t + PFD < nblk_run:
                st_wload(t + PFD)
                st_xload(t + PFD)
        S.barrier()
        A.release(plan_mark)
        fg_bc = A.alloc("fg_bc", [128, D], F32)
        S.dma("sp", lambda e: e.dma_start(out=fg_bc[:], in_=final_g.partition_broadcast(128)), writes=["fg_bc"])
        Gk = [[A.alloc("G%d_%d" % (jj, k), [128, D], BF16) for k in range(8)] for jj in range(2)]
        accA = [A.alloc("accA%d" % i, [128, D], F32) for i in range(2)]
        xpt = [A.alloc("xpt%d" % i, [128, D], F32) for i in range(2)]
        junkf = A.alloc("junkf", [128, D], F32)
        ssf = [A.alloc("ssf%d" % i, [128, 4], F32) for i in range(2)]
        for jj in range(2):
            for k in range(8):
                S.pool(lambda e, jj=jj, k=k: e.memset(Gk[jj][k][:], 0.0), writes=["G%d_%d" % (jj, k)])
        for i in range(NT):
            jj = i % 2
            S.dma("sp", lambda e, i=i, jj=jj: e.dma_start(out=xpt[jj][:], in_=x2p_d[i * 128:(i + 1) * 128, :]), reads=["x2p_d%d" % i], writes=["xpt%d" % jj])
            for k in range(8):
                S.dma("pool", lambda e, i=i, jj=jj, k=k: e.indirect_dma_start(out=Gk[jj][k][:], out_offset=None, in_=ys_d,
                                                                            in_offset=bass.IndirectOffsetOnAxis(ap=d8i_all[:, i, k:k + 1], axis=0), bounds_check=bcreg(e), oob_is_err=False),
                      reads=["d8i%d" % i, "G%d_%d" % (jj, k)], writes=["G%d_%d" % (jj, k)])
            an = "accA%d" % jj
            S.dve(lambda e, i=i, jj=jj: e.tensor_scalar(out=accA[jj][:], in0=Gk[jj][0][:], scalar1=w8_all[:, i, 0:1], scalar2=None, op0=ALU.mult), reads=["G%d_0" % jj, "w8_all%d" % i], writes=[an])
            for k in range(1, 8):
                S.dve(lambda e, i=i, jj=jj, k=k: e.scalar_tensor_tensor(out=accA[jj][:], in0=Gk[jj][k][:], scalar=w8_all[:, i, k:k + 1], in1=accA[jj][:], op0=ALU.mult, op1=ALU.add),
                      reads=["G%d_%d" % (jj, k), "w8_all%d" % i, an], writes=[an])
            S.dve(lambda e, jj=jj: e.tensor_tensor(out=accA[jj][:], in0=accA[jj][:], in1=gate2_bc[:], op=ALU.mult), reads=[an, "gate2_bc"], writes=[an])
            S.dve(lambda e, jj=jj: e.tensor_tensor(out=accA[jj][:], in0=accA[jj][:], in1=xpt[jj][:], op=ALU.add), reads=[an, "xpt%d" % jj], writes=[an])
            sn = "ssf%d" % jj
            S.act(lambda e, jj=jj: e.activation(out=junkf[:], in_=accA[jj][:], func=AF.Square, accum_out=ssf[jj][:, 0:1]), reads=[an], writes=["junkf", sn])
            S.dve(lambda e, jj=jj: e.tensor_scalar(out=ssf[jj][:, 1:2], in0=ssf[jj][:, 0:1], scalar1=1.0 / D, scalar2=EPS, op0=ALU.mult, op1=ALU.add), reads=[sn], writes=[sn + "b"])
            S.act(lambda e, jj=jj: e.activation(out=ssf[jj][:, 2:3], in_=ssf[jj][:, 1:2], func=AF.Sqrt), reads=[sn + "b"], writes=[sn + "c"])
            S.dve(lambda e, jj=jj: e.reciprocal(out=ssf[jj][:, 3:4], in_=ssf[jj][:, 2:3]), reads=[sn + "c"], writes=[sn + "d"])
            S.dve(lambda e, jj=jj: e.scalar_tensor_tensor(out=accA[jj][:], in0=accA[jj][:], scalar=ssf[jj][:, 3:4], in1=fg_bc[:], op0=ALU.mult, op1=ALU.mult),
                  reads=[an, sn + "d", "fg_bc"], writes=[an])
            S.dma("sp", lambda e, i=i, jj=jj: e.dma_start(out=out[i * 128:(i + 1) * 128, :], in_=accA[jj][:]), reads=[an], writes=["out%d" % i])
        S.emit(st)
    return nc, S


_CACHE = {}


def pack_expert_weights(wg, wu, wd):
    o = np.empty((256, 128, 6144), np.float32)
    o[:, :, 0:2048] = wg.reshape(256, 8, 128, 256).transpose(0, 2, 1, 3).reshape(256, 128, 2048)
    o[:, :, 2048:4096] = wu.reshape(256, 8, 128, 256).transpose(0, 2, 1, 3).reshape(256, 128, 2048)
    o[:, :, 4096:6144] = wd.reshape(256, 2, 128, 1024).transpose(0, 2, 1, 3).reshape(256, 128, 2048)
    return o.reshape(256 * 128, 6144)


def kernel(**inputs):
    n = 8
    if "nc" not in _CACHE:
        _CACHE["nc"] = build(stage=int(os.environ.get("KSTAGE", "99")))[0]
    nc = _CACHE["nc"]
    shared = {k: np.ascontiguousarray(v[0]) for k, v in inputs.items()
              if k not in ("x", "c", "lb_logits", "final_g", "w_exp_gate", "w_exp_up", "w_exp_down")}
    shared["w_exp_all"] = pack_expert_weights(inputs["w_exp_gate"][0], inputs["w_exp_up"][0], inputs["w_exp_down"][0])
    shared["lb_logits"] = np.ascontiguousarray(inputs["lb_logits"])
    shared["final_g"] = np.ascontiguousarray(inputs["final_g"])
    in_maps = []
    for b in range(n):
        m = dict(shared)
        m["x"] = np.ascontiguousarray(inputs["x"][b])
        m["c"] = np.ascontiguousarray(inputs["c"][b])
        in_maps.append(m)
    res = run_bass_kernel_spmd(nc, in_maps, core_ids=list(range(n)))
    return np.stack([r["out"] for r in res.results], axis=0)
```

```python
import os
from contextlib import ExitStack
import numpy as np
import concourse.bass as bass
import concourse.mybir as mybir
from concourse.bass_utils import run_bass_kernel_spmd

F32 = mybir.dt.float32
BF16 = mybir.dt.bfloat16
I32 = mybir.dt.int32
AF = mybir.ActivationFunctionType
ALU = mybir.AluOpType
AX = mybir.AxisListType

D = 1024
SEQ = 4096
NT = SEQ // 128
EPS = 1e-6
NBLK = 512


class Sched:
    ENGS = ("pe", "act", "dve", "pool", "sp")
    DMA_RING = {"sp": 12, "pool": 12, "act": 6}

    def __init__(self, nc):
        self.nc = nc
        self.ops = []

    def op(self, eng, fn, reads=(), writes=(), dma=False, extra=(), nobar=False):
        self.ops.append(dict(eng=eng, fn=fn, reads=tuple(reads), writes=tuple(writes), dma=dma, extra=tuple(extra), nobar=nobar))
        return len(self.ops) - 1

    def pe(self, fn, reads=(), writes=()):
        return self.op("pe", fn, reads, writes)

    def act(self, fn, reads=(), writes=()):
        return self.op("act", fn, reads, writes)

    def dve(self, fn, reads=(), writes=()):
        return self.op("dve", fn, reads, writes)

    def pool(self, fn, reads=(), writes=()):
        return self.op("pool", fn, reads, writes)

    def dma(self, eng, fn, reads=(), writes=(), nobar=False):
        return self.op(eng, fn, reads, writes, dma=True, nobar=nobar)

    def barrier(self):
        n = len(self.ops)
        lastc = {}
        dmas = []
        for i, o in enumerate(self.ops):
            if o["dma"]:
                if not o["nobar"]:
                    dmas.append(i)
            else:
                lastc[o["eng"]] = i
        start = getattr(self, "_bar_from", 0)
        ex = [i for i in dmas if i >= start] + list(lastc.values())
        for e in self.ENGS:
            self.op(e, lambda eng: eng.nop(), extra=ex)
        self._bar_from = len(self.ops)

    def emit(self, stack):
        nc = self.nc
        ops = self.ops
        n = len(ops)
        last_w, readers, deps = {}, {}, [None] * n
        for i, o in enumerate(ops):
            d = set(o["extra"])
            for b in o["reads"]:
                w = last_w.get(b)
                if w is not None:
                    d.add(w)
            for b in o["writes"]:
                w = last_w.get(b)
                if w is not None:
                    d.add(w)
                d.update(readers.get(b, ()))
            d.discard(i)
            for b in o["reads"]:
                readers.setdefault(b, []).append(i)
            for b in o["writes"]:
                last_w[b] = i
                readers[b] = []
            deps[i] = d
        signal = [False] * n
        for i, o in enumerate(ops):
            for j in deps[i]:
                pj = ops[j]
                if pj["dma"]:
                    continue
                if pj["eng"] != o["eng"] or pj["eng"] != "pe":
                    signal[j] = True
        seq = [0] * n
        cnt = {e: 0 for e in self.ENGS}
        dcnt = {e: 0 for e in self.ENGS}
        dnum = [0] * n
        for i, o in enumerate(ops):
            if o["dma"]:
                dnum[i] = dcnt[o["eng"]]
                dcnt[o["eng"]] += 1
            elif signal[i]:
                cnt[o["eng"]] += 1
                seq[i] = cnt[o["eng"]]
        esem = {e: stack.enter_context(nc.semaphore("S_" + e)) for e in self.ENGS}
        dsem = {}
        for e, r in self.DMA_RING.items():
            if dcnt[e] > 0:
                dsem[e] = [stack.enter_context(nc.semaphore("D_%s%d" % (e, k))) for k in range(r)]
        self.stats = dict(n_ops=n, signals=dict(cnt), dmas=dict(dcnt))
        per_eng = {e: [i for i, o in enumerate(ops) if o["eng"] == e] for e in self.ENGS}
        RING = self.DMA_RING

        def run_engine(e, eng):
            known = {x: 0 for x in self.ENGS}
            dknown = {}
            for i in per_eng[e]:
                o = ops[i]
                wc, wd = {}, {}
                for j in deps[i]:
                    pj = ops[j]
                    if pj["dma"]:
                        r = RING[pj["eng"]]
                        key = (pj["eng"], dnum[j] % r)
                        v = 16 * (dnum[j] // r + 1)
                        if dknown.get(key, 0) < v:
                            wd[key] = max(wd.get(key, 0), v)
                    else:
                        if pj["eng"] == e and e == "pe":
                            continue
                        if known[pj["eng"]] < seq[j]:
                            wc[pj["eng"]] = max(wc.get(pj["eng"], 0), seq[j])
                if o["dma"]:
                    r = RING[e]
                    if dnum[i] >= r:
                        key = (e, dnum[i] % r)
                        v = 16 * (dnum[i] // r)
                        if dknown.get(key, 0) < v:
                            wd[key] = max(wd.get(key, 0), v)
                for x, v in wc.items():
                    eng.wait_ge(esem[x], v)
                    known[x] = v
                for key, v in wd.items():
                    eng.wait_ge(dsem[key[0]][key[1]], v)
                    dknown[key] = v
                ins = o["fn"](eng)
                if o["dma"]:
                    ins.then_inc(dsem[e][dnum[i] % RING[e]], 16)
                elif signal[i]:
                    ins.then_inc(esem[e], 1)
            if dcnt[e] > 0:
                r = RING[e]
                for k in range(min(r, dcnt[e])):
                    last = ((dcnt[e] - 1 - k) // r) * r + k
                    v = 16 * (last // r + 1)
                    if dknown.get((e, k), 0) < v:
                        eng.wait_ge(dsem[e][k], v)

        with nc.Block() as block:

            @block.tensor
            def _(eng):
                run_engine("pe", eng)

            @block.scalar
            def _(eng):
                run_engine("act", eng)

            @block.vector
            def _(eng):
                run_engine("dve", eng)

            @block.gpsimd
            def _(eng):
                run_engine("pool", eng)

            @block.sync
            def _(eng):
                run_engine("sp", eng)


class Arena:
    def __init__(self, nc, limit=228 * 1024):
        self.nc, self.off, self.limit, self.n = nc, 17 * 1024, limit, 0

    def alloc(self, name, shape, dt):
        esz = 4 if dt in (F32, I32) else 2
        nbytes = int(np.prod(shape[1:])) * esz
        self.off = (self.off + 31) // 32 * 32
        self.n += 1
        t = self.nc.alloc_sbuf_tensor_at("%s_%d" % (name, self.n), list(shape), dt, offset=self.off)
        self.off += nbytes
        assert self.off <= self.limit, ("SBUF overflow", name, self.off)
        return t

    def mark(self):
        return self.off

    def release(self, m):
        self.off = m


def build(stage=99, debug=False):
    nc = bass.Bass("TRN2", target_bir_lowering=False)
    kin = "ExternalInput"

    def din(name, shape):
        return nc.dram_tensor(name, list(shape), F32, kind=kin).ap()

    x = din("x", [SEQ, D])
    c = din("c", [D])
    ada_w = din("ada_w", [D, 6 * D])
    ada_b = din("ada_b", [6 * D])
    norm1_g = din("norm1_g", [D])
    w_in = din("w_in", [D, 6400])
    lb_logits = din("lb_logits", [2, 512])
    hg_norm_g = din("hg_norm_g", [512])
    w_branch_a = din("w_branch_a", [512, D])
    w_branch_b = din("w_branch_b", [256, D])
    w_out = din("w_out", [D, D])
    norm2_g = din("norm2_g", [D])
    w_router = din("w_router", [D, 256])
    router_bias = din("router_bias", [256])
    w_exp_all = din("w_exp_all", [256 * 128, 6144])
    w_sh_gate = din("w_sh_gate", [D, 256])
    w_sh_up = din("w_sh_up", [D, 256])
    w_sh_down = din("w_sh_down", [256, D])
    final_g = din("final_g", [D])
    out = nc.dram_tensor("out", [SEQ, D], F32, kind="ExternalOutput").ap()
    skind = "ExternalOutput" if debug else "Internal"
    mod_d = nc.dram_tensor("mod_d", [48, 128], F32, kind=skind).ap()
    yaT_d = nc.dram_tensor("yaT_d", [4, 128, SEQ], BF16, kind=skind).ap()
    ybT_d = nc.dram_tensor("ybT_d", [2, 128, SEQ], BF16, kind=skind).ap()
    x2p_d = nc.dram_tensor("x2p_d", [SEQ, D], F32, kind=skind).ap()
    h2_d = nc.dram_tensor("h2_d", [SEQ, D], BF16, kind=skind).ap()
    xs_d = nc.dram_tensor("xs_d", [NBLK * 128, D], BF16, kind="Internal").ap()
    ys_d = nc.dram_tensor("ys_d", [NBLK * 128, D], BF16, kind="Internal").ap()
    blk_d = nc.dram_tensor("blk_d", [2, NBLK], I32, kind=skind).ap()
    w16_d = [nc.dram_tensor("w16_d%d" % i, [128 * 128, 6144], BF16, kind="Internal").ap() for i in range(2)]
    dbg_hT = nc.dram_tensor("dbg_hT", [8, 128, SEQ], BF16, kind="ExternalOutput").ap() if debug else None
    dbg_d8 = nc.dram_tensor("dbg_d8", [128, NT * 8], I32, kind="ExternalOutput").ap() if debug else None
    dbg_w8 = nc.dram_tensor("dbg_w8", [128, NT * 8], F32, kind="ExternalOutput").ap() if debug else None

    st = ExitStack()
    with st:
        st.enter_context(nc.allow_low_precision("bf16 matmul operands, fp32 accumulation"))
        st.enter_context(nc.allow_non_contiguous_dma("small strided parameter loads"))
        S = Sched(nc)
        A = Arena(nc)
        PS = [nc.alloc_psum_tensor("ps%d" % i, [128, 512], F32) for i in range(8)]
        CV = {"n": 0}

        def convert_experts(k):
            for _ in range(k):
                e_ = CV["n"]
                if e_ >= 256:
                    return
                CV["n"] += 1
                S.dma("pool", lambda e, e_=e_: e.dma_start(out=w16_d[e_ // 128][(e_ % 128) * 128:(e_ % 128 + 1) * 128, :], in_=w_exp_all[e_ * 128:(e_ + 1) * 128, :]),
                      writes=["w16_%d" % e_], nobar=True)
        PSB = [p[:].bitcast(BF16) for p in PS]

        ident_f = A.alloc("ident_f", [128, 128], F32)
        ident_b = A.alloc("ident_b", [128, 128], BF16)
        io_i = A.alloc("io_i", [128, 128], I32)
        io_f = A.alloc("io_f", [128, 128], F32)
        S.pool(lambda e: e.iota(io_i[:], pattern=[[1, 128]], base=0, channel_multiplier=-1), writes=["io_i"])
        S.dve(lambda e: e.tensor_copy(out=io_f[:], in_=io_i[:]), reads=["io_i"], writes=["io_f"])
        S.dve(lambda e: e.tensor_single_scalar(out=ident_f[:], in_=io_f[:], scalar=0.0, op=ALU.is_equal), reads=["io_f"], writes=["ident_f"])
        S.dve(lambda e: e.tensor_copy(out=ident_b[:], in_=ident_f[:]), reads=["ident_f"], writes=["ident_b"])
        ones_b = A.alloc("ones_b", [128, 128], BF16)
        S.dve(lambda e: e.memset(ones_b[:], 1.0), writes=["ones_b"])
        modT = A.alloc("modT", [128, 48], F32)
        gate1_bc = A.alloc("gate1_bc", [128, D], F32)
        gate2_bc = A.alloc("gate2_bc", [128, D], F32)
        a_bc = A.alloc("a_bc", [128, D], F32)
        sh_bc = A.alloc("sh_bc", [128, D], F32)
        hT = A.alloc("hT", [128, 8, SEQ], BF16)
        base_mark = A.mark()

        def HTn(i):
            return "hTt%d" % i

        m0 = A.mark()
        sc = A.alloc("sc", [128, 8], F32)
        crow = A.alloc("crow", [8, 128], F32)
        adab = A.alloc("adab", [48, 128], F32)
        S.dma("sp", lambda e: e.dma_start(out=crow[:], in_=c.rearrange("(k p) -> k p", p=128)), writes=["crow"])
        S.dma("sp", lambda e: e.dma_start(out=adab[:], in_=ada_b.rearrange("(k p) -> k p", p=128)), writes=["adab"])
        S.pe(lambda e: e.transpose(out=PS[1][:, 0:8], in_=crow[:, :], identity=ident_f[0:8, 0:8]), reads=["crow", "ident_f"], writes=["ps1"])
        S.act(lambda e: e.activation(out=sc[:], in_=PS[1][:, 0:8], func=AF.Silu), reads=["ps1"], writes=["sc"])
        awb = [A.alloc("awb%d" % i, [128, 8, 1024], F32) for i in range(2)]
        for pc in range(6):
            buf = awb[pc % 2]
            bn = "awb%d" % (pc % 2)
            S.dma("sp", lambda e, buf=buf, pc=pc: e.dma_start(
                out=buf[:], in_=ada_w[:, pc * 1024:(pc + 1) * 1024].rearrange("(k p) c -> p k c", p=128)), writes=[bn])

            def f(e, buf=buf, pc=pc):
                ins = None
                for j in range(8):
                    jc = pc * 8 + j
                    for k in range(8):
                        ins = e.matmul(PS[0][:, jc:jc + 1], lhsT=buf[:, k, j * 128:(j + 1) * 128], rhs=sc[:, k:k + 1],
                                       start=(k == 0), stop=(k == 7))
                return ins
            S.pe(f, reads=[bn, "sc"], writes=["ps0"])
        S.dve(lambda e: e.tensor_copy(out=modT[:], in_=PS[0][:, 0:48]), reads=["ps0"], writes=["modT"])
        S.pe(lambda e: e.transpose(out=PS[1][0:48, 0:128], in_=modT[:, 0:48], identity=ident_f[:]), reads=["modT", "ident_f"], writes=["ps1"])
        modrow = A.alloc("modrow", [48, 128], F32)
        S.dve(lambda e: e.tensor_tensor(out=modrow[:], in0=PS[1][0:48, 0:128], in1=adab[:], op=ALU.add), reads=["ps1", "adab"], writes=["modrow"])
        S.dma("sp", lambda e: e.dma_start(out=mod_d, in_=modrow[:]), reads=["modrow"], writes=["mod_d"])

        def mod_bc(dst, name, j0):
            src = mod_d[j0:j0 + 8, :].rearrange("a b -> (a b)").partition_broadcast(128)
            S.dma("sp", lambda e: e.dma_start(out=dst[:], in_=src), reads=["mod_d"], writes=[name])

        mod_bc(gate1_bc, "gate1_bc", 16)
        mod_bc(gate2_bc, "gate2_bc", 40)

        def load_norm_consts(gvec, j_shift, j_scale):
            mod_bc(sh_bc, "sh_bc", j_shift)
            mod_bc(a_bc, "a_bc", j_scale)
            gtmp = A.alloc("gtmp", [128, D], F32)
            S.dma("sp", lambda e: e.dma_start(out=gtmp[:], in_=gvec.partition_broadcast(128)), writes=["gtmp"])
            S.dve(lambda e: e.scalar_tensor_tensor(out=a_bc[:], in0=a_bc[:], scalar=1.0, in1=gtmp[:], op0=ALU.add, op1=ALU.mult),
                  reads=["a_bc", "gtmp"], writes=["a_bc"])

        def norm_mod_T(xt, xname, hb, hbname, ss, ssname, junk, junkname):
            S.act(lambda e: e.activation(out=junk[:], in_=xt[:], func=AF.Square, accum_out=ss[:, 0:1]),
                  reads=[xname], writes=[junkname, ssname])
            S.dve(lambda e: e.tensor_scalar(out=ss[:, 1:2], in0=ss[:, 0:1], scalar1=1.0 / D, scalar2=EPS, op0=ALU.mult, op1=ALU.add),
                  reads=[ssname], writes=[ssname + "b"])
            S.act(lambda e: e.activation(out=ss[:, 3:4], in_=ss[:, 1:2], func=AF.Sqrt),
                  reads=[ssname + "b"], writes=[ssname + "d"])
            S.dve(lambda e: e.reciprocal(out=ss[:, 2:3], in_=ss[:, 3:4]),
                  reads=[ssname + "d"], writes=[ssname + "c"])
            S.dve(lambda e: e.scalar_tensor_tensor(out=junk[:], in0=xt[:], scalar=ss[:, 2:3], in1=a_bc[:], op0=ALU.mult, op1=ALU.mult),
                  reads=[xname, ssname + "c", "a_bc", junkname], writes=[junkname])
            S.dve(lambda e: e.tensor_tensor(out=hb[:], in0=junk[:], in1=sh_bc[:], op=ALU.add),
                  reads=[junkname, "sh_bc"], writes=[hbname])

        if stage <= 0:
            zt = A.alloc("zt", [128, D], F32)
            S.dve(lambda e: e.memset(zt[:], 0.0), writes=["zt"])
            S.dma("sp", lambda e: e.dma_start(out=out[0:128, :], in_=zt[:]), reads=["zt"])
            S.emit(st)
            return nc, S
        load_norm_consts(norm1_g, 0, 8)
        xts = [A.alloc("xt%d" % i, [128, D], F32) for i in range(3)]
        junks = [A.alloc("junk%d" % i, [128, D], F32) for i in range(2)]
        hbs = [A.alloc("hb%d" % i, [128, D], BF16) for i in range(2)]
        ssq1 = A.alloc("ssq1", [128, NT], F32)
        rstd1 = A.alloc("rstd1", [128, NT], F32)
        for i in range(NT):
            xt, xn_ = xts[i % 3], "xt%d" % (i % 3)
            S.dma("sp", lambda e, xt=xt, i=i: e.dma_start(out=xt[:], in_=x[i * 128:(i + 1) * 128, :]), writes=[xn_])
            S.act(lambda e, xt=xt, i=i: e.activation(out=junks[i % 2][:], in_=xt[:], func=AF.Square, accum_out=ssq1[:, i:i + 1]),
                  reads=[xn_], writes=["junk%d" % (i % 2), "ssq1_%d" % i])
            convert_experts(1)
        S.dve(lambda e: e.tensor_scalar(out=rstd1[:], in0=ssq1[:], scalar1=1.0 / D, scalar2=EPS, op0=ALU.mult, op1=ALU.add), reads=["ssq1_%d" % i for i in range(NT)], writes=["rstd1a"])
        S.act(lambda e: e.activation(out=ssq1[:], in_=rstd1[:], func=AF.Sqrt), reads=["rstd1a"], writes=["ssq1b"])
        S.dve(lambda e: e.reciprocal(out=rstd1[:], in_=ssq1[:]), reads=["ssq1b", "rstd1a"], writes=["rstd1"])
        for i in range(NT):
            xt, xn_ = xts[i % 3], "xt%d" % (i % 3)
            S.dma("sp", lambda e, xt=xt, i=i: e.dma_start(out=xt[:], in_=x[i * 128:(i + 1) * 128, :]), writes=[xn_])
            j = i % 2
            S.dve(lambda e, xt=xt, i=i, j=j: e.scalar_tensor_tensor(out=junks[j][:], in0=xt[:], scalar=rstd1[:, i:i + 1], in1=a_bc[:], op0=ALU.mult, op1=ALU.mult),
                  reads=[xn_, "rstd1", "a_bc"], writes=["junk%d" % j])
            S.dve(lambda e, j=j: e.tensor_tensor(out=hbs[j][:], in0=junks[j][:], in1=sh_bc[:], op=ALU.add), reads=["junk%d" % j, "sh_bc"], writes=["hb%d" % j])
            pb = 2 + (i % 2)

            def f(e, i=i, j=j, pb=pb):
                ins = None
                for kc in range(8):
                    ins = e.transpose(out=PSB[pb][:, kc * 128:(kc + 1) * 128], in_=hbs[j][:, kc * 128:(kc + 1) * 128], identity=ident_b[:])
                return ins
            S.pe(f, reads=["hb%d" % j, "ident_b"], writes=["ps%d" % pb])
            S.act(lambda e, i=i, pb=pb: e.activation(out=hT[:, :, i * 128:(i + 1) * 128], in_=PSB[pb][:, 0:1024].rearrange("p (k t) -> p k t", k=8), func=AF.Copy),
                  reads=["ps%d" % pb], writes=["hTt%d" % i])
        A.release(m0)
        if os.environ.get('KBAR', '1') == '1':
            S.barrier()
        if debug:
            for kc in range(8):
                S.dma("sp", lambda e, kc=kc: e.dma_start(out=dbg_hT[kc], in_=hT[:, kc, :]), reads=["hTt%d" % i for i in range(NT)])
        if stage <= 1:
            zt = A.alloc("zt", [128, D], F32)
            S.dve(lambda e: e.memset(zt[:], 0.0), writes=["zt"])
            S.dma("sp", lambda e: e.dma_start(out=out[0:128, :], in_=zt[:]), reads=["zt"])
            S.emit(st)
            return nc, S

        m2 = A.mark()
        wA = A.alloc("wA", [128, 8, 2048], BF16)
        S.dma("pool", lambda e: e.dma_start(out=wA[:], in_=w_in[:, 0:2048].rearrange("(k p) c -> p k c", p=128)), writes=["wA"])
        lbt = A.alloc("lbt", [128, 2, 4], F32)
        lb = A.alloc("lb", [128, 4], F32)
        oml = A.alloc("oml", [128, 4], F32)
        for r_ in range(2):
            S.dma("sp", lambda e, r_=r_: e.dma_start(out=lbt[:, r_, :], in_=lb_logits[r_].rearrange("(h k) -> k h", k=128)), writes=["lbt"])
        S.dve(lambda e: e.tensor_tensor(out=lb[:], in0=lbt[:, 0, :], in1=lbt[:, 1, :], op=ALU.subtract), reads=["lbt"], writes=["lb"])
        S.act(lambda e: e.activation(out=lb[:], in_=lb[:], func=AF.Sigmoid), reads=["lb"], writes=["lb"])
        S.dve(lambda e: e.tensor_scalar(out=oml[:], in0=lb[:], scalar1=-1.0, scalar2=1.0, op0=ALU.mult, op1=ALU.add), reads=["lb"], writes=["oml"])
        gn_bc = A.alloc("gn_bc", [128, 512], F32)
        S.dma("sp", lambda e: e.dma_start(out=gn_bc[:], in_=hg_norm_g.partition_broadcast(128)), writes=["gn_bc"])
        rmask = A.alloc("rmask", [128, 8, 64], F32)
        S.dve(lambda e: e.memset(rmask[:], 1.0), writes=["rmask"])
        S.dve(lambda e: e.memset(rmask[:, :, 0:1], 0.0), reads=["rmask"], writes=["rmask"])
        hmask = A.alloc("hmask", [128, 128], F32)
        S.dve(lambda e: e.tensor_single_scalar(out=hmask[:], in_=io_f[:], scalar=0.0, op=ALU.is_ge), reads=["io_f"], writes=["hmask"])
        S.dve(lambda e: e.memset(hmask[0:64, 64:128], 0.0), reads=["hmask"], writes=["hmask"])
        Sst = A.alloc("Sst", [128, 4, 128], F32)
        S.dve(lambda e: e.memset(Sst[:], 0.0), writes=["Sst"])
        sbf = [A.alloc("sbf%d" % i, [128, 512], BF16) for i in range(2)]
        sig = A.alloc("sig", [128, 512], F32)
        ff = A.alloc("ff", [128, 512], F32)
        logf = A.alloc("logf", [128, 512], F32)
        bb = A.alloc("bb", [128, 8, 64], F32)
        eb = A.alloc("eb", [128, 512], F32)
        enb = A.alloc("enb", [128, 512], F32)
        kk = A.alloc("kk", [128, 512], F32)
        dd = A.alloc("dd", [128, 8, 64], F32)
        ed = A.alloc("ed", [128, 512], F32)
        dec = A.alloc("dec", [128, 8], F32)
        qe = A.alloc("qe", [128, 512], BF16)
        ke = A.alloc("ke", [128, 512], BF16)
        kendT = A.alloc("kendT", [128, 512], BF16)
        kend_tm = A.alloc("kend_tm", [128, 512], BF16)
        v_sb = A.alloc("v_sb", [128, 512], BF16)
        ATb = A.alloc("ATb", [128, 4, 128], BF16)
        sg = A.alloc("sg", [128, 512], F32)
        t1 = A.alloc("t1", [128, 4, 128], F32)
        t2 = A.alloc("t2", [128, 512], F32)
        ya = A.alloc("ya", [128, 512], BF16)
        yaT_sb = A.alloc("yaT_sb", [128, 4, 128], BF16)
        ssq = A.alloc("ssq", [128, 12], F32)
        junkh = A.alloc("junkh", [128, 128], F32)
        bbf = bb[:].rearrange("p a b -> p (a b)")
        ddf = dd[:].rearrange("p a b -> p (a b)")
        rmf = rmask[:].rearrange("p a b -> p (a b)")
        sig2 = [sig, A.alloc("sigB", [128, 512], F32)]
        v_sb2 = [v_sb, A.alloc("v_sbB", [128, 512], BF16)]
        sg2 = [sg, A.alloc("sgB", [128, 512], F32)]
        qsb2 = [A.alloc("qsbA", [128, 512], F32), A.alloc("qsbB", [128, 512], F32)]

        def hg_proj(i):
            tok = slice(i * 128, (i + 1) * 128)
            hr = [HTn(i)]
            def fproj(e):
                ins = None
                for h in range(4):
                    for kc in range(8):
                        ins = e.matmul(PS[0][:, h * 128:(h + 1) * 128], lhsT=wA[:, kc, h * 128:(h + 1) * 128], rhs=hT[:, kc, tok], start=(kc == 0), stop=(kc == 7))
                for h in range(4):
                    for kc in range(8):
                        ins = e.matmul(PS[1][:, h * 128:(h + 1) * 128], lhsT=wA[:, kc, 512 + h * 128:512 + (h + 1) * 128], rhs=hT[:, kc, tok], start=(kc == 0), stop=(kc == 7))
                for kc in range(8):
                    ins = e.matmul(PS[2][:, :], lhsT=hT[:, kc, tok], rhs=wA[:, kc, 1024:1536], start=(kc == 0), stop=(kc == 7))
                for kc in range(8):
                    ins = e.matmul(PS[3][:, :], lhsT=hT[:, kc, tok], rhs=wA[:, kc, 1536:2048], start=(kc == 0), stop=(kc == 7))
                return ins
            S.pe(fproj, reads=hr + ["wA"], writes=["ps0", "ps1", "ps2", "ps3"])
            S.act(lambda e: e.activation(out=sig2[i % 2][:], in_=PS[1][:, :], func=AF.Sigmoid), reads=["ps1"], writes=["sig%d" % (i % 2)])
            S.act(lambda e: e.activation(out=v_sb2[i % 2][:], in_=PS[2][:, :], func=AF.Copy), reads=["ps2"], writes=["v_sb%d" % (i % 2)])
            S.act(lambda e: e.activation(out=sg2[i % 2][:], in_=PS[3][:, :], func=AF.Sigmoid), reads=["ps3"], writes=["sg%d" % (i % 2)])
            S.dve(lambda e: e.tensor_tensor(out=sg2[i % 2][:], in0=PS[3][:, :], in1=sg2[i % 2][:], op=ALU.mult), reads=["ps3", "sg%d" % (i % 2)], writes=["sg%d" % (i % 2)])
            S.act(lambda e: e.activation(out=qsb2[i % 2][:], in_=PS[0][:, :], func=AF.Copy), reads=["ps0"], writes=["qsb%d" % (i % 2)])

        def hg_main(i):
            tok = slice(i * 128, (i + 1) * 128)
            def faff(e):
                ins = None
                for h in range(4):
                    ins = e.tensor_scalar(out=ff[:, h * 128:(h + 1) * 128], in0=sig2[i % 2][:, h * 128:(h + 1) * 128], scalar1=oml[:, h:h + 1], scalar2=lb[:, h:h + 1], op0=ALU.mult, op1=ALU.add)
                return ins
            S.dve(faff, reads=["sig%d" % (i % 2), "oml", "lb"], writes=["ff"])
            S.act(lambda e: e.activation(out=logf[:], in_=ff[:], func=AF.Ln), reads=["ff"], writes=["logf"])
            S.pool(lambda e: e.tensor_scalar(out=kk[:], in0=ff[:], scalar1=-1.0, scalar2=1.0, op0=ALU.mult, op1=ALU.add), reads=["ff"], writes=["kk"])
            S.dve(lambda e: e.tensor_tensor_scan(out=bbf, data0=rmf, data1=logf[:], initial=0.0, op0=ALU.mult, op1=ALU.add), reads=["rmask", "logf"], writes=["bb"])
            S.act(lambda e: e.activation(out=eb[:], in_=bbf, func=AF.Exp), reads=["bb"], writes=["eb"])
            S.act(lambda e: e.activation(out=enb[:], in_=bbf, func=AF.Exp, scale=-1.0), reads=["bb"], writes=["enb"])
            S.act(lambda e: e.activation(out=dec[:], in_=bb[:, :, 63], func=AF.Exp), reads=["bb"], writes=["dec"])
            S.dve(lambda e: e.tensor_tensor(out=dd[:], in0=bb[:, :, 63:64].to_broadcast([128, 8, 64]), in1=bb[:], op=ALU.subtract), reads=["bb"], writes=["dd"])
            S.act(lambda e: e.activation(out=ed[:], in_=ddf, func=AF.Exp), reads=["dd"], writes=["ed"])
            S.dve(lambda e: e.tensor_tensor(out=qe[:], in0=qsb2[i % 2][:], in1=eb[:], op=ALU.mult), reads=["qsb%d" % (i % 2), "eb"], writes=["qe"])
            S.pool(lambda e: e.tensor_tensor(out=ke[:], in0=kk[:], in1=enb[:], op=ALU.mult), reads=["kk", "enb"], writes=["ke"])
            S.pool(lambda e: e.tensor_tensor(out=kendT[:], in0=kk[:], in1=ed[:], op=ALU.mult), reads=["kk", "ed"], writes=["kendT"])
            if i + 1 < NT:
                hg_proj(i + 1)

            def ftr(e):
                ins = None
                for h in range(4):
                    ins = e.transpose(out=PSB[7][:, h * 128:(h + 1) * 128], in_=kendT[:, h * 128:(h + 1) * 128], identity=ident_b[:])
                return ins
            S.pe(ftr, reads=["kendT", "ident_b"], writes=["ps7"])
            S.act(lambda e: e.activation(out=kend_tm[:], in_=PSB[7][:, 0:512], func=AF.Copy), reads=["ps7"], writes=["kend_tm"])

            def fat(e):
                ins = None
                for h in range(4):
                    ins = e.matmul(PS[4][:, h * 128:(h + 1) * 128], lhsT=ke[:, h * 128:(h + 1) * 128], rhs=qe[:, h * 128:(h + 1) * 128], start=True, stop=True)
                return ins
            S.pe(fat, reads=["ke", "qe"], writes=["ps4"])
            S.dve(lambda e: e.tensor_tensor(out=ATb[:], in0=PS[4][:, :].rearrange("p (h t) -> p h t", h=4), in1=hmask[:].unsqueeze(1).to_broadcast([128, 4, 128]), op=ALU.mult),
                  reads=["ps4", "hmask"], writes=["ATb"])
            for ci in range(2):
                r0 = 64 * ci
                S.act(lambda e, ci=ci: e.activation(out=sbf[ci][:], in_=Sst[:].rearrange("p h v -> p (h v)"), func=AF.Copy), reads=["Sst"], writes=["sbf%d" % ci])

                def fu(e, r0=r0):
                    ins = None
                    for h in range(4):
                        ins = e.matmul(PS[5][:, h * 128:(h + 1) * 128], lhsT=kend_tm[r0:r0 + 64, h * 128:(h + 1) * 128], rhs=v_sb2[i % 2][r0:r0 + 64, h * 128:(h + 1) * 128], start=True, stop=True)
                    return ins
                S.pe(fu, reads=["kend_tm", "v_sb%d" % (i % 2)], writes=["ps5"])
                S.dve(lambda e, ci=ci: e.tensor_tensor(out=Sst[:], in0=Sst[:], in1=dec[:].rearrange("p (h c) -> p h c", c=2)[:, :, ci:ci + 1].to_broadcast([128, 4, 128]), op=ALU.mult),
                      reads=["Sst", "dec"], writes=["Sst"])
                S.dve(lambda e: e.tensor_tensor(out=Sst[:].rearrange("p h v -> p (h v)"), in0=Sst[:].rearrange("p h v -> p (h v)"), in1=PS[5][:, :], op=ALU.add),
                      reads=["Sst", "ps5"], writes=["Sst"])

            def fo(e):
                ins = None
                for h in range(4):
                    hc = slice(h * 128, (h + 1) * 128)
                    e.matmul(PS[6][:, hc], lhsT=ATb[:, h, :], rhs=v_sb2[i % 2][:, hc], start=True, stop=False)
                    e.matmul(PS[6][0:64, hc], lhsT=qe[:, h * 128:h * 128 + 64], rhs=sbf[0][:, hc], start=False, stop=False)
                    ins = e.matmul(PS[6][64:128, hc], lhsT=qe[:, h * 128 + 64:h * 128 + 128], rhs=sbf[1][:, hc], start=False, stop=True)
                return ins
            S.pe(fo, reads=["ATb", "v_sb%d" % (i % 2), "qe", "sbf0", "sbf1"], writes=["ps6"])

            def fsq(e):
                ins = None
                for h in range(4):
                    ins = e.activation(out=junkh[:], in_=PS[6][:, h * 128:(h + 1) * 128], func=AF.Square, accum_out=ssq[:, h:h + 1])
                return ins
            S.act(fsq, reads=["ps6"], writes=["ssq", "junkh"])
            S.dve(lambda e: e.tensor_scalar(out=ssq[:, 4:8], in0=ssq[:, 0:4], scalar1=1.0 / 128, scalar2=EPS, op0=ALU.mult, op1=ALU.add), reads=["ssq"], writes=["ssqb"])
            S.act(lambda e: e.activation(out=ssq[:, 8:12], in_=ssq[:, 4:8], func=AF.Ln), reads=["ssqb"], writes=["ssqc"])
            S.act(lambda e: e.activation(out=ssq[:, 4:8], in_=ssq[:, 8:12], func=AF.Exp, scale=-0.5), reads=["ssqc", "ssqb"], writes=["ssqb"])
            S.dve(lambda e: e.tensor_tensor(out=t1[:], in0=PS[6][:, :].rearrange("p (h v) -> p h v", h=4), in1=ssq[:, 4:8].unsqueeze(2).to_broadcast([128, 4, 128]), op=ALU.mult),
                  reads=["ps6", "ssqb"], writes=["t1"])
            S.pool(lambda e: e.tensor_tensor(out=t2[:], in0=t1[:].rearrange("p h v -> p (h v)"), in1=sg2[i % 2][:], op=ALU.mult), reads=["t1", "sg%d" % (i % 2)], writes=["t2"])
            S.pool(lambda e: e.tensor_tensor(out=ya[:], in0=t2[:], in1=gn_bc[:], op=ALU.mult), reads=["t2", "gn_bc"], writes=["ya"])

            def fyt(e):
                ins = None
                for h in range(4):
                    ins = e.transpose(out=PSB[7][:, 512 + h * 128:512 + (h + 1) * 128], in_=ya[:, h * 128:(h + 1) * 128], identity=ident_b[:])
                return ins
            S.pe(fyt, reads=["ya", "ident_b"], writes=["ps7b"])
            S.act(lambda e: e.activation(out=yaT_sb[:].rearrange("p f t -> p (f t)"), in_=PSB[7][:, 512:1024], func=AF.Copy), reads=["ps7b"], writes=["yaT_sb"])
            S.dma("sp", lambda e, tok=tok: e.dma_start(out=yaT_d[:, :, tok].rearrange("f p t -> p f t"), in_=yaT_sb[:]), reads=["yaT_sb"], writes=["yaT_d"])
            if i < NT - 2:
                convert_experts(3)
        hg_proj(0)
        for i in range(NT):
            hg_main(i)
        A.release(m2)
        S.barrier()
        if stage <= 2:
            zt = A.alloc("zt", [128, D], F32)
            S.dve(lambda e: e.memset(zt[:], 0.0), writes=["zt"])
            S.dma("sp", lambda e: e.dma_start(out=out[0:128, :], in_=zt[:]), reads=["zt"])
            S.emit(st)
            return nc, S

        m3 = A.mark()
        wB = A.alloc("wB", [128, 8, 2304], BF16)
        S.dma("pool", lambda e: e.dma_start(out=wB[:], in_=w_in[:, 2048:4352].rearrange("(k p) c -> p k c", p=128)), writes=["wB"])
        mk = A.alloc("mk", [128, 4, 128], BF16)
        mk0 = A.alloc("mk0", [128, 4, 128], BF16)
        S.dve(lambda e: e.tensor_single_scalar(out=mk[:, 0, :], in_=io_f[:], scalar=0.0, op=ALU.is_le), reads=["io_f"], writes=["mk"])
        S.dve(lambda e: e.tensor_single_scalar(out=mk[:, 1, :], in_=io_f[:], scalar=0.0, op=ALU.is_ge), reads=["io_f", "mk"], writes=["mk"])
        S.dve(lambda e: e.tensor_copy(out=mk[:, 2:4, :], in_=mk[:, 0:2, :]), reads=["mk"], writes=["mk"])
        S.dve(lambda e: e.tensor_copy(out=mk0[:], in_=mk[:]), reads=["mk"], writes=["mk0"])
        S.dve(lambda e: e.memset(mk0[:, 0, :], 0.0), reads=["mk0"], writes=["mk0"])
        S.dve(lambda e: e.memset(mk0[:, 2, :], 0.0), reads=["mk0"], writes=["mk0"])
        QT = A.alloc("QT", [128, 2, SEQ], BF16)
        S.dve(lambda e: e.memset(QT[64:128, 0, :], 0.0), writes=["QTz0"])
        S.dve(lambda e: e.memset(QT[0:64, 1, :], 0.0), writes=["QTz1"])
        KT = A.alloc("KT", [128, SEQ], BF16)
        Vb = A.alloc("Vb", [128, 32, 128], BF16)
        numT = A.alloc("numT", [128, SEQ], F32)
        denT = A.alloc("denT", [128, SEQ], F32)
        PT2 = [A.alloc("PT%d" % i, [128, 512], BF16) for i in range(2)]
        PTm2 = [A.alloc("PTm%d" % i, [128, 512], BF16) for i in range(2)]
        ybo = A.alloc("ybo", [128, SEQ], BF16)
        allh = [HTn(i) for i in range(NT)]
        for hp in range(2):
            for g, dil in enumerate((1, 4, 16)[:int(os.environ.get('KG', '3'))]):
                qc = 256 * g + 128 * hp
                kc0 = 768 + qc
                vc0 = 1536 + qc
                for tb in range(8):
                    tsl = slice(tb * 512, (tb + 1) * 512)

                    def fq(e, tsl=tsl, qc=qc, kc0=kc0):
                        ins = None
                        for kc in range(8):
                            ins = e.matmul(PS[0][:, :], lhsT=wB[:, kc, qc:qc + 128], rhs=hT[:, kc, tsl], start=(kc == 0), stop=(kc == 7))
                        for kc in range(8):
                            ins = e.matmul(PS[1][:, :], lhsT=wB[:, kc, kc0:kc0 + 128], rhs=hT[:, kc, tsl], start=(kc == 0), stop=(kc == 7))
                        return ins
                    S.pe(fq, reads=allh + ["wB"], writes=["ps0", "ps1"])
                    S.act(lambda e, tsl=tsl: e.activation(out=QT[0:64, 0, tsl], in_=PS[0][0:64, :], func=AF.Copy, scale=0.125), reads=["ps0", "QTz0"], writes=["QTa"])
                    S.act(lambda e, tsl=tsl: e.activation(out=QT[64:128, 1, tsl], in_=PS[0][64:128, :], func=AF.Copy, scale=0.125), reads=["ps0", "QTz1"], writes=["QTb"])
                    S.dve(lambda e, tsl=tsl: e.tensor_copy(out=KT[:, tsl], in_=PS[1][:, :]), reads=["ps1"], writes=["KT"])
                L = SEQ // dil
                nb = L // 128
                blocks = [(r, n_) for r in range(dil) for n_ in range(nb)]

                def tokslice(r, n_, dil=dil):
                    st_ = 128 * n_ * dil + r
                    return slice(st_, st_ + 127 * dil + 1, dil) if dil > 1 else slice(st_, st_ + 128)
                for b4 in range(8):
                    def fv(e, b4=b4, vc0=vc0, blocks=blocks, tokslice=tokslice):
                        ins = None
                        for q_ in range(4):
                            r, n_ = blocks[b4 * 4 + q_]
                            for kc in range(8):
                                ins = e.matmul(PS[2][:, q_ * 128:(q_ + 1) * 128], lhsT=hT[:, kc, tokslice(r, n_)], rhs=wB[:, kc, vc0:vc0 + 128], start=(kc == 0), stop=(kc == 7))
                        return ins
                    S.pe(fv, reads=allh + ["wB"], writes=["ps2"])
                    S.act(lambda e, b4=b4: e.activation(out=Vb[:, b4 * 4:(b4 + 1) * 4, :].rearrange("p a b -> p (a b)"), in_=PS[2][:, :], func=AF.Copy), reads=["ps2"], writes=["Vb"])
                def blk_params(bi):
                    r, n_ = blocks[bi]
                    qs = tokslice(r, n_)
                    ks = [tokslice(r, n_ - 1) if n_ > 0 else qs, qs]
                    vbi = [bi - 1 if n_ > 0 else bi, bi]
                    return n_, qs, ks, vbi

                def att_A(bi):
                    n_, qs, ks, vbi = blk_params(bi)
                    pb = 3 + (bi % 2)
                    p2 = bi % 2

                    def fs(e):
                        ins = None
                        for h in range(2):
                            for kb in range(2):
                                ins = e.matmul(PS[pb][:, (h * 2 + kb) * 128:(h * 2 + kb + 1) * 128], lhsT=KT[:, ks[kb]], rhs=QT[:, h, qs], start=True, stop=True)
                        return ins
                    S.pe(fs, reads=["QTa", "QTb", "KT"], writes=["ps%d" % pb])
                    S.act(lambda e: e.activation(out=PT2[p2][:], in_=PS[pb][:, :], func=AF.Exp), reads=["ps%d" % pb], writes=["PT%d" % p2])
                    mm_ = mk if n_ > 0 else mk0
                    S.dve(lambda e: e.tensor_tensor(out=PTm2[p2][:], in0=PT2[p2][:], in1=mm_[:].rearrange("p a b -> p (a b)"), op=ALU.mult), reads=["PT%d" % p2, "mk", "mk0"], writes=["PTm%d" % p2])

                def att_B(bi):
                    n_, qs, ks, vbi = blk_params(bi)
                    ob = 5 + (bi % 2)
                    p2 = bi % 2

                    def fpv(e):
                        ins = None
                        for h in range(2):
                            for kb in range(2):
                                ins = e.matmul(PS[ob][h * 64:(h + 1) * 64, 0:128], lhsT=Vb[:, vbi[kb], h * 64:(h + 1) * 64], rhs=PTm2[p2][:, (h * 2 + kb) * 128:(h * 2 + kb + 1) * 128], start=(kb == 0), stop=(kb == 1))
                        for h in range(2):
                            for kb in range(2):
                                ins = e.matmul(PS[ob][h * 64:(h + 1) * 64, 128:256], lhsT=ones_b[:, 0:64], rhs=PTm2[p2][:, (h * 2 + kb) * 128:(h * 2 + kb + 1) * 128], start=(kb == 0), stop=(kb == 1))
                        return ins
                    S.pe(fpv, reads=["Vb", "PTm%d" % p2, "ones_b"], writes=["ps%d" % ob])
                    if bi % 2 == 0 and not (hp == 1 and g == 2):
                        convert_experts(1)
                    if g == 0:
                        S.act(lambda e: e.activation(out=numT[:, qs], in_=PS[ob][:, 0:128], func=AF.Copy), reads=["ps%d" % ob], writes=["numT"])
                        S.act(lambda e: e.activation(out=denT[:, qs], in_=PS[ob][:, 128:256], func=AF.Copy), reads=["ps%d" % ob], writes=["denT"])
                    else:
                        S.dve(lambda e: e.tensor_tensor(out=numT[:, qs], in0=PS[ob][:, 0:128], in1=numT[:, qs], op=ALU.add), reads=["ps%d" % ob, "numT"], writes=["numT"])
                        S.dve(lambda e: e.tensor_tensor(out=denT[:, qs], in0=PS[ob][:, 128:256], in1=denT[:, qs], op=ALU.add), reads=["ps%d" % ob, "denT"], writes=["denT"])

                nbk = min(len(blocks), int(os.environ.get('KNB', '9999')))
                for bi in range(nbk + 1):
                    if bi < nbk:
                        att_A(bi)
                    if bi >= 1:
                        att_B(bi - 1)
            S.dve(lambda e: e.reciprocal(out=denT[:], in_=denT[:]), reads=["denT"], writes=["denT"])
            S.dve(lambda e: e.tensor_tensor(out=ybo[:], in0=numT[:], in1=denT[:], op=ALU.mult), reads=["numT", "denT"], writes=["ybo"])
            S.dma("sp", lambda e, hp=hp: e.dma_start(out=ybT_d[hp], in_=ybo[:]), reads=["ybo"], writes=["ybT_d"])
        A.release(m3)
        S.barrier()
        if stage <= 3:
            zt = A.alloc("zt", [128, D], F32)
            S.dve(lambda e: e.memset(zt[:], 0.0), writes=["zt"])
            S.dma("sp", lambda e: e.dma_start(out=out[0:128, :], in_=zt[:]), reads=["zt"])
            S.emit(st)
            return nc, S

        m4 = A.mark()
        wC = A.alloc("wC", [128, 8, 2048], BF16)
        wa = A.alloc("wa", [128, 4, 1024], BF16)
        wb_ = A.alloc("wb_", [128, 2, 1024], BF16)
        wo = A.alloc("wo", [128, 8, 1024], BF16)
        S.dma("pool", lambda e: e.dma_start(out=wC[:], in_=w_in[:, 4352:6400].rearrange("(k p) c -> p k c", p=128)), writes=["wC"])
        S.dma("pool", lambda e: e.dma_start(out=wa[:], in_=w_branch_a.rearrange("(k p) c -> p k c", p=128)), writes=["wa"])
        S.dma("pool", lambda e: e.dma_start(out=wb_[:], in_=w_branch_b.rearrange("(k p) c -> p k c", p=128)), writes=["wb_"])
        S.dma("pool", lambda e: e.dma_start(out=wo[:], in_=w_out.rearrange("(k p) c -> p k c", p=128)), writes=["wo"])
        yaTb = [A.alloc("yaTb%d" % i, [128, 4, 512], BF16) for i in range(2)]
        ybTb = [A.alloc("ybTb%d" % i, [128, 2, 512], BF16) for i in range(2)]
        mergedT = A.alloc("mergedT", [128, 8, 512], BF16)
        sga = [A.alloc("sga%d" % i, [128, 512], F32) for i in range(2)]
        sgb = [A.alloc("sgb%d" % i, [128, 512], F32) for i in range(2)]
        m1 = [A.alloc("m1_%d" % i, [128, 512], F32) for i in range(2)]
        m2_ = [A.alloc("m2_%d" % i, [128, 512], F32) for i in range(2)]
        xt2 = [A.alloc("xt2_%d" % i, [128, D], F32) for i in range(2)]
        zt2 = [A.alloc("zt2_%d" % i, [128, D], F32) for i in range(2)]
        for tb in range(8):
            tsl = slice(tb * 512, (tb + 1) * 512)
            j = tb % 2
            S.dma("sp", lambda e, j=j, tsl=tsl: e.dma_start(out=yaTb[j][:], in_=yaT_d[:, :, tsl].rearrange("f p t -> p f t")), reads=["yaT_d"], writes=["yaTb%d" % j])
            S.dma("sp", lambda e, j=j, tsl=tsl: e.dma_start(out=ybTb[j][:], in_=ybT_d[:, :, tsl].rearrange("f p t -> p f t")), reads=["ybT_d"], writes=["ybTb%d" % j])
            hrd = [HTn(tb * 4 + q_) for q_ in range(4)]
            for cc in range(8):
                csl = slice(cc * 128, (cc + 1) * 128)
                pp = cc % 2
                b0 = 4 * pp

                def fm(e, j=j, tsl=tsl, csl=csl, cc=cc, b0=b0):
                    ins = None
                    for fc in range(4):
                        ins = e.matmul(PS[b0][:, :], lhsT=wa[:, fc, csl], rhs=yaTb[j][:, fc, :], start=(fc == 0), stop=(fc == 3))
                    for fc in range(2):
                        ins = e.matmul(PS[b0 + 1][:, :], lhsT=wb_[:, fc, csl], rhs=ybTb[j][:, fc, :], start=(fc == 0), stop=(fc == 1))
                    for kc in range(8):
                        ins = e.matmul(PS[b0 + 2][:, :], lhsT=wC[:, kc, csl], rhs=hT[:, kc, tsl], start=(kc == 0), stop=(kc == 7))
                    for kc in range(8):
                        ins = e.matmul(PS[b0 + 3][:, :], lhsT=wC[:, kc, 1024 + cc * 128:1024 + (cc + 1) * 128], rhs=hT[:, kc, tsl], start=(kc == 0), stop=(kc == 7))
                    return ins
                S.pe(fm, reads=hrd + ["wa", "wb_", "wC", "yaTb%d" % j, "ybTb%d" % j], writes=["ps%d" % (b0 + k_) for k_ in range(4)])
                S.act(lambda e, pp=pp, b0=b0: e.activation(out=sga[pp][:], in_=PS[b0 + 2][:, :], func=AF.Sigmoid), reads=["ps%d" % (b0 + 2)], writes=["sga%d" % pp])
                S.act(lambda e, pp=pp, b0=b0: e.activation(out=sgb[pp][:], in_=PS[b0 + 3][:, :], func=AF.Sigmoid), reads=["ps%d" % (b0 + 3)], writes=["sgb%d" % pp])
                S.dve(lambda e, pp=pp, b0=b0: e.tensor_tensor(out=m1[pp][:], in0=PS[b0][:, :], in1=sga[pp][:], op=ALU.mult), reads=["ps%d" % b0, "sga%d" % pp], writes=["m1_%d" % pp])
                S.dve(lambda e, pp=pp, b0=b0: e.tensor_tensor(out=m2_[pp][:], in0=PS[b0 + 1][:, :], in1=sgb[pp][:], op=ALU.mult), reads=["ps%d" % (b0 + 1), "sgb%d" % pp], writes=["m2_%d" % pp])
                S.pool(lambda e, cc=cc, pp=pp: e.tensor_tensor(out=mergedT[:, cc, :], in0=m1[pp][:], in1=m2_[pp][:], op=ALU.add), reads=["m1_%d" % pp, "m2_%d" % pp], writes=["mergedT%d" % cc])
                if tb < 7:
                    convert_experts(1)
            for q_ in range(4):
                i = tb * 4 + q_
                jj = i % 2
                S.dma("sp", lambda e, i=i, jj=jj: e.dma_start(out=xt2[jj][:], in_=x[i * 128:(i + 1) * 128, :]), writes=["xt2_%d" % jj])

                zb = 2 * (q_ % 2)

                def fz(e, q_=q_, zb=zb):
                    ins = None
                    for half in range(2):
                        for cc in range(8):
                            ins = e.matmul(PS[zb + half][:, :], lhsT=mergedT[:, cc, q_ * 128:(q_ + 1) * 128], rhs=wo[:, cc, half * 512:(half + 1) * 512], start=(cc == 0), stop=(cc == 7))
                    return ins
                S.pe(fz, reads=["mergedT%d" % cc for cc in range(8)] + ["wo"], writes=["ps%d" % zb, "ps%d" % (zb + 1)])
                for half in range(2):
                    hs = slice(half * 512, (half + 1) * 512)
                    S.dve(lambda e, jj=jj, half=half, hs=hs, zb=zb: e.tensor_tensor(out=zt2[jj][:, hs], in0=PS[zb + half][:, :], in1=gate1_bc[:, hs], op=ALU.mult),
                          reads=["ps%d" % (zb + half), "gate1_bc"], writes=["zt2_%d_%d" % (jj, half)])
                S.pool(lambda e, jj=jj: e.tensor_tensor(out=zt2[jj][:], in0=zt2[jj][:], in1=xt2[jj][:], op=ALU.add),
                       reads=["zt2_%d_0" % jj, "zt2_%d_1" % jj, "xt2_%d" % jj], writes=["zt2_%d_0" % jj, "zt2_%d_1" % jj])
                S.dma("sp", lambda e, i=i, jj=jj: e.dma_start(out=x2p_d[i * 128:(i + 1) * 128, :], in_=zt2[jj][:]), reads=["zt2_%d_0" % jj, "zt2_%d_1" % jj], writes=["x2p_d%d" % i])
        A.release(m4)
        S.barrier()
        A.release(base_mark)
        A.off -= 8 * SEQ * 2
        off_sb = (A.off + 31) // 32 * 32
        sb_all = A.alloc("sb_all", [128, NT, 256], F32)
        pos_all = A.alloc("pos_all", [128, NT, 256], F32)
        m8_all = A.alloc("m8_all", [128, NT, 8], F32)
        w8_all = A.alloc("w8_all", [128, NT, 8], F32)
        carry = A.alloc("carry", [128, 256], F32)
        route_mark = A.mark()
        m5 = A.mark()
        load_norm_consts(norm2_g, 24, 32)
        wr = A.alloc("wr", [128, 8, 256], BF16)
        wsg = A.alloc("wsg", [128, 8, 512], BF16)
        wsd = A.alloc("wsd", [128, 2, 1024], BF16)
        S.dma("pool", lambda e: e.dma_start(out=wr[:], in_=w_router.rearrange("(k p) c -> p k c", p=128)), writes=["wr"])
        S.dma("pool", lambda e: e.dma_start(out=wsg[:, :, 0:256], in_=w_sh_gate.rearrange("(k p) c -> p k c", p=128)), writes=["wsg_a"])
        S.dma("pool", lambda e: e.dma_start(out=wsg[:, :, 256:512], in_=w_sh_up.rearrange("(k p) c -> p k c", p=128)), writes=["wsg_b"])
        S.dma("pool", lambda e: e.dma_start(out=wsd[:], in_=w_sh_down.rearrange("(k p) c -> p k c", p=128)), writes=["wsd"])
        rb_bc = A.alloc("rb_bc", [128, 256], F32)
        S.dma("sp", lambda e: e.dma_start(out=rb_bc[:], in_=router_bias.partition_broadcast(128)), writes=["rb_bc"])
        tri_b = A.alloc("tri_b", [128, 128], BF16)
        S.dve(lambda e: e.tensor_single_scalar(out=tri_b[:], in_=io_f[:], scalar=0.0, op=ALU.is_gt), reads=["io_f"], writes=["tri_b"])
        S.dve(lambda e: e.memset(carry[:], 0.0), writes=["carry"])
        junk2 = [A.alloc("junk2_%d" % i, [128, D], F32) for i in range(2)]
        h2b = [A.alloc("h2b%d" % i, [128, D], BF16) for i in range(2)]
        ss2 = [A.alloc("ss2_%d" % i, [128, 4], F32) for i in range(2)]
        h2T = [A.alloc("h2T%d" % i, [128, 8, 128], BF16) for i in range(2)]
        scr = A.alloc("scr", [128, 256], F32)
        maskb = A.alloc("maskb", [128, 256], BF16)
        jk = A.alloc("jk", [128, 256], F32)
        s8 = A.alloc("s8", [128, 12], F32)
        sgl = A.alloc("sgl", [128, 256], F32)
        actT = A.alloc("actT", [128, 2, 128], BF16)
        yt = [A.alloc("yt%d" % i, [128, D], F32) for i in range(2)]
        NX1 = 3
        x1t = [A.alloc("x1u%d" % i, [128, D], F32) for i in range(NX1)]
        ssq_all = A.alloc("ssq_all", [128, NT], F32)
        rstd_all = A.alloc("rstd_all", [128, NT], F32)
        for i in range(NT):
            jx = i % NX1
            S.dma("sp", lambda e, i=i, jx=jx: e.dma_start(out=x1t[jx][:], in_=x2p_d[i * 128:(i + 1) * 128, :]), reads=["x2p_d%d" % i], writes=["x1u%d" % jx])
            S.act(lambda e, i=i, jx=jx: e.activation(out=junk2[i % 2][:], in_=x1t[jx][:], func=AF.Square, accum_out=ssq_all[:, i:i + 1]),
                  reads=["x1u%d" % jx], writes=["junk2_%d" % (i % 2), "ssq%d" % i])
        S.dve(lambda e: e.tensor_scalar(out=rstd_all[:], in0=ssq_all[:], scalar1=1.0 / D, scalar2=EPS, op0=ALU.mult, op1=ALU.add), reads=["ssq%d" % i for i in range(NT)], writes=["rstd_a"])
        S.act(lambda e: e.activation(out=ssq_all[:], in_=rstd_all[:], func=AF.Sqrt), reads=["rstd_a"], writes=["ssq_b"])
        S.dve(lambda e: e.reciprocal(out=rstd_all[:], in_=ssq_all[:]), reads=["ssq_b", "rstd_a"], writes=["rstd_all"])

        def stP(i):
            j = i % 2
            jx = i % NX1
            S.dma("sp", lambda e: e.dma_start(out=x1t[jx][:], in_=x2p_d[i * 128:(i + 1) * 128, :]), reads=["x2p_d%d" % i], writes=["x1u%d" % jx])
            S.dve(lambda e: e.scalar_tensor_tensor(out=junk2[j][:], in0=x1t[jx][:], scalar=rstd_all[:, i:i + 1], in1=a_bc[:], op0=ALU.mult, op1=ALU.mult),
                  reads=["x1u%d" % jx, "rstd_all", "a_bc"], writes=["junk2_%d" % j])
            S.dve(lambda e: e.tensor_tensor(out=h2b[j][:], in0=junk2[j][:], in1=sh_bc[:], op=ALU.add), reads=["junk2_%d" % j, "sh_bc"], writes=["h2b%d" % j])
            S.dma("sp", lambda e: e.dma_start(out=h2_d[i * 128:(i + 1) * 128, :], in_=h2b[j][:]), reads=["h2b%d" % j], writes=["h2_d%d" % i])

            def ft(e, j=j):
                ins = None
                for kc in range(8):
                    ins = e.transpose(out=PSB[0][:, kc * 128:(kc + 1) * 128], in_=h2b[j][:, kc * 128:(kc + 1) * 128], identity=ident_b[:])
                return ins
            S.pe(ft, reads=["h2b%d" % j, "ident_b"], writes=["ps0"])
            S.act(lambda e, j=j: e.activation(out=h2T[j][:].rearrange("p k t -> p (k t)"), in_=PSB[0][:, 0:1024], func=AF.Copy), reads=["ps0"], writes=["h2T%d" % j])

        def stQ(i):
            j = i % 2

            def frt(e, j=j):
                ins = None
                for kc in range(8):
                    ins = e.matmul(PS[1][:, 0:256], lhsT=h2T[j][:, kc, :], rhs=wr[:, kc, :], start=(kc == 0), stop=(kc == 7))
                return ins
            S.pe(frt, reads=["h2T%d" % j, "wr"], writes=["ps1"])
            S.act(lambda e: e.activation(out=scr[:], in_=PS[1][:, 0:256], func=AF.Sigmoid), reads=["ps1"], writes=["scr"])
            S.dve(lambda e, i=i: e.tensor_tensor(out=sb_all[:, i, :], in0=scr[:], in1=rb_bc[:], op=ALU.add), reads=["scr", "rb_bc"], writes=["sb_all%d" % i])
            S.dve(lambda e, i=i: e.max(out=m8_all[:, i, :], in_=sb_all[:, i, :]), reads=["sb_all%d" % i], writes=["m8_all%d" % i])
            S.dve(lambda e, i=i: e.tensor_scalar(out=maskb[:], in0=sb_all[:, i, :], scalar1=m8_all[:, i, 7:8], scalar2=None, op0=ALU.is_ge),
                  reads=["sb_all%d" % i, "m8_all%d" % i], writes=["maskb"])
            for k in range(8):
                S.dve(lambda e, i=i, k=k: e.scalar_tensor_tensor(out=jk[:], in0=sb_all[:, i, :], scalar=m8_all[:, i, k:k + 1], in1=scr[:], op0=ALU.is_equal, op1=ALU.mult, accum_out=s8[:, k:k + 1]),
                      reads=["sb_all%d" % i, "m8_all%d" % i, "scr"], writes=["s8_%d" % k])
            S.dve(lambda e: e.tensor_reduce(out=s8[:, 8:9], in_=s8[:, 0:8], axis=AX.X, op=ALU.add), reads=["s8_%d" % k for k in range(8)], writes=["s8s"])
            S.dve(lambda e: e.reciprocal(out=s8[:, 9:10], in_=s8[:, 8:9]), reads=["s8s"], writes=["s8r"])
            S.dve(lambda e, i=i: e.tensor_scalar(out=w8_all[:, i, :], in0=s8[:, 0:8], scalar1=s8[:, 9:10], scalar2=2.5, op0=ALU.mult, op1=ALU.mult),
                  reads=["s8r"] + ["s8_%d" % k for k in range(8)], writes=["w8_all%d" % i])

            def fsh(e, j=j):
                ins = None
                for m in range(4):
                    for kc in range(8):
                        ins = e.matmul(PS[3][:, m * 128:(m + 1) * 128], lhsT=wsg[:, kc, m * 128:(m + 1) * 128], rhs=h2T[j][:, kc, :], start=(kc == 0), stop=(kc == 7))
                return ins
            S.pe(fsh, reads=["h2T%d" % j, "wsg_a", "wsg_b"], writes=["ps3"])
            S.act(lambda e: e.activation(out=sgl[:], in_=PS[3][:, 0:256], func=AF.Sigmoid), reads=["ps3"], writes=["sgl"])
            S.dve(lambda e: e.tensor_tensor(out=sgl[:], in0=PS[3][:, 0:256], in1=sgl[:], op=ALU.mult), reads=["ps3", "sgl"], writes=["sgl"])
            S.dve(lambda e: e.tensor_tensor(out=actT[:].rearrange("p m t -> p (m t)"), in0=PS[3][:, 256:512], in1=sgl[:], op=ALU.mult), reads=["ps3", "sgl"], writes=["actT"])

            def frk(e):
                e.matmul(PS[2][:, 0:256], lhsT=tri_b[:], rhs=maskb[:], start=True, stop=True)
                return e.matmul(PS[2][:, 256:512], lhsT=ones_b[:], rhs=maskb[:], start=True, stop=True)
            S.pe(frk, reads=["tri_b", "ones_b", "maskb"], writes=["ps2"])
            S.dve(lambda e, i=i: e.tensor_tensor(out=pos_all[:, i, :], in0=PS[2][:, 0:256], in1=carry[:], op=ALU.add), reads=["ps2", "carry"], writes=["pos_all%d" % i])
            S.dve(lambda e: e.tensor_tensor(out=carry[:], in0=PS[2][:, 256:512], in1=carry[:], op=ALU.add), reads=["ps2", "carry"], writes=["carry"])

            def fsd(e):
                ins = None
                for half in range(2):
                    for m in range(2):
                        ins = e.matmul(PS[4 + half][:, :], lhsT=actT[:, m, :], rhs=wsd[:, m, half * 512:(half + 1) * 512], start=(m == 0), stop=(m == 1))
                return ins
            S.pe(fsd, reads=["actT", "wsd"], writes=["ps4", "ps5"])
            for half in range(2):
                hs = slice(half * 512, (half + 1) * 512)
                S.dve(lambda e, j=j, half=half, hs=hs: e.tensor_tensor(out=yt[j][:, hs], in0=PS[4 + half][:, :], in1=gate2_bc[:, hs], op=ALU.mult),
                      reads=["ps%d" % (4 + half), "gate2_bc"], writes=["yt%d_%d" % (j, half)])
            S.dve(lambda e, j=j, i=i: e.tensor_tensor(out=yt[j][:], in0=yt[j][:], in1=x1t[i % NX1][:], op=ALU.add),
                  reads=["yt%d_0" % j, "yt%d_1" % j, "x1u%d" % (i % NX1)], writes=["yt%d_0" % j, "yt%d_1" % j])
            S.dma("sp", lambda e, i=i, j=j: e.dma_start(out=x2p_d[i * 128:(i + 1) * 128, :], in_=yt[j][:]), reads=["yt%d_0" % j, "yt%d_1" % j], writes=["x2p_d%d" % i])

        stP(0)
        for i in range(NT):
            if i + 1 < NT:
                stP(i + 1)
            stQ(i)
        A.release(m5)
        S.barrier()
        if stage <= 4:
            zt = A.alloc("zt", [128, D], F32)
            S.dve(lambda e: e.memset(zt[:], 0.0), writes=["zt"])
            S.dma("sp", lambda e: e.dma_start(out=out[0:128, :], in_=zt[:]), reads=["zt"])
            S.emit(st)
            return nc, S

        PST = {}

        def bcreg(e):
            if "bc" not in PST:
                PST["bc"] = e.alloc_register("bc")
                e.reg_mov(PST["bc"], NBLK * 128 - 1)
            return PST["bc"]

        A.release(route_mark)
        cntp = A.alloc("cntp", [128, 256], F32)
        nbf = A.alloc("nbf", [128, 256], F32)
        nbi = A.alloc("nbi", [128, 256], I32)
        tmpa = A.alloc("tmpa", [128, 256], F32)
        tmpb = A.alloc("tmpb", [128, 256], F32)
        padded = A.alloc("padded", [128, 256], F32)
        pend = A.alloc("pend", [128, 256], F32)
        pstart = A.alloc("pstart", [128, 256], F32)
        ones256 = A.alloc("ones256", [128, 256], F32)
        S.dve(lambda e: e.memset(ones256[:], 1.0), writes=["ones256"])
        S.dve(lambda e: e.tensor_scalar(out=cntp[:], in0=carry[:], scalar1=63.5, scalar2=1.0 / 128, op0=ALU.add, op1=ALU.mult), reads=["carry"], writes=["cntp"])
        S.dve(lambda e: e.tensor_copy(out=nbi[:], in_=cntp[:]), reads=["cntp"], writes=["nbi"])
        S.dve(lambda e: e.tensor_copy(out=nbf[:], in_=nbi[:]), reads=["nbi"], writes=["nbf"])
        S.dve(lambda e: e.scalar_tensor_tensor(out=tmpa[:], in0=nbf[:], scalar=128.0, in1=carry[:], op0=ALU.mult, op1=ALU.is_lt), reads=["nbf", "carry"], writes=["tmpa"])
        S.dve(lambda e: e.tensor_scalar(out=tmpb[:], in0=nbf[:], scalar1=128.0, scalar2=-128.0, op0=ALU.mult, op1=ALU.add), reads=["nbf"], writes=["tmpb"])
        S.dve(lambda e: e.tensor_tensor(out=tmpb[:], in0=tmpb[:], in1=carry[:], op=ALU.is_ge), reads=["tmpb", "carry"], writes=["tmpb"])
        S.dve(lambda e: e.tensor_tensor(out=nbf[:], in0=nbf[:], in1=tmpa[:], op=ALU.add), reads=["nbf", "tmpa"], writes=["nbf"])
        S.dve(lambda e: e.tensor_tensor(out=nbf[:], in0=nbf[:], in1=tmpb[:], op=ALU.subtract), reads=["nbf", "tmpb"], writes=["nbf"])
        S.dve(lambda e: e.tensor_scalar(out=padded[:], in0=nbf[:], scalar1=128.0, scalar2=None, op0=ALU.mult), reads=["nbf"], writes=["padded"])
        S.dve(lambda e: e.tensor_tensor_scan(out=pend[:], data0=ones256[:], data1=padded[:], initial=0.0, op0=ALU.mult, op1=ALU.add), reads=["ones256", "padded"], writes=["pend"])
        S.dve(lambda e: e.tensor_tensor(out=pstart[:], in0=pend[:], in1=padded[:], op=ALU.subtract), reads=["pend", "padded"], writes=["pstart"])
        bvi = A.alloc("bvi", [128, 4], I32)
        bvf = A.alloc("bvf", [128, 4], F32)
        blkf = A.alloc("blkf", [128, 8], F32)
        blki = A.alloc("blki", [128, 8], I32)
        S.pool(lambda e: e.iota(bvi[:], pattern=[[128 * 128, 4]], base=0, channel_multiplier=128), writes=["bvi"])
        S.dve(lambda e: e.tensor_copy(out=bvf[:], in_=bvi[:]), reads=["bvi"], writes=["bvf"])
        for jb in range(4):
            S.dve(lambda e, jb=jb: e.tensor_scalar(out=tmpa[:], in0=pend[:], scalar1=bvf[:, jb:jb + 1], scalar2=0.0, op0=ALU.is_le, op1=ALU.add, accum_out=blkf[:, jb:jb + 1]),
                  reads=["pend", "bvf", "tmpa"], writes=["tmpa", "blkf%d" % jb])
        S.dve(lambda e: e.tensor_scalar(out=blkf[:, 0:4], in0=blkf[:, 0:4], scalar1=255.0, scalar2=None, op0=ALU.min), reads=["blkf%d" % jb for jb in range(4)], writes=["blkfa"])
        S.dve(lambda e: e.tensor_tensor(out=blkf[:, 4:8], in0=bvf[:], in1=pend[:, 255:256].to_broadcast([128, 4]), op=ALU.is_lt), reads=["bvf", "pend"], writes=["blkfb"])
        S.dve(lambda e: e.tensor_copy(out=blki[:], in_=blkf[:]), reads=["blkfa", "blkfb"], writes=["blki"])
        for r_ in range(2):
            S.dma("sp", lambda e, r_=r_: e.dma_start(out=blk_d[r_].rearrange("(j p) -> p j", p=128), in_=blki[:, r_ * 4:(r_ + 1) * 4]), reads=["blki"], writes=["blk_d%d" % r_])
        blkrow = A.alloc("blkrow", [1, 2 * NBLK], I32)
        S.dma("sp", lambda e: e.dma_start(out=blkrow[:], in_=blk_d.rearrange("r b -> (r b)").unsqueeze(0)), reads=["blk_d0", "blk_d1"], writes=["blkrow"])
        d8f = A.alloc("d8f", [128, 8], F32)
        d8i_all = A.alloc("d8i_all", [128, NT, 8], I32)
        keyt = A.alloc("keyt", [128, 256], F32)
        tmpk = [A.alloc("tmpk%d" % i, [128, 256], F32) for i in range(2)]
        h2g = [A.alloc("h2g%d" % i, [128, D], BF16) for i in range(3)]
        plan_mark = A.mark()
        for i in range(NT):
            j = i % 3
            S.dma("sp", lambda e, i=i, j=j: e.dma_start(out=h2g[j][:], in_=h2_d[i * 128:(i + 1) * 128, :]), reads=["h2_d%d" % i], writes=["h2g%d" % j])
            S.dve(lambda e, i=i: e.tensor_tensor(out=keyt[:], in0=pos_all[:, i, :], in1=pstart[:], op=ALU.add), reads=["pos_all%d" % i, "pstart"], writes=["keyt"])
            for k in range(8):
                tk = tmpk[k % 2]
                S.dve(lambda e, i=i, k=k, tk=tk: e.scalar_tensor_tensor(out=tk[:], in0=sb_all[:, i, :], scalar=m8_all[:, i, k:k + 1], in1=keyt[:], op0=ALU.is_equal, op1=ALU.mult),
                      reads=["sb_all%d" % i, "m8_all%d" % i, "keyt"], writes=["tmpk%d" % (k % 2)])
                S.dve(lambda e, k=k, tk=tk: e.tensor_reduce(out=d8f[:, k:k + 1], in_=tk[:], axis=AX.X, op=ALU.max), reads=["tmpk%d" % (k % 2)], writes=["d8f%d" % k])
            S.dve(lambda e, i=i: e.tensor_copy(out=d8i_all[:, i, :], in_=d8f[:]), reads=["d8f%d" % k for k in range(8)], writes=["d8i%d" % i])
            for k in range(8):
                S.dma("pool", lambda e, i=i, j=j, k=k: e.indirect_dma_start(
                    out=xs_d, out_offset=bass.IndirectOffsetOnAxis(ap=d8i_all[:, i, k:k + 1], axis=0),
                    in_=h2g[j][:], in_offset=None, bounds_check=bcreg(e), oob_is_err=False),
                    reads=["h2g%d" % j, "d8i%d" % i], writes=["xs_sc_%d_%d" % (i, k)])
        if debug:
            S.dma("sp", lambda e: e.dma_start(out=dbg_d8, in_=d8i_all[:].rearrange("p a b -> p (a b)")), reads=["d8i%d" % i for i in range(NT)])
            S.dma("sp", lambda e: e.dma_start(out=dbg_w8, in_=w8_all[:].rearrange("p a b -> p (a b)")), reads=["w8_all%d" % i for i in range(NT)])
        S.barrier()
        if stage <= 5:
            zt = A.alloc("zt", [128, D], F32)
            S.dve(lambda e: e.memset(zt[:], 0.0), writes=["zt"])
            S.dma("sp", lambda e: e.dma_start(out=out[0:128, :], in_=zt[:]), reads=["zt"])
            S.emit(st)
            return nc, S

        A.release(plan_mark)
        blkbc_i = A.alloc("blkbc_i", [128, 2 * NBLK], I32)
        blkbc_f = A.alloc("blkbc_f", [128, 2 * NBLK], F32)
        idxw_f = A.alloc("idxw_f", [128, NBLK], F32)
        idxw = A.alloc("idxw", [128, NBLK], I32)
        pcol_i = A.alloc("pcol_i", [128, 1], I32)
        pcol = A.alloc("pcol", [128, 1], F32)
        S.dma("sp", lambda e: e.dma_start(out=blkbc_i[:], in_=blk_d.rearrange("r b -> (r b)").partition_broadcast(128)), reads=["blk_d0", "blk_d1"], writes=["blkbc_i"])
        S.dve(lambda e: e.tensor_copy(out=blkbc_f[:], in_=blkbc_i[:]), reads=["blkbc_i"], writes=["blkbc_f"])
        S.pool(lambda e: e.iota(pcol_i[:], pattern=[[0, 1]], base=0, channel_multiplier=1), writes=["pcol_i"])
        S.dve(lambda e: e.tensor_copy(out=pcol[:], in_=pcol_i[:]), reads=["pcol_i"], writes=["pcol"])
        S.dve(lambda e: e.tensor_scalar(out=idxw_f[:], in0=blkbc_f[:, 0:NBLK], scalar1=128.0, scalar2=pcol[:, 0:1], op0=ALU.mult, op1=ALU.add), reads=["blkbc_f", "pcol"], writes=["idxw_f"])
        S.dve(lambda e: e.tensor_scalar(out=blkbc_f[:, NBLK:2 * NBLK], in0=blkbc_f[:, NBLK:2 * NBLK], scalar1=-1.0e6, scalar2=1.0e6, op0=ALU.mult, op1=ALU.add), reads=["blkbc_f"], writes=["blkbc_f"])
        S.dve(lambda e: e.tensor_tensor(out=idxw_f[:], in0=idxw_f[:], in1=blkbc_f[:, NBLK:2 * NBLK], op=ALU.add), reads=["idxw_f", "blkbc_f"], writes=["idxw_f"])
        idxw2 = A.alloc("idxw2", [128, NBLK], I32)
        gef = A.alloc("gef", [128, NBLK], F32)
        S.dve(lambda e: e.tensor_scalar(out=gef[:], in0=idxw_f[:], scalar1=16384.0, scalar2=1.0e6, op0=ALU.is_ge, op1=ALU.mult), reads=["idxw_f"], writes=["gef"])
        S.dve(lambda e: e.tensor_tensor(out=blkbc_f[:, 0:NBLK], in0=idxw_f[:], in1=gef[:], op=ALU.add), reads=["idxw_f", "gef", "blkbc_f"], writes=["blkbc_f"])
        S.dve(lambda e: e.tensor_copy(out=idxw[:], in_=blkbc_f[:, 0:NBLK]), reads=["blkbc_f"], writes=["idxw"])
        S.dve(lambda e: e.tensor_scalar(out=idxw_f[:], in0=idxw_f[:], scalar1=1.0e6 - 16384.0, scalar2=None, op0=ALU.add), reads=["idxw_f", "blkbc_f"], writes=["idxw_f"])
        S.dve(lambda e: e.tensor_tensor(out=idxw_f[:], in0=idxw_f[:], in1=gef[:], op=ALU.subtract), reads=["idxw_f", "gef"], writes=["idxw_f"])
        S.dve(lambda e: e.tensor_copy(out=idxw2[:], in_=idxw_f[:]), reads=["idxw_f"], writes=["idxw2"])
        convert_experts(256)
        NWB = 6
        NXB = 6
        PFD = NWB - 1
        A2 = Arena(nc)
        A2.off, A2.limit, A2.n = off_sb, off_sb + 2 * NT * 256 * 4, 5000
        wbuf = [A2.alloc("wbuf%d" % i, [128, 6144], BF16) for i in range(5)] + [A.alloc("wbuf%d" % i, [128, 6144], BF16) for i in range(5, NWB)]
        for i in range(NWB):
            S.pool(lambda e, i=i: e.memset(wbuf[i][:], 0.0), writes=["wbuf%dh0" % i, "wbuf%dh1" % i])
        Xb = [A.alloc("Xb%d" % i, [128, D], BF16) for i in range(NXB)]
        XT = [A.alloc("XT%d" % i, [128, 8, 128], BF16) for i in range(2)]
        Yb = [A.alloc("Yb%d" % i, [128, D], BF16) for i in range(2)]
        actE = [A.alloc("actE%d" % i, [128, 2, 128], BF16) for i in range(2)]
        sglE = [A.alloc("sglE%d" % i, [128, 256], F32) for i in range(2)]
        wreg = {}
        W16N = ["w16_%d" % e_ for e_ in range(256)]

        def wbound(e):
            if "r" not in wreg:
                wreg["r"] = e.alloc_register("wbnd")
                e.reg_mov(wreg["r"], 128 * 128 - 1)
            return wreg["r"]
        nblk_run = int(os.environ.get("KNBLK", str(NBLK)))

        def st_wload(b):
            s_ = b % NWB
            for hf, ix in ((0, idxw), (1, idxw2)):
                S.dma("pool", lambda e, hf=hf, ix=ix: e.indirect_dma_start(out=wbuf[s_][:], out_offset=None, in_=w16_d[hf],
                                                                         in_offset=bass.IndirectOffsetOnAxis(ap=ix[:, b:b + 1], axis=0), bounds_check=wbound(e), oob_is_err=False),
                      reads=["idxw", "idxw2"] + W16N, writes=["wbuf%dh%d" % (s_, hf)])

        def st_xload(b):
            jx = b % NXB
            if os.environ.get("KNOX", "0") == "1" and b > 8:
                return
            S.dma("sp", lambda e: e.dma_start(out=Xb[jx][:], in_=xs_d[b * 128:(b + 1) * 128, :]), writes=["Xb%d" % jx])

        def st_A(b):
            j, jx = b % 2, b % NXB

            def fxt(e):
                ins = None
                for kc in range(8):
                    ins = e.transpose(out=PSB[j][:, kc * 128:(kc + 1) * 128], in_=Xb[jx][:, kc * 128:(kc + 1) * 128], identity=ident_b[:])
                return ins
            S.pe(fxt, reads=["Xb%d" % jx, "ident_b"], writes=["ps%d" % j])
            S.act(lambda e: e.activation(out=XT[j][:].rearrange("p k t -> p (k t)"), in_=PSB[j][:, 0:1024], func=AF.Copy), reads=["ps%d" % j], writes=["XT%d" % j])

        def st_B(b):
            j, s_ = b % 2, b % NWB
            gb_ = 2 + j

            def fgu(e):
                ins = None
                for m in range(4):
                    base = (0 if m < 2 else 2048) + (m % 2) * 128
                    for kc in range(8):
                        ins = e.matmul(PS[gb_][:, m * 128:(m + 1) * 128], lhsT=wbuf[s_][:, base + kc * 256:base + kc * 256 + 128], rhs=XT[j][:, kc, :], start=(kc == 0), stop=(kc == 7))
                return ins
            S.pe(fgu, reads=["XT%d" % j, "wbuf%dh0" % s_, "wbuf%dh1" % s_], writes=["ps%d" % gb_])
            S.act(lambda e: e.activation(out=sglE[j][:], in_=PS[gb_][:, 0:256], func=AF.Silu), reads=["ps%d" % gb_], writes=["sglE%d" % j])
            S.dve(lambda e: e.tensor_tensor(out=actE[j][:].rearrange("p m t -> p (m t)"), in0=PS[gb_][:, 256:512], in1=sglE[j][:], op=ALU.mult), reads=["ps%d" % gb_, "sglE%d" % j], writes=["actE%d" % j])

        def st_C(b):
            j, s_ = b % 2, b % NWB
            yb_ = 4 + 2 * j

            def fy(e):
                ins = None
                for half in range(2):
                    for m in range(2):
                        ins = e.matmul(PS[yb_ + half][:, :], lhsT=actE[j][:, m, :], rhs=wbuf[s_][:, 4096 + m * 1024 + half * 512:4096 + m * 1024 + (half + 1) * 512], start=(m == 0), stop=(m == 1))
                return ins
            S.pe(fy, reads=["actE%d" % j, "wbuf%dh0" % s_, "wbuf%dh1" % s_], writes=["ps%d" % yb_, "ps%d" % (yb_ + 1)])
            S.act(lambda e: e.activation(out=Yb[j][:, 0:512], in_=PS[yb_][:, :], func=AF.Copy), reads=["ps%d" % yb_], writes=["Yb%d_0" % j])
            S.dve(lambda e: e.tensor_copy(out=Yb[j][:, 512:1024], in_=PS[yb_ + 1][:, :]), reads=["ps%d" % (yb_ + 1)], writes=["Yb%d_1" % j])
            if os.environ.get("KNOX", "0") == "1" and b > 8:
                return
            S.dma("sp", lambda e: e.dma_start(out=ys_d[b * 128:(b + 1) * 128, :], in_=Yb[j][:]), reads=["Yb%d_0" % j, "Yb%d_1" % j], writes=["ys_st_%d" % b])

        for b in range(min(PFD, nblk_run)):
            st_wload(b)
        for b in range(min(PFD, nblk_run)):
            st_xload(b)
        for t in range(-1, nblk_run + 1):
            if 0 <= t + 1 < nblk_run:
                st_A(t + 1)
            if 0 <= t < nblk_run:
                st_B(t)
            if 0 <= t - 1 < nblk_run:
                st_C(t - 1)
            if t >= 0 and t + PFD < nblk_run:
                st_wload(t + PFD)
                st_xload(t + PFD)
        S.barrier()
        A.release(plan_mark)
        fg_bc = A.alloc("fg_bc", [128, D], F32)
        S.dma("sp", lambda e: e.dma_start(out=fg_bc[:], in_=final_g.partition_broadcast(128)), writes=["fg_bc"])
        Gk = [[A.alloc("G%d_%d" % (jj, k), [128, D], BF16) for k in range(8)] for jj in range(2)]
        accA = [A.alloc("accA%d" % i, [128, D], F32) for i in range(2)]
        xpt = [A.alloc("xpt%d" % i, [128, D], F32) for i in range(2)]
        junkf = A.alloc("junkf", [128, D], F32)
        ssf = [A.alloc("ssf%d" % i, [128, 4], F32) for i in range(2)]
        for jj in range(2):
            for k in range(8):
                S.pool(lambda e, jj=jj, k=k: e.memset(Gk[jj][k][:], 0.0), writes=["G%d_%d" % (jj, k)])
        for i in range(NT):
            jj = i % 2
            S.dma("sp", lambda e, i=i, jj=jj: e.dma_start(out=xpt[jj][:], in_=x2p_d[i * 128:(i + 1) * 128, :]), reads=["x2p_d%d" % i], writes=["xpt%d" % jj])
            for k in range(8):
                S.dma("pool", lambda e, i=i, jj=jj, k=k: e.indirect_dma_start(out=Gk[jj][k][:], out_offset=None, in_=ys_d,
                                                                            in_offset=bass.IndirectOffsetOnAxis(ap=d8i_all[:, i, k:k + 1], axis=0), bounds_check=bcreg(e), oob_is_err=False),
                      reads=["d8i%d" % i, "G%d_%d" % (jj, k)], writes=["G%d_%d" % (jj, k)])
            an = "accA%d" % jj
            S.dve(lambda e, i=i, jj=jj: e.tensor_scalar(out=accA[jj][:], in0=Gk[jj][0][:], scalar1=w8_all[:, i, 0:1], scalar2=None, op0=ALU.mult), reads=["G%d_0" % jj, "w8_all%d" % i], writes=[an])
            for k in range(1, 8):
                S.dve(lambda e, i=i, jj=jj, k=k: e.scalar_tensor_tensor(out=accA[jj][:], in0=Gk[jj][k][:], scalar=w8_all[:, i, k:k + 1], in1=accA[jj][:], op0=ALU.mult, op1=ALU.add),
                      reads=["G%d_%d" % (jj, k), "w8_all%d" % i, an], writes=[an])
            S.dve(lambda e, jj=jj: e.tensor_tensor(out=accA[jj][:], in0=accA[jj][:], in1=gate2_bc[:], op=ALU.mult), reads=[an, "gate2_bc"], writes=[an])
            S.dve(lambda e, jj=jj: e.tensor_tensor(out=accA[jj][:], in0=accA[jj][:], in1=xpt[jj][:], op=ALU.add), reads=[an, "xpt%d" % jj], writes=[an])
            sn = "ssf%d" % jj
            S.act(lambda e, jj=jj: e.activation(out=junkf[:], in_=accA[jj][:], func=AF.Square, accum_out=ssf[jj][:, 0:1]), reads=[an], writes=["junkf", sn])
            S.dve(lambda e, jj=jj: e.tensor_scalar(out=ssf[jj][:, 1:2], in0=ssf[jj][:, 0:1], scalar1=1.0 / D, scalar2=EPS, op0=ALU.mult, op1=ALU.add), reads=[sn], writes=[sn + "b"])
            S.act(lambda e, jj=jj: e.activation(out=ssf[jj][:, 2:3], in_=ssf[jj][:, 1:2], func=AF.Sqrt), reads=[sn + "b"], writes=[sn + "c"])
            S.dve(lambda e, jj=jj: e.reciprocal(out=ssf[jj][:, 3:4], in_=ssf[jj][:, 2:3]), reads=[sn + "c"], writes=[sn + "d"])
            S.dve(lambda e, jj=jj: e.scalar_tensor_tensor(out=accA[jj][:], in0=accA[jj][:], scalar=ssf[jj][:, 3:4], in1=fg_bc[:], op0=ALU.mult, op1=ALU.mult),
                  reads=[an, sn + "d", "fg_bc"], writes=[an])
            S.dma("sp", lambda e, i=i, jj=jj: e.dma_start(out=out[i * 128:(i + 1) * 128, :], in_=accA[jj][:]), reads=[an], writes=["out%d" % i])
        S.emit(st)
    return nc, S


_CACHE = {}


def pack_expert_weights(wg, wu, wd):
    o = np.empty((256, 128, 6144), np.float32)
    o[:, :, 0:2048] = wg.reshape(256, 8, 128, 256).transpose(0, 2, 1, 3).reshape(256, 128, 2048)
    o[:, :, 2048:4096] = wu.reshape(256, 8, 128, 256).transpose(0, 2, 1, 3).reshape(256, 128, 2048)
    o[:, :, 4096:6144] = wd.reshape(256, 2, 128, 1024).transpose(0, 2, 1, 3).reshape(256, 128, 2048)
    return o.reshape(256 * 128, 6144)


def kernel(**inputs):
    n = 8
    if "nc" not in _CACHE:
        _CACHE["nc"] = build(stage=int(os.environ.get("KSTAGE", "99")))[0]
    nc = _CACHE["nc"]
    shared = {k: np.ascontiguousarray(v[0]) for k, v in inputs.items()
              if k not in ("x", "c", "lb_logits", "final_g", "w_exp_gate", "w_exp_up", "w_exp_down")}
    shared["w_exp_all"] = pack_expert_weights(inputs["w_exp_gate"][0], inputs["w_exp_up"][0], inputs["w_exp_down"][0])
    shared["lb_logits"] = np.ascontiguousarray(inputs["lb_logits"])
    shared["final_g"] = np.ascontiguousarray(inputs["final_g"])
    in_maps = []
    for b in range(n):
        m = dict(shared)
        m["x"] = np.ascontiguousarray(inputs["x"][b])
        m["c"] = np.ascontiguousarray(inputs["c"][b])
        in_maps.append(m)
    res = run_bass_kernel_spmd(nc, in_maps, core_ids=list(range(n)))
    return np.stack([r["out"] for r in res.results], axis=0)
```

```python
import os
from contextlib import ExitStack
import numpy as np
import concourse.bass as bass
import concourse.mybir as mybir
from concourse.bass_utils import run_bass_kernel_spmd

F32 = mybir.dt.float32
BF16 = mybir.dt.bfloat16
I32 = mybir.dt.int32
AF = mybir.ActivationFunctionType
ALU = mybir.AluOpType
AX = mybir.AxisListType

D = 1024
SEQ = 4096
NT = SEQ // 128
EPS = 1e-6
NBLK = 512


class Sched:
    ENGS = ("pe", "act", "dve", "pool", "sp")
    DMA_RING = {"sp": 12, "pool": 12, "act": 6}

    def __init__(self, nc):
        self.nc = nc
        self.ops = []

    def op(self, eng, fn, reads=(), writes=(), dma=False, extra=(), nobar=False):
        self.ops.append(dict(eng=eng, fn=fn, reads=tuple(reads), writes=tuple(writes), dma=dma, extra=tuple(extra), nobar=nobar))
        return len(self.ops) - 1

    def pe(self, fn, reads=(), writes=()):
        return self.op("pe", fn, reads, writes)

    def act(self, fn, reads=(), writes=()):
        return self.op("act", fn, reads, writes)

    def dve(self, fn, reads=(), writes=()):
        return self.op("dve", fn, reads, writes)

    def pool(self, fn, reads=(), writes=()):
        return self.op("pool", fn, reads, writes)

    def dma(self, eng, fn, reads=(), writes=(), nobar=False):
        return self.op(eng, fn, reads, writes, dma=True, nobar=nobar)

    def barrier(self):
        n = len(self.ops)
        lastc = {}
        dmas = []
        for i, o in enumerate(self.ops):
            if o["dma"]:
                if not o["nobar"]:
                    dmas.append(i)
            else:
                lastc[o["eng"]] = i
        start = getattr(self, "_bar_from", 0)
        ex = [i for i in dmas if i >= start] + list(lastc.values())
        for e in self.ENGS:
            self.op(e, lambda eng: eng.nop(), extra=ex)
        self._bar_from = len(self.ops)

    def emit(self, stack):
        nc = self.nc
        ops = self.ops
        n = len(ops)
        last_w, readers, deps = {}, {}, [None] * n
        for i, o in enumerate(ops):
            d = set(o["extra"])
            for b in o["reads"]:
                w = last_w.get(b)
                if w is not None:
                    d.add(w)
            for b in o["writes"]:
                w = last_w.get(b)
                if w is not None:
                    d.add(w)
                d.update(readers.get(b, ()))
            d.discard(i)
            for b in o["reads"]:
                readers.setdefault(b, []).append(i)
            for b in o["writes"]:
                last_w[b] = i
                readers[b] = []
            deps[i] = d
        signal = [False] * n
        for i, o in enumerate(ops):
            for j in deps[i]:
                pj = ops[j]
                if pj["dma"]:
                    continue
                if pj["eng"] != o["eng"] or pj["eng"] != "pe":
                    signal[j] = True
        seq = [0] * n
        cnt = {e: 0 for e in self.ENGS}
        dcnt = {e: 0 for e in self.ENGS}
        dnum = [0] * n
        for i, o in enumerate(ops):
            if o["dma"]:
                dnum[i] = dcnt[o["eng"]]
                dcnt[o["eng"]] += 1
            elif signal[i]:
                cnt[o["eng"]] += 1
                seq[i] = cnt[o["eng"]]
        esem = {e: stack.enter_context(nc.semaphore("S_" + e)) for e in self.ENGS}
        dsem = {}
        for e, r in self.DMA_RING.items():
            if dcnt[e] > 0:
                dsem[e] = [stack.enter_context(nc.semaphore("D_%s%d" % (e, k))) for k in range(r)]
        self.stats = dict(n_ops=n, signals=dict(cnt), dmas=dict(dcnt))
        per_eng = {e: [i for i, o in enumerate(ops) if o["eng"] == e] for e in self.ENGS}
        RING = self.DMA_RING

        def run_engine(e, eng):
            known = {x: 0 for x in self.ENGS}
            dknown = {}
            for i in per_eng[e]:
                o = ops[i]
                wc, wd = {}, {}
                for j in deps[i]:
                    pj = ops[j]
                    if pj["dma"]:
                        r = RING[pj["eng"]]
                        key = (pj["eng"], dnum[j] % r)
                        v = 16 * (dnum[j] // r + 1)
                        if dknown.get(key, 0) < v:
                            wd[key] = max(wd.get(key, 0), v)
                    else:
                        if pj["eng"] == e and e == "pe":
                            continue
                        if known[pj["eng"]] < seq[j]:
                            wc[pj["eng"]] = max(wc.get(pj["eng"], 0), seq[j])
                if o["dma"]:
                    r = RING[e]
                    if dnum[i] >= r:
                        key = (e, dnum[i] % r)
                        v = 16 * (dnum[i] // r)
                        if dknown.get(key, 0) < v:
                            wd[key] = max(wd.get(key, 0), v)
                for x, v in wc.items():
                    eng.wait_ge(esem[x], v)
                    known[x] = v
                for key, v in wd.items():
                    eng.wait_ge(dsem[key[0]][key[1]], v)
                    dknown[key] = v
                ins = o["fn"](eng)
                if o["dma"]:
                    ins.then_inc(dsem[e][dnum[i] % RING[e]], 16)
                elif signal[i]:
                    ins.then_inc(esem[e], 1)
            if dcnt[e] > 0:
                r = RING[e]
                for k in range(min(r, dcnt[e])):
                    last = ((dcnt[e] - 1 - k) // r) * r + k
                    v = 16 * (last // r + 1)
                    if dknown.get((e, k), 0) < v:
                        eng.wait_ge(dsem[e][k], v)

        with nc.Block() as block:

            @block.tensor
            def _(eng):
                run_engine("pe", eng)

            @block.scalar
            def _(eng):
                run_engine("act", eng)

            @block.vector
            def _(eng):
                run_engine("dve", eng)

            @block.gpsimd
            def _(eng):
                run_engine("pool", eng)

            @block.sync
            def _(eng):
                run_engine("sp", eng)


class Arena:
    def __init__(self, nc, limit=228 * 1024):
        self.nc, self.off, self.limit, self.n = nc, 17 * 1024, limit, 0

    def alloc(self, name, shape, dt):
        esz = 4 if dt in (F32, I32) else 2
        nbytes = int(np.prod(shape[1:])) * esz
        self.off = (self.off + 31) // 32 * 32
        self.n += 1
        t = self.nc.alloc_sbuf_tensor_at("%s_%d" % (name, self.n), list(shape), dt, offset=self.off)
        self.off += nbytes
        assert self.off <= self.limit, ("SBUF overflow", name, self.off)
        return t

    def mark(self):
        return self.off

    def release(self, m):
        self.off = m


def build(stage=99, debug=False):
    nc = bass.Bass("TRN2", target_bir_lowering=False)
    kin = "ExternalInput"

    def din(name, shape):
        return nc.dram_tensor(name, list(shape), F32, kind=kin).ap()

    x = din("x", [SEQ, D])
    c = din("c", [D])
    ada_w = din("ada_w", [D, 6 * D])
    ada_b = din("ada_b", [6 * D])
    norm1_g = din("norm1_g", [D])
    w_in = din("w_in", [D, 6400])
    lb_logits = din("lb_logits", [2, 512])
    hg_norm_g = din("hg_norm_g", [512])
    w_branch_a = din("w_branch_a", [512, D])
    w_branch_b = din("w_branch_b", [256, D])
    w_out = din("w_out", [D, D])
    norm2_g = din("norm2_g", [D])
    w_router = din("w_router", [D, 256])
    router_bias = din("router_bias", [256])
    w_exp_all = din("w_exp_all", [256 * 128, 6144])
    w_sh_gate = din("w_sh_gate", [D, 256])
    w_sh_up = din("w_sh_up", [D, 256])
    w_sh_down = din("w_sh_down", [256, D])
    final_g = din("final_g", [D])
    out = nc.dram_tensor("out", [SEQ, D], F32, kind="ExternalOutput").ap()
    skind = "ExternalOutput" if debug else "Internal"
    mod_d = nc.dram_tensor("mod_d", [48, 128], F32, kind=skind).ap()
    yaT_d = nc.dram_tensor("yaT_d", [4, 128, SEQ], BF16, kind=skind).ap()
    ybT_d = nc.dram_tensor("ybT_d", [2, 128, SEQ], BF16, kind=skind).ap()
    x2p_d = nc.dram_tensor("x2p_d", [SEQ, D], F32, kind=skind).ap()
    h2_d = nc.dram_tensor("h2_d", [SEQ, D], BF16, kind=skind).ap()
    xs_d = nc.dram_tensor("xs_d", [NBLK * 128, D], BF16, kind="Internal").ap()
    ys_d = nc.dram_tensor("ys_d", [NBLK * 128, D], BF16, kind="Internal").ap()
    blk_d = nc.dram_tensor("blk_d", [2, NBLK], I32, kind=skind).ap()
    w16_d = [nc.dram_tensor("w16_d%d" % i, [128 * 128, 6144], BF16, kind="Internal").ap() for i in range(2)]
    dbg_hT = nc.dram_tensor("dbg_hT", [8, 128, SEQ], BF16, kind="ExternalOutput").ap() if debug else None
    dbg_d8 = nc.dram_tensor("dbg_d8", [128, NT * 8], I32, kind="ExternalOutput").ap() if debug else None
    dbg_w8 = nc.dram_tensor("dbg_w8", [128, NT * 8], F32, kind="ExternalOutput").ap() if debug else None

    st = ExitStack()
    with st:
        st.enter_context(nc.allow_low_precision("bf16 matmul operands, fp32 accumulation"))
        st.enter_context(nc.allow_non_contiguous_dma("small strided parameter loads"))
        S = Sched(nc)
        A = Arena(nc)
        PS = [nc.alloc_psum_tensor("ps%d" % i, [128, 512], F32) for i in range(8)]
        CV = {"n": 0}

        def convert_experts(k):
            for _ in range(k):
                e_ = CV["n"]
                if e_ >= 256:
                    return
                CV["n"] += 1
                S.dma("pool", lambda e, e_=e_: e.dma_start(out=w16_d[e_ // 128][(e_ % 128) * 128:(e_ % 128 + 1) * 128, :], in_=w_exp_all[e_ * 128:(e_ + 1) * 128, :]),
                      writes=["w16_%d" % e_], nobar=True)
        PSB = [p[:].bitcast(BF16) for p in PS]

        ident_f = A.alloc("ident_f", [128, 128], F32)
        ident_b = A.alloc("ident_b", [128, 128], BF16)
        io_i = A.alloc("io_i", [128, 128], I32)
        io_f = A.alloc("io_f", [128, 128], F32)
        S.pool(lambda e: e.iota(io_i[:], pattern=[[1, 128]], base=0, channel_multiplier=-1), writes=["io_i"])
        S.dve(lambda e: e.tensor_copy(out=io_f[:], in_=io_i[:]), reads=["io_i"], writes=["io_f"])
        S.dve(lambda e: e.tensor_single_scalar(out=ident_f[:], in_=io_f[:], scalar=0.0, op=ALU.is_equal), reads=["io_f"], writes=["ident_f"])
        S.dve(lambda e: e.tensor_copy(out=ident_b[:], in_=ident_f[:]), reads=["ident_f"], writes=["ident_b"])
        ones_b = A.alloc("ones_b", [128, 128], BF16)
        S.dve(lambda e: e.memset(ones_b[:], 1.0), writes=["ones_b"])
        modT = A.alloc("modT", [128, 48], F32)
        gate1_bc = A.alloc("gate1_bc", [128, D], F32)
        gate2_bc = A.alloc("gate2_bc", [128, D], F32)
        a_bc = A.alloc("a_bc", [128, D], F32)
        sh_bc = A.alloc("sh_bc", [128, D], F32)
        hT = A.alloc("hT", [128, 8, SEQ], BF16)
        base_mark = A.mark()

        def HTn(i):
            return "hTt%d" % i

        m0 = A.mark()
        sc = A.alloc("sc", [128, 8], F32)
        crow = A.alloc("crow", [8, 128], F32)
        adab = A.alloc("adab", [48, 128], F32)
        S.dma("sp", lambda e: e.dma_start(out=crow[:], in_=c.rearrange("(k p) -> k p", p=128)), writes=["crow"])
        S.dma("sp", lambda e: e.dma_start(out=adab[:], in_=ada_b.rearrange("(k p) -> k p", p=128)), writes=["adab"])
        S.pe(lambda e: e.transpose(out=PS[1][:, 0:8], in_=crow[:, :], identity=ident_f[0:8, 0:8]), reads=["crow", "ident_f"], writes=["ps1"])
        S.act(lambda e: e.activation(out=sc[:], in_=PS[1][:, 0:8], func=AF.Silu), reads=["ps1"], writes=["sc"])
        awb = [A.alloc("awb%d" % i, [128, 8, 1024], F32) for i in range(2)]
        for pc in range(6):
            buf = awb[pc % 2]
            bn = "awb%d" % (pc % 2)
            S.dma("sp", lambda e, buf=buf, pc=pc: e.dma_start(
                out=buf[:], in_=ada_w[:, pc * 1024:(pc + 1) * 1024].rearrange("(k p) c -> p k c", p=128)), writes=[bn])

            def f(e, buf=buf, pc=pc):
                ins = None
                for j in range(8):
                    jc = pc * 8 + j
                    for k in range(8):
                        ins = e.matmul(PS[0][:, jc:jc + 1], lhsT=buf[:, k, j * 128:(j + 1) * 128], rhs=sc[:, k:k + 1],
                                       start=(k == 0), stop=(k == 7))
                return ins
            S.pe(f, reads=[bn, "sc"], writes=["ps0"])
        S.dve(lambda e: e.tensor_copy(out=modT[:], in_=PS[0][:, 0:48]), reads=["ps0"], writes=["modT"])
        S.pe(lambda e: e.transpose(out=PS[1][0:48, 0:128], in_=modT[:, 0:48], identity=ident_f[:]), reads=["modT", "ident_f"], writes=["ps1"])
        modrow = A.alloc("modrow", [48, 128], F32)
        S.dve(lambda e: e.tensor_tensor(out=modrow[:], in0=PS[1][0:48, 0:128], in1=adab[:], op=ALU.add), reads=["ps1", "adab"], writes=["modrow"])
        S.dma("sp", lambda e: e.dma_start(out=mod_d, in_=modrow[:]), reads=["modrow"], writes=["mod_d"])

        def mod_bc(dst, name, j0):
            src = mod_d[j0:j0 + 8, :].rearrange("a b -> (a b)").partition_broadcast(128)
            S.dma("sp", lambda e: e.dma_start(out=dst[:], in_=src), reads=["mod_d"], writes=[name])

        mod_bc(gate1_bc, "gate1_bc", 16)
        mod_bc(gate2_bc, "gate2_bc", 40)

        def load_norm_consts(gvec, j_shift, j_scale):
            mod_bc(sh_bc, "sh_bc", j_shift)
            mod_bc(a_bc, "a_bc", j_scale)
            gtmp = A.alloc("gtmp", [128, D], F32)
            S.dma("sp", lambda e: e.dma_start(out=gtmp[:], in_=gvec.partition_broadcast(128)), writes=["gtmp"])
            S.dve(lambda e: e.scalar_tensor_tensor(out=a_bc[:], in0=a_bc[:], scalar=1.0, in1=gtmp[:], op0=ALU.add, op1=ALU.mult),
                  reads=["a_bc", "gtmp"], writes=["a_bc"])

        def norm_mod_T(xt, xname, hb, hbname, ss, ssname, junk, junkname):
            S.act(lambda e: e.activation(out=junk[:], in_=xt[:], func=AF.Square, accum_out=ss[:, 0:1]),
                  reads=[xname], writes=[junkname, ssname])
            S.dve(lambda e: e.tensor_scalar(out=ss[:, 1:2], in0=ss[:, 0:1], scalar1=1.0 / D, scalar2=EPS, op0=ALU.mult, op1=ALU.add),
                  reads=[ssname], writes=[ssname + "b"])
            S.act(lambda e: e.activation(out=ss[:, 3:4], in_=ss[:, 1:2], func=AF.Sqrt),
                  reads=[ssname + "b"], writes=[ssname + "d"])
            S.dve(lambda e: e.reciprocal(out=ss[:, 2:3], in_=ss[:, 3:4]),
                  reads=[ssname + "d"], writes=[ssname + "c"])
            S.dve(lambda e: e.scalar_tensor_tensor(out=junk[:], in0=xt[:], scalar=ss[:, 2:3], in1=a_bc[:], op0=ALU.mult, op1=ALU.mult),
                  reads=[xname, ssname + "c", "a_bc", junkname], writes=[junkname])
            S.dve(lambda e: e.tensor_tensor(out=hb[:], in0=junk[:], in1=sh_bc[:], op=ALU.add),
                  reads=[junkname, "sh_bc"], writes=[hbname])

        if stage <= 0:
            zt = A.alloc("zt", [128, D], F32)
            S.dve(lambda e: e.memset(zt[:], 0.0), writes=["zt"])
            S.dma("sp", lambda e: e.dma_start(out=out[0:128, :], in_=zt[:]), reads=["zt"])
            S.emit(st)
            return nc, S
        load_norm_consts(norm1_g, 0, 8)
        xts = [A.alloc("xt%d" % i, [128, D], F32) for i in range(3)]
        junks = [A.alloc("junk%d" % i, [128, D], F32) for i in range(2)]
        hbs = [A.alloc("hb%d" % i, [128, D], BF16) for i in range(2)]
        ssq1 = A.alloc("ssq1", [128, NT], F32)
        rstd1 = A.alloc("rstd1", [128, NT], F32)
        for i in range(NT):
            xt, xn_ = xts[i % 3], "xt%d" % (i % 3)
            S.dma("sp", lambda e, xt=xt, i=i: e.dma_start(out=xt[:], in_=x[i * 128:(i + 1) * 128, :]), writes=[xn_])
            S.act(lambda e, xt=xt, i=i: e.activation(out=junks[i % 2][:], in_=xt[:], func=AF.Square, accum_out=ssq1[:, i:i + 1]),
                  reads=[xn_], writes=["junk%d" % (i % 2), "ssq1_%d" % i])
            convert_experts(1)
        S.dve(lambda e: e.tensor_scalar(out=rstd1[:], in0=ssq1[:], scalar1=1.0 / D, scalar2=EPS, op0=ALU.mult, op1=ALU.add), reads=["ssq1_%d" % i for i in range(NT)], writes=["rstd1a"])
        S.act(lambda e: e.activation(out=ssq1[:], in_=rstd1[:], func=AF.Sqrt), reads=["rstd1a"], writes=["ssq1b"])
        S.dve(lambda e: e.reciprocal(out=rstd1[:], in_=ssq1[:]), reads=["ssq1b", "rstd1a"], writes=["rstd1"])
        for i in range(NT):
            xt, xn_ = xts[i % 3], "xt%d" % (i % 3)
            S.dma("sp", lambda e, xt=xt, i=i: e.dma_start(out=xt[:], in_=x[i * 128:(i + 1) * 128, :]), writes=[xn_])
            j = i % 2
            S.dve(lambda e, xt=xt, i=i, j=j: e.scalar_tensor_tensor(out=junks[j][:], in0=xt[:], scalar=rstd1[:, i:i + 1], in1=a_bc[:], op0=ALU.mult, op1=ALU.mult),
                  reads=[xn_, "rstd1", "a_bc"], writes=["junk%d" % j])
            S.dve(lambda e, j=j: e.tensor_tensor(out=hbs[j][:], in0=junks[j][:], in1=sh_bc[:], op=ALU.add), reads=["junk%d" % j, "sh_bc"], writes=["hb%d" % j])
            pb = 2 + (i % 2)

            def f(e, i=i, j=j, pb=pb):
                ins = None
                for kc in range(8):
                    ins = e.transpose(out=PSB[pb][:, kc * 128:(kc + 1) * 128], in_=hbs[j][:, kc * 128:(kc + 1) * 128], identity=ident_b[:])
                return ins
            S.pe(f, reads=["hb%d" % j, "ident_b"], writes=["ps%d" % pb])
            S.act(lambda e, i=i, pb=pb: e.activation(out=hT[:, :, i * 128:(i + 1) * 128], in_=PSB[pb][:, 0:1024].rearrange("p (k t) -> p k t", k=8), func=AF.Copy),
                  reads=["ps%d" % pb], writes=["hTt%d" % i])
        A.release(m0)
        if os.environ.get('KBAR', '1') == '1':
            S.barrier()
        if debug:
            for kc in range(8):
                S.dma("sp", lambda e, kc=kc: e.dma_start(out=dbg_hT[kc], in_=hT[:, kc, :]), reads=["hTt%d" % i for i in range(NT)])
        if stage <= 1:
            zt = A.alloc("zt", [128, D], F32)
            S.dve(lambda e: e.memset(zt[:], 0.0), writes=["zt"])
            S.dma("sp", lambda e: e.dma_start(out=out[0:128, :], in_=zt[:]), reads=["zt"])
            S.emit(st)
            return nc, S

        m2 = A.mark()
        wA = A.alloc("wA", [128, 8, 2048], BF16)
        S.dma("pool", lambda e: e.dma_start(out=wA[:], in_=w_in[:, 0:2048].rearrange("(k p) c -> p k c", p=128)), writes=["wA"])
        lbt = A.alloc("lbt", [128, 2, 4], F32)
        lb = A.alloc("lb", [128, 4], F32)
        oml = A.alloc("oml", [128, 4], F32)
        for r_ in range(2):
            S.dma("sp", lambda e, r_=r_: e.dma_start(out=lbt[:, r_, :], in_=lb_logits[r_].rearrange("(h k) -> k h", k=128)), writes=["lbt"])
        S.dve(lambda e: e.tensor_tensor(out=lb[:], in0=lbt[:, 0, :], in1=lbt[:, 1, :], op=ALU.subtract), reads=["lbt"], writes=["lb"])
        S.act(lambda e: e.activation(out=lb[:], in_=lb[:], func=AF.Sigmoid), reads=["lb"], writes=["lb"])
        S.dve(lambda e: e.tensor_scalar(out=oml[:], in0=lb[:], scalar1=-1.0, scalar2=1.0, op0=ALU.mult, op1=ALU.add), reads=["lb"], writes=["oml"])
        gn_bc = A.alloc("gn_bc", [128, 512], F32)
        S.dma("sp", lambda e: e.dma_start(out=gn_bc[:], in_=hg_norm_g.partition_broadcast(128)), writes=["gn_bc"])
        rmask = A.alloc("rmask", [128, 8, 64], F32)
        S.dve(lambda e: e.memset(rmask[:], 1.0), writes=["rmask"])
        S.dve(lambda e: e.memset(rmask[:, :, 0:1], 0.0), reads=["rmask"], writes=["rmask"])
        hmask = A.alloc("hmask", [128, 128], F32)
        S.dve(lambda e: e.tensor_single_scalar(out=hmask[:], in_=io_f[:], scalar=0.0, op=ALU.is_ge), reads=["io_f"], writes=["hmask"])
        S.dve(lambda e: e.memset(hmask[0:64, 64:128], 0.0), reads=["hmask"], writes=["hmask"])
        Sst = A.alloc("Sst", [128, 4, 128], F32)
        S.dve(lambda e: e.memset(Sst[:], 0.0), writes=["Sst"])
        sbf = [A.alloc("sbf%d" % i, [128, 512], BF16) for i in range(2)]
        sig = A.alloc("sig", [128, 512], F32)
        ff = A.alloc("ff", [128, 512], F32)
        logf = A.alloc("logf", [128, 512], F32)
        bb = A.alloc("bb", [128, 8, 64], F32)
        eb = A.alloc("eb", [128, 512], F32)
        enb = A.alloc("enb", [128, 512], F32)
        kk = A.alloc("kk", [128, 512], F32)
        dd = A.alloc("dd", [128, 8, 64], F32)
        ed = A.alloc("ed", [128, 512], F32)
        dec = A.alloc("dec", [128, 8], F32)
        qe = A.alloc("qe", [128, 512], BF16)
        ke = A.alloc("ke", [128, 512], BF16)
        kendT = A.alloc("kendT", [128, 512], BF16)
        kend_tm = A.alloc("kend_tm", [128, 512], BF16)
        v_sb = A.alloc("v_sb", [128, 512], BF16)
        ATb = A.alloc("ATb", [128, 4, 128], BF16)
        sg = A.alloc("sg", [128, 512], F32)
        t1 = A.alloc("t1", [128, 4, 128], F32)
        t2 = A.alloc("t2", [128, 512], F32)
        ya = A.alloc("ya", [128, 512], BF16)
        yaT_sb = A.alloc("yaT_sb", [128, 4, 128], BF16)
        ssq = A.alloc("ssq", [128, 12], F32)
        junkh = A.alloc("junkh", [128, 128], F32)
        bbf = bb[:].rearrange("p a b -> p (a b)")
        ddf = dd[:].rearrange("p a b -> p (a b)")
        rmf = rmask[:].rearrange("p a b -> p (a b)")
        sig2 = [sig, A.alloc("sigB", [128, 512], F32)]
        v_sb2 = [v_sb, A.alloc("v_sbB", [128, 512], BF16)]
        sg2 = [sg, A.alloc("sgB", [128, 512], F32)]
        qsb2 = [A.alloc("qsbA", [128, 512], F32), A.alloc("qsbB", [128, 512], F32)]

        def hg_proj(i):
            tok = slice(i * 128, (i + 1) * 128)
            hr = [HTn(i)]
            def fproj(e):
                ins = None
                for h in range(4):
                    for kc in range(8):
                        ins = e.matmul(PS[0][:, h * 128:(h + 1) * 128], lhsT=wA[:, kc, h * 128:(h + 1) * 128], rhs=hT[:, kc, tok], start=(kc == 0), stop=(kc == 7))
                for h in range(4):
                    for kc in range(8):
                        ins = e.matmul(PS[1][:, h * 128:(h + 1) * 128], lhsT=wA[:, kc, 512 + h * 128:512 + (h + 1) * 128], rhs=hT[:, kc, tok], start=(kc == 0), stop=(kc == 7))
                for kc in range(8):
                    ins = e.matmul(PS[2][:, :], lhsT=hT[:, kc, tok], rhs=wA[:, kc, 1024:1536], start=(kc == 0), stop=(kc == 7))
                for kc in range(8):
                    ins = e.matmul(PS[3][:, :], lhsT=hT[:, kc, tok], rhs=wA[:, kc, 1536:2048], start=(kc == 0), stop=(kc == 7))
                return ins
            S.pe(fproj, reads=hr + ["wA"], writes=["ps0", "ps1", "ps2", "ps3"])
            S.act(lambda e: e.activation(out=sig2[i % 2][:], in_=PS[1][:, :], func=AF.Sigmoid), reads=["ps1"], writes=["sig%d" % (i % 2)])
            S.act(lambda e: e.activation(out=v_sb2[i % 2][:], in_=PS[2][:, :], func=AF.Copy), reads=["ps2"], writes=["v_sb%d" % (i % 2)])
            S.act(lambda e: e.activation(out=sg2[i % 2][:], in_=PS[3][:, :], func=AF.Sigmoid), reads=["ps3"], writes=["sg%d" % (i % 2)])
            S.dve(lambda e: e.tensor_tensor(out=sg2[i % 2][:], in0=PS[3][:, :], in1=sg2[i % 2][:], op=ALU.mult), reads=["ps3", "sg%d" % (i % 2)], writes=["sg%d" % (i % 2)])
            S.act(lambda e: e.activation(out=qsb2[i % 2][:], in_=PS[0][:, :], func=AF.Copy), reads=["ps0"], writes=["qsb%d" % (i % 2)])

        def hg_main(i):
            tok = slice(i * 128, (i + 1) * 128)
            def faff(e):
                ins = None
                for h in range(4):
                    ins = e.tensor_scalar(out=ff[:, h * 128:(h + 1) * 128], in0=sig2[i % 2][:, h * 128:(h + 1) * 128], scalar1=oml[:, h:h + 1], scalar2=lb[:, h:h + 1], op0=ALU.mult, op1=ALU.add)
                return ins
            S.dve(faff, reads=["sig%d" % (i % 2), "oml", "lb"], writes=["ff"])
            S.act(lambda e: e.activation(out=logf[:], in_=ff[:], func=AF.Ln), reads=["ff"], writes=["logf"])
            S.pool(lambda e: e.tensor_scalar(out=kk[:], in0=ff[:], scalar1=-1.0, scalar2=1.0, op0=ALU.mult, op1=ALU.add), reads=["ff"], writes=["kk"])
            S.dve(lambda e: e.tensor_tensor_scan(out=bbf, data0=rmf, data1=logf[:], initial=0.0, op0=ALU.mult, op1=ALU.add), reads=["rmask", "logf"], writes=["bb"])
            S.act(lambda e: e.activation(out=eb[:], in_=bbf, func=AF.Exp), reads=["bb"], writes=["eb"])
            S.act(lambda e: e.activation(out=enb[:], in_=bbf, func=AF.Exp, scale=-1.0), reads=["bb"], writes=["enb"])
            S.act(lambda e: e.activation(out=dec[:], in_=bb[:, :, 63], func=AF.Exp), reads=["bb"], writes=["dec"])
            S.dve(lambda e: e.tensor_tensor(out=dd[:], in0=bb[:, :, 63:64].to_broadcast([128, 8, 64]), in1=bb[:], op=ALU.subtract), reads=["bb"], writes=["dd"])
            S.act(lambda e: e.activation(out=ed[:], in_=ddf, func=AF.Exp), reads=["dd"], writes=["ed"])
            S.dve(lambda e: e.tensor_tensor(out=qe[:], in0=qsb2[i % 2][:], in1=eb[:], op=ALU.mult), reads=["qsb%d" % (i % 2), "eb"], writes=["qe"])
            S.pool(lambda e: e.tensor_tensor(out=ke[:], in0=kk[:], in1=enb[:], op=ALU.mult), reads=["kk", "enb"], writes=["ke"])
            S.pool(lambda e: e.tensor_tensor(out=kendT[:], in0=kk[:], in1=ed[:], op=ALU.mult), reads=["kk", "ed"], writes=["kendT"])
            if i + 1 < NT:
                hg_proj(i + 1)

            def ftr(e):
                ins = None
                for h in range(4):
                    ins = e.transpose(out=PSB[7][:, h * 128:(h + 1) * 128], in_=kendT[:, h * 128:(h + 1) * 128], identity=ident_b[:])
                return ins
            S.pe(ftr, reads=["kendT", "ident_b"], writes=["ps7"])
            S.act(lambda e: e.activation(out=kend_tm[:], in_=PSB[7][:, 0:512], func=AF.Copy), reads=["ps7"], writes=["kend_tm"])

            def fat(e):
                ins = None
                for h in range(4):
                    ins = e.matmul(PS[4][:, h * 128:(h + 1) * 128], lhsT=ke[:, h * 128:(h + 1) * 128], rhs=qe[:, h * 128:(h + 1) * 128], start=True, stop=True)
                return ins
            S.pe(fat, reads=["ke", "qe"], writes=["ps4"])
            S.dve(lambda e: e.tensor_tensor(out=ATb[:], in0=PS[4][:, :].rearrange("p (h t) -> p h t", h=4), in1=hmask[:].unsqueeze(1).to_broadcast([128, 4, 128]), op=ALU.mult),
                  reads=["ps4", "hmask"], writes=["ATb"])
            for ci in range(2):
                r0 = 64 * ci
                S.act(lambda e, ci=ci: e.activation(out=sbf[ci][:], in_=Sst[:].rearrange("p h v -> p (h v)"), func=AF.Copy), reads=["Sst"], writes=["sbf%d" % ci])

                def fu(e, r0=r0):
                    ins = None
                    for h in range(4):
                        ins = e.matmul(PS[5][:, h * 128:(h + 1) * 128], lhsT=kend_tm[r0:r0 + 64, h * 128:(h + 1) * 128], rhs=v_sb2[i % 2][r0:r0 + 64, h * 128:(h + 1) * 128], start=True, stop=True)
                    return ins
                S.pe(fu, reads=["kend_tm", "v_sb%d" % (i % 2)], writes=["ps5"])
                S.dve(lambda e, ci=ci: e.tensor_tensor(out=Sst[:], in0=Sst[:], in1=dec[:].rearrange("p (h c) -> p h c", c=2)[:, :, ci:ci + 1].to_broadcast([128, 4, 128]), op=ALU.mult),
                      reads=["Sst", "dec"], writes=["Sst"])
                S.dve(lambda e: e.tensor_tensor(out=Sst[:].rearrange("p h v -> p (h v)"), in0=Sst[:].rearrange("p h v -> p (h v)"), in1=PS[5][:, :], op=ALU.add),
                      reads=["Sst", "ps5"], writes=["Sst"])

            def fo(e):
                ins = None
                for h in range(4):
                    hc = slice(h * 128, (h + 1) * 128)
                    e.matmul(PS[6][:, hc], lhsT=ATb[:, h, :], rhs=v_sb2[i % 2][:, hc], start=True, stop=False)
                    e.matmul(PS[6][0:64, hc], lhsT=qe[:, h * 128:h * 128 + 64], rhs=sbf[0][:, hc], start=False, stop=False)
                    ins = e.matmul(PS[6][64:128, hc], lhsT=qe[:, h * 128 + 64:h * 128 + 128], rhs=sbf[1][:, hc], start=False, stop=True)
                return ins
            S.pe(fo, reads=["ATb", "v_sb%d" % (i % 2), "qe", "sbf0", "sbf1"], writes=["ps6"])

            def fsq(e):
                ins = None
                for h in range(4):
                    ins = e.activation(out=junkh[:], in_=PS[6][:, h * 128:(h + 1) * 128], func=AF.Square, accum_out=ssq[:, h:h + 1])
                return ins
            S.act(fsq, reads=["ps6"], writes=["ssq", "junkh"])
            S.dve(lambda e: e.tensor_scalar(out=ssq[:, 4:8], in0=ssq[:, 0:4], scalar1=1.0 / 128, scalar2=EPS, op0=ALU.mult, op1=ALU.add), reads=["ssq"], writes=["ssqb"])
            S.act(lambda e: e.activation(out=ssq[:, 8:12], in_=ssq[:, 4:8], func=AF.Ln), reads=["ssqb"], writes=["ssqc"])
            S.act(lambda e: e.activation(out=ssq[:, 4:8], in_=ssq[:, 8:12], func=AF.Exp, scale=-0.5), reads=["ssqc", "ssqb"], writes=["ssqb"])
            S.dve(lambda e: e.tensor_tensor(out=t1[:], in0=PS[6][:, :].rearrange("p (h v) -> p h v", h=4), in1=ssq[:, 4:8].unsqueeze(2).to_broadcast([128, 4, 128]), op=ALU.mult),
                  reads=["ps6", "ssqb"], writes=["t1"])
            S.pool(lambda e: e.tensor_tensor(out=t2[:], in0=t1[:].rearrange("p h v -> p (h v)"), in1=sg2[i % 2][:], op=ALU.mult), reads=["t1", "sg%d" % (i % 2)], writes=["t2"])
            S.pool(lambda e: e.tensor_tensor(out=ya[:], in0=t2[:], in1=gn_bc[:], op=ALU.mult), reads=["t2", "gn_bc"], writes=["ya"])

            def fyt(e):
                ins = None
                for h in range(4):
                    ins = e.transpose(out=PSB[7][:, 512 + h * 128:512 + (h + 1) * 128], in_=ya[:, h * 128:(h + 1) * 128], identity=ident_b[:])
                return ins
            S.pe(fyt, reads=["ya", "ident_b"], writes=["ps7b"])
            S.act(lambda e: e.activation(out=yaT_sb[:].rearrange("p f t -> p (f t)"), in_=PSB[7][:, 512:1024], func=AF.Copy), reads=["ps7b"], writes=["yaT_sb"])
            S.dma("sp", lambda e, tok=tok: e.dma_start(out=yaT_d[:, :, tok].rearrange("f p t -> p f t"), in_=yaT_sb[:]), reads=["yaT_sb"], writes=["yaT_d"])
            if i < NT - 2:
                convert_experts(3)
        hg_proj(0)
        for i in range(NT):
            hg_main(i)
        A.release(m2)
        S.barrier()
        if stage <= 2:
            zt = A.alloc("zt", [128, D], F32)
            S.dve(lambda e: e.memset(zt[:], 0.0), writes=["zt"])
            S.dma("sp", lambda e: e.dma_start(out=out[0:128, :], in_=zt[:]), reads=["zt"])
            S.emit(st)
            return nc, S

        m3 = A.mark()
        wB = A.alloc("wB", [128, 8, 2304], BF16)
        S.dma("pool", lambda e: e.dma_start(out=wB[:], in_=w_in[:, 2048:4352].rearrange("(k p) c -> p k c", p=128)), writes=["wB"])
        mk = A.alloc("mk", [128, 4, 128], BF16)
        mk0 = A.alloc("mk0", [128, 4, 128], BF16)
        S.dve(lambda e: e.tensor_single_scalar(out=mk[:, 0, :], in_=io_f[:], scalar=0.0, op=ALU.is_le), reads=["io_f"], writes=["mk"])
        S.dve(lambda e: e.tensor_single_scalar(out=mk[:, 1, :], in_=io_f[:], scalar=0.0, op=ALU.is_ge), reads=["io_f", "mk"], writes=["mk"])
        S.dve(lambda e: e.tensor_copy(out=mk[:, 2:4, :], in_=mk[:, 0:2, :]), reads=["mk"], writes=["mk"])
        S.dve(lambda e: e.tensor_copy(out=mk0[:], in_=mk[:]), reads=["mk"], writes=["mk0"])
        S.dve(lambda e: e.memset(mk0[:, 0, :], 0.0), reads=["mk0"], writes=["mk0"])
        S.dve(lambda e: e.memset(mk0[:, 2, :], 0.0), reads=["mk0"], writes=["mk0"])
        QT = A.alloc("QT", [128, 2, SEQ], BF16)
        S.dve(lambda e: e.memset(QT[64:128, 0, :], 0.0), writes=["QTz0"])
        S.dve(lambda e: e.memset(QT[0:64, 1, :], 0.0), writes=["QTz1"])
        KT = A.alloc("KT", [128, SEQ], BF16)
        Vb = A.alloc("Vb", [128, 32, 128], BF16)
        numT = A.alloc("numT", [128, SEQ], F32)
        denT = A.alloc("denT", [128, SEQ], F32)
        PT2 = [A.alloc("PT%d" % i, [128, 512], BF16) for i in range(2)]
        PTm2 = [A.alloc("PTm%d" % i, [128, 512], BF16) for i in range(2)]
        ybo = A.alloc("ybo", [128, SEQ], BF16)
        allh = [HTn(i) for i in range(NT)]
        for hp in range(2):
            for g, dil in enumerate((1, 4, 16)[:int(os.environ.get('KG', '3'))]):
                qc = 256 * g + 128 * hp
                kc0 = 768 + qc
                vc0 = 1536 + qc
                for tb in range(8):
                    tsl = slice(tb * 512, (tb + 1) * 512)

                    def fq(e, tsl=tsl, qc=qc, kc0=kc0):
                        ins = None
                        for kc in range(8):
                            ins = e.matmul(PS[0][:, :], lhsT=wB[:, kc, qc:qc + 128], rhs=hT[:, kc, tsl], start=(kc == 0), stop=(kc == 7))
                        for kc in range(8):
                            ins = e.matmul(PS[1][:, :], lhsT=wB[:, kc, kc0:kc0 + 128], rhs=hT[:, kc, tsl], start=(kc == 0), stop=(kc == 7))
                        return ins
                    S.pe(fq, reads=allh + ["wB"], writes=["ps0", "ps1"])
                    S.act(lambda e, tsl=tsl: e.activation(out=QT[0:64, 0, tsl], in_=PS[0][0:64, :], func=AF.Copy, scale=0.125), reads=["ps0", "QTz0"], writes=["QTa"])
                    S.act(lambda e, tsl=tsl: e.activation(out=QT[64:128, 1, tsl], in_=PS[0][64:128, :], func=AF.Copy, scale=0.125), reads=["ps0", "QTz1"], writes=["QTb"])
                    S.dve(lambda e, tsl=tsl: e.tensor_copy(out=KT[:, tsl], in_=PS[1][:, :]), reads=["ps1"], writes=["KT"])
                L = SEQ // dil
                nb = L // 128
                blocks = [(r, n_) for r in range(dil) for n_ in range(nb)]

                def tokslice(r, n_, dil=dil):
                    st_ = 128 * n_ * dil + r
                    return slice(st_, st_ + 127 * dil + 1, dil) if dil > 1 else slice(st_, st_ + 128)
                for b4 in range(8):
                    def fv(e, b4=b4, vc0=vc0, blocks=blocks, tokslice=tokslice):
                        ins = None
                        for q_ in range(4):
                            r, n_ = blocks[b4 * 4 + q_]
                            for kc in range(8):
                                ins = e.matmul(PS[2][:, q_ * 128:(q_ + 1) * 128], lhsT=hT[:, kc, tokslice(r, n_)], rhs=wB[:, kc, vc0:vc0 + 128], start=(kc == 0), stop=(kc == 7))
                        return ins
                    S.pe(fv, reads=allh + ["wB"], writes=["ps2"])
                    S.act(lambda e, b4=b4: e.activation(out=Vb[:, b4 * 4:(b4 + 1) * 4, :].rearrange("p a b -> p (a b)"), in_=PS[2][:, :], func=AF.Copy), reads=["ps2"], writes=["Vb"])
                def blk_params(bi):
                    r, n_ = blocks[bi]
                    qs = tokslice(r, n_)
                    ks = [tokslice(r, n_ - 1) if n_ > 0 else qs, qs]
                    vbi = [bi - 1 if n_ > 0 else bi, bi]
                    return n_, qs, ks, vbi

                def att_A(bi):
                    n_, qs, ks, vbi = blk_params(bi)
                    pb = 3 + (bi % 2)
                    p2 = bi % 2

                    def fs(e):
                        ins = None
                        for h in range(2):
                            for kb in range(2):
                                ins = e.matmul(PS[pb][:, (h * 2 + kb) * 128:(h * 2 + kb + 1) * 128], lhsT=KT[:, ks[kb]], rhs=QT[:, h, qs], start=True, stop=True)
                        return ins
                    S.pe(fs, reads=["QTa", "QTb", "KT"], writes=["ps%d" % pb])
                    S.act(lambda e: e.activation(out=PT2[p2][:], in_=PS[pb][:, :], func=AF.Exp), reads=["ps%d" % pb], writes=["PT%d" % p2])
                    mm_ = mk if n_ > 0 else mk0
                    S.dve(lambda e: e.tensor_tensor(out=PTm2[p2][:], in0=PT2[p2][:], in1=mm_[:].rearrange("p a b -> p (a b)"), op=ALU.mult), reads=["PT%d" % p2, "mk", "mk0"], writes=["PTm%d" % p2])

                def att_B(bi):
                    n_, qs, ks, vbi = blk_params(bi)
                    ob = 5 + (bi % 2)
                    p2 = bi % 2

                    def fpv(e):
                        ins = None
                        for h in range(2):
                            for kb in range(2):
                                ins = e.matmul(PS[ob][h * 64:(h + 1) * 64, 0:128], lhsT=Vb[:, vbi[kb], h * 64:(h + 1) * 64], rhs=PTm2[p2][:, (h * 2 + kb) * 128:(h * 2 + kb + 1) * 128], start=(kb == 0), stop=(kb == 1))
                        for h in range(2):
                            for kb in range(2):
                                ins = e.matmul(PS[ob][h * 64:(h + 1) * 64, 128:256], lhsT=ones_b[:, 0:64], rhs=PTm2[p2][:, (h * 2 + kb) * 128:(h * 2 + kb + 1) * 128], start=(kb == 0), stop=(kb == 1))
                        return ins
                    S.pe(fpv, reads=["Vb", "PTm%d" % p2, "ones_b"], writes=["ps%d" % ob])
                    if bi % 2 == 0 and not (hp == 1 and g == 2):
                        convert_experts(1)
                    if g == 0:
                        S.act(lambda e: e.activation(out=numT[:, qs], in_=PS[ob][:, 0:128], func=AF.Copy), reads=["ps%d" % ob], writes=["numT"])
                        S.act(lambda e: e.activation(out=denT[:, qs], in_=PS[ob][:, 128:256], func=AF.Copy), reads=["ps%d" % ob], writes=["denT"])
                    else:
                        S.dve(lambda e: e.tensor_tensor(out=numT[:, qs], in0=PS[ob][:, 0:128], in1=numT[:, qs], op=ALU.add), reads=["ps%d" % ob, "numT"], writes=["numT"])
                        S.dve(lambda e: e.tensor_tensor(out=denT[:, qs], in0=PS[ob][:, 128:256], in1=denT[:, qs], op=ALU.add), reads=["ps%d" % ob, "denT"], writes=["denT"])

                nbk = min(len(blocks), int(os.environ.get('KNB', '9999')))
                for bi in range(nbk + 1):
                    if bi < nbk:
                        att_A(bi)
                    if bi >= 1:
                        att_B(bi - 1)
            S.dve(lambda e: e.reciprocal(out=denT[:], in_=denT[:]), reads=["denT"], writes=["denT"])
            S.dve(lambda e: e.tensor_tensor(out=ybo[:], in0=numT[:], in1=denT[:], op=ALU.mult), reads=["numT", "denT"], writes=["ybo"])
            S.dma("sp", lambda e, hp=hp: e.dma_start(out=ybT_d[hp], in_=ybo[:]), reads=["ybo"], writes=["ybT_d"])
        A.release(m3)
        S.barrier()
        if stage <= 3:
            zt = A.alloc("zt", [128, D], F32)
            S.dve(lambda e: e.memset(zt[:], 0.0), writes=["zt"])
            S.dma("sp", lambda e: e.dma_start(out=out[0:128, :], in_=zt[:]), reads=["zt"])
            S.emit(st)
            return nc, S

        m4 = A.mark()
        wC = A.alloc("wC", [128, 8, 2048], BF16)
        wa = A.alloc("wa", [128, 4, 1024], BF16)
        wb_ = A.alloc("wb_", [128, 2, 1024], BF16)
        wo = A.alloc("wo", [128, 8, 1024], BF16)
        S.dma("pool", lambda e: e.dma_start(out=wC[:], in_=w_in[:, 4352:6400].rearrange("(k p) c -> p k c", p=128)), writes=["wC"])
        S.dma("pool", lambda e: e.dma_start(out=wa[:], in_=w_branch_a.rearrange("(k p) c -> p k c", p=128)), writes=["wa"])
        S.dma("pool", lambda e: e.dma_start(out=wb_[:], in_=w_branch_b.rearrange("(k p) c -> p k c", p=128)), writes=["wb_"])
        S.dma("pool", lambda e: e.dma_start(out=wo[:], in_=w_out.rearrange("(k p) c -> p k c", p=128)), writes=["wo"])
        yaTb = [A.alloc("yaTb%d" % i, [128, 4, 512], BF16) for i in range(2)]
        ybTb = [A.alloc("ybTb%d" % i, [128, 2, 512], BF16) for i in range(2)]
        mergedT = A.alloc("mergedT", [128, 8, 512], BF16)
        sga = [A.alloc("sga%d" % i, [128, 512], F32) for i in range(2)]
        sgb = [A.alloc("sgb%d" % i, [128, 512], F32) for i in range(2)]
        m1 = [A.alloc("m1_%d" % i, [128, 512], F32) for i in range(2)]
        m2_ = [A.alloc("m2_%d" % i, [128, 512], F32) for i in range(2)]
        xt2 = [A.alloc("xt2_%d" % i, [128, D], F32) for i in range(2)]
        zt2 = [A.alloc("zt2_%d" % i, [128, D], F32) for i in range(2)]
        for tb in range(8):
            tsl = slice(tb * 512, (tb + 1) * 512)
            j = tb % 2
            S.dma("sp", lambda e, j=j, tsl=tsl: e.dma_start(out=yaTb[j][:], in_=yaT_d[:, :, tsl].rearrange("f p t -> p f t")), reads=["yaT_d"], writes=["yaTb%d" % j])
            S.dma("sp", lambda e, j=j, tsl=tsl: e.dma_start(out=ybTb[j][:], in_=ybT_d[:, :, tsl].rearrange("f p t -> p f t")), reads=["ybT_d"], writes=["ybTb%d" % j])
            hrd = [HTn(tb * 4 + q_) for q_ in range(4)]
            for cc in range(8):
                csl = slice(cc * 128, (cc + 1) * 128)
                pp = cc % 2
                b0 = 4 * pp

                def fm(e, j=j, tsl=tsl, csl=csl, cc=cc, b0=b0):
                    ins = None
                    for fc in range(4):
                        ins = e.matmul(PS[b0][:, :], lhsT=wa[:, fc, csl], rhs=yaTb[j][:, fc, :], start=(fc == 0), stop=(fc == 3))
                    for fc in range(2):
                        ins = e.matmul(PS[b0 + 1][:, :], lhsT=wb_[:, fc, csl], rhs=ybTb[j][:, fc, :], start=(fc == 0), stop=(fc == 1))
                    for kc in range(8):
                        ins = e.matmul(PS[b0 + 2][:, :], lhsT=wC[:, kc, csl], rhs=hT[:, kc, tsl], start=(kc == 0), stop=(kc == 7))
                    for kc in range(8):
                        ins = e.matmul(PS[b0 + 3][:, :], lhsT=wC[:, kc, 1024 + cc * 128:1024 + (cc + 1) * 128], rhs=hT[:, kc, tsl], start=(kc == 0), stop=(kc == 7))
                    return ins
                S.pe(fm, reads=hrd + ["wa", "wb_", "wC", "yaTb%d" % j, "ybTb%d" % j], writes=["ps%d" % (b0 + k_) for k_ in range(4)])
                S.act(lambda e, pp=pp, b0=b0: e.activation(out=sga[pp][:], in_=PS[b0 + 2][:, :], func=AF.Sigmoid), reads=["ps%d" % (b0 + 2)], writes=["sga%d" % pp])
                S.act(lambda e, pp=pp, b0=b0: e.activation(out=sgb[pp][:], in_=PS[b0 + 3][:, :], func=AF.Sigmoid), reads=["ps%d" % (b0 + 3)], writes=["sgb%d" % pp])
                S.dve(lambda e, pp=pp, b0=b0: e.tensor_tensor(out=m1[pp][:], in0=PS[b0][:, :], in1=sga[pp][:], op=ALU.mult), reads=["ps%d" % b0, "sga%d" % pp], writes=["m1_%d" % pp])
                S.dve(lambda e, pp=pp, b0=b0: e.tensor_tensor(out=m2_[pp][:], in0=PS[b0 + 1][:, :], in1=sgb[pp][:], op=ALU.mult), reads=["ps%d" % (b0 + 1), "sgb%d" % pp], writes=["m2_%d" % pp])
                S.pool(lambda e, cc=cc, pp=pp: e.tensor_tensor(out=mergedT[:, cc, :], in0=m1[pp][:], in1=m2_[pp][:], op=ALU.add), reads=["m1_%d" % pp, "m2_%d" % pp], writes=["mergedT%d" % cc])
                if tb < 7:
                    convert_experts(1)
            for q_ in range(4):
                i = tb * 4 + q_
                jj = i % 2
                S.dma("sp", lambda e, i=i, jj=jj: e.dma_start(out=xt2[jj][:], in_=x[i * 128:(i + 1) * 128, :]), writes=["xt2_%d" % jj])

                zb = 2 * (q_ % 2)

                def fz(e, q_=q_, zb=zb):
                    ins = None
                    for half in range(2):
                        for cc in range(8):
                            ins = e.matmul(PS[zb + half][:, :], lhsT=mergedT[:, cc, q_ * 128:(q_ + 1) * 128], rhs=wo[:, cc, half * 512:(half + 1) * 512], start=(cc == 0), stop=(cc == 7))
                    return ins
                S.pe(fz, reads=["mergedT%d" % cc for cc in range(8)] + ["wo"], writes=["ps%d" % zb, "ps%d" % (zb + 1)])
                for half in range(2):
                    hs = slice(half * 512, (half + 1) * 512)
                    S.dve(lambda e, jj=jj, half=half, hs=hs, zb=zb: e.tensor_tensor(out=zt2[jj][:, hs], in0=PS[zb + half][:, :], in1=gate1_bc[:, hs], op=ALU.mult),
                          reads=["ps%d" % (zb + half), "gate1_bc"], writes=["zt2_%d_%d" % (jj, half)])
                S.pool(lambda e, jj=jj: e.tensor_tensor(out=zt2[jj][:], in0=zt2[jj][:], in1=xt2[jj][:], op=ALU.add),
                       reads=["zt2_%d_0" % jj, "zt2_%d_1" % jj, "xt2_%d" % jj], writes=["zt2_%d_0" % jj, "zt2_%d_1" % jj])
                S.dma("sp", lambda e, i=i, jj=jj: e.dma_start(out=x2p_d[i * 128:(i + 1) * 128, :], in_=zt2[jj][:]), reads=["zt2_%d_0" % jj, "zt2_%d_1" % jj], writes=["x2p_d%d" % i])
        A.release(m4)
        S.barrier()
        A.release(base_mark)
        A.off -= 8 * SEQ * 2
        off_sb = (A.off + 31) // 32 * 32
        sb_all = A.alloc("sb_all", [128, NT, 256], F32)
        pos_all = A.alloc("pos_all", [128, NT, 256], F32)
        m8_all = A.alloc("m8_all", [128, NT, 8], F32)
        w8_all = A.alloc("w8_all", [128, NT, 8], F32)
        carry = A.alloc("carry", [128, 256], F32)
        route_mark = A.mark()
        m5 = A.mark()
        load_norm_consts(norm2_g, 24, 32)
        wr = A.alloc("wr", [128, 8, 256], BF16)
        wsg = A.alloc("wsg", [128, 8, 512], BF16)
        wsd = A.alloc("wsd", [128, 2, 1024], BF16)
        S.dma("pool", lambda e: e.dma_start(out=wr[:], in_=w_router.rearrange("(k p) c -> p k c", p=128)), writes=["wr"])
        S.dma("pool", lambda e: e.dma_start(out=wsg[:, :, 0:256], in_=w_sh_gate.rearrange("(k p) c -> p k c", p=128)), writes=["wsg_a"])
        S.dma("pool", lambda e: e.dma_start(out=wsg[:, :, 256:512], in_=w_sh_up.rearrange("(k p) c -> p k c", p=128)), writes=["wsg_b"])
        S.dma("pool", lambda e: e.dma_start(out=wsd[:], in_=w_sh_down.rearrange("(k p) c -> p k c", p=128)), writes=["wsd"])
        rb_bc = A.alloc("rb_bc", [128, 256], F32)
        S.dma("sp", lambda e: e.dma_start(out=rb_bc[:], in_=router_bias.partition_broadcast(128)), writes=["rb_bc"])
        tri_b = A.alloc("tri_b", [128, 128], BF16)
        S.dve(lambda e: e.tensor_single_scalar(out=tri_b[:], in_=io_f[:], scalar=0.0, op=ALU.is_gt), reads=["io_f"], writes=["tri_b"])
        S.dve(lambda e: e.memset(carry[:], 0.0), writes=["carry"])
        junk2 = [A.alloc("junk2_%d" % i, [128, D], F32) for i in range(2)]
        h2b = [A.alloc("h2b%d" % i, [128, D], BF16) for i in range(2)]
        ss2 = [A.alloc("ss2_%d" % i, [128, 4], F32) for i in range(2)]
        h2T = [A.alloc("h2T%d" % i, [128, 8, 128], BF16) for i in range(2)]
        scr = A.alloc("scr", [128, 256], F32)
        maskb = A.alloc("maskb", [128, 256], BF16)
        jk = A.alloc("jk", [128, 256], F32)
        s8 = A.alloc("s8", [128, 12], F32)
        m8t = A.alloc("m8t", [128, 8], F32)
        sgl = A.alloc("sgl", [128, 256], F32)
        actT = A.alloc("actT", [128, 2, 128], BF16)
        yt = [A.alloc("yt%d" % i, [128, D], F32) for i in range(2)]
        NX1 = 3
        x1t = [A.alloc("x1u%d" % i, [128, D], F32) for i in range(NX1)]
        ssq_all = A.alloc("ssq_all", [128, NT], F32)
        rstd_all = A.alloc("rstd_all", [128, NT], F32)
        for i in range(NT):
            jx = i % NX1
            S.dma("sp", lambda e, i=i, jx=jx: e.dma_start(out=x1t[jx][:], in_=x2p_d[i * 128:(i + 1) * 128, :]), reads=["x2p_d%d" % i], writes=["x1u%d" % jx])
            S.act(lambda e, i=i, jx=jx: e.activation(out=junk2[i % 2][:], in_=x1t[jx][:], func=AF.Square, accum_out=ssq_all[:, i:i + 1]),
                  reads=["x1u%d" % jx], writes=["junk2_%d" % (i % 2), "ssq%d" % i])
        S.dve(lambda e: e.tensor_scalar(out=rstd_all[:], in0=ssq_all[:], scalar1=1.0 / D, scalar2=EPS, op0=ALU.mult, op1=ALU.add), reads=["ssq%d" % i for i in range(NT)], writes=["rstd_a"])
        S.act(lambda e: e.activation(out=ssq_all[:], in_=rstd_all[:], func=AF.Sqrt), reads=["rstd_a"], writes=["ssq_b"])
        S.dve(lambda e: e.reciprocal(out=rstd_all[:], in_=ssq_all[:]), reads=["ssq_b", "rstd_a"], writes=["rstd_all"])

        def stP(i):
            j = i % 2
            jx = i % NX1
            S.dma("sp", lambda e: e.dma_start(out=x1t[jx][:], in_=x2p_d[i * 128:(i + 1) * 128, :]), reads=["x2p_d%d" % i], writes=["x1u%d" % jx])
            S.dve(lambda e: e.scalar_tensor_tensor(out=junk2[j][:], in0=x1t[jx][:], scalar=rstd_all[:, i:i + 1], in1=a_bc[:], op0=ALU.mult, op1=ALU.mult),
                  reads=["x1u%d" % jx, "rstd_all", "a_bc"], writes=["junk2_%d" % j])
            S.dve(lambda e: e.tensor_tensor(out=h2b[j][:], in0=junk2[j][:], in1=sh_bc[:], op=ALU.add), reads=["junk2_%d" % j, "sh_bc"], writes=["h2b%d" % j])
            S.dma("sp", lambda e: e.dma_start(out=h2_d[i * 128:(i + 1) * 128, :], in_=h2b[j][:]), reads=["h2b%d" % j], writes=["h2_d%d" % i])

            def ft(e, j=j):
                ins = None
                for kc in range(8):
                    ins = e.transpose(out=PSB[0][:, kc * 128:(kc + 1) * 128], in_=h2b[j][:, kc * 128:(kc + 1) * 128], identity=ident_b[:])
                return ins
            S.pe(ft, reads=["h2b%d" % j, "ident_b"], writes=["ps0"])
            S.act(lambda e, j=j: e.activation(out=h2T[j][:].rearrange("p k t -> p (k t)"), in_=PSB[0][:, 0:1024], func=AF.Copy), reads=["ps0"], writes=["h2T%d" % j])

        def stQ(i):
            j = i % 2

            def frt(e, j=j):
                ins = None
                for kc in range(8):
                    ins = e.matmul(PS[1][:, 0:256], lhsT=h2T[j][:, kc, :], rhs=wr[:, kc, :], start=(kc == 0), stop=(kc == 7))
                return ins
            S.pe(frt, reads=["h2T%d" % j, "wr"], writes=["ps1"])
            S.act(lambda e: e.activation(out=scr[:], in_=PS[1][:, 0:256], func=AF.Sigmoid), reads=["ps1"], writes=["scr"])
            S.dve(lambda e, i=i: e.tensor_tensor(out=sb_all[:, i, :], in0=scr[:], in1=rb_bc[:], op=ALU.add), reads=["scr", "rb_bc"], writes=["sb_all%d" % i])
            S.dve(lambda e, i=i: e.max(out=m8t[:], in_=sb_all[:, i, :]), reads=["sb_all%d" % i], writes=["m8t"])
            S.dve(lambda e, i=i: e.tensor_scalar(out=maskb[:], in0=sb_all[:, i, :], scalar1=m8t[:, 7:8], scalar2=None, op0=ALU.is_ge),
                  reads=["sb_all%d" % i, "m8t"], writes=["maskb"])
            S.dve(lambda e, i=i: e.scalar_tensor_tensor(out=sb_all[:, i, :], in0=sb_all[:, i, :], scalar=m8t[:, 7:8], in1=scr[:], op0=ALU.is_ge, op1=ALU.mult),
                  reads=["sb_all%d" % i, "m8t", "scr", "maskb"], writes=["sb_all%d" % i])
            S.dve(lambda e, i=i: e.max(out=m8_all[:, i, :], in_=sb_all[:, i, :]), reads=["sb_all%d" % i], writes=["m8_all%d" % i])
            S.dve(lambda e, i=i: e.tensor_reduce(out=s8[:, 8:9], in_=m8_all[:, i, :], axis=AX.X, op=ALU.add), reads=["m8_all%d" % i], writes=["s8s"])
            S.dve(lambda e: e.reciprocal(out=s8[:, 9:10], in_=s8[:, 8:9]), reads=["s8s"], writes=["s8r"])
            S.dve(lambda e, i=i: e.tensor_scalar(out=w8_all[:, i, :], in0=m8_all[:, i, :], scalar1=s8[:, 9:10], scalar2=2.5, op0=ALU.mult, op1=ALU.mult),
                  reads=["s8r", "m8_all%d" % i], writes=["w8_all%d" % i])

            def fsh(e, j=j):
                ins = None
                for m in range(4):
                    for kc in range(8):
                        ins = e.matmul(PS[3][:, m * 128:(m + 1) * 128], lhsT=wsg[:, kc, m * 128:(m + 1) * 128], rhs=h2T[j][:, kc, :], start=(kc == 0), stop=(kc == 7))
                return ins
            S.pe(fsh, reads=["h2T%d" % j, "wsg_a", "wsg_b"], writes=["ps3"])
            S.act(lambda e: e.activation(out=sgl[:], in_=PS[3][:, 0:256], func=AF.Sigmoid), reads=["ps3"], writes=["sgl"])
            S.dve(lambda e: e.tensor_tensor(out=sgl[:], in0=PS[3][:, 0:256], in1=sgl[:], op=ALU.mult), reads=["ps3", "sgl"], writes=["sgl"])
            S.dve(lambda e: e.tensor_tensor(out=actT[:].rearrange("p m t -> p (m t)"), in0=PS[3][:, 256:512], in1=sgl[:], op=ALU.mult), reads=["ps3", "sgl"], writes=["actT"])

            def frk(e):
                e.matmul(PS[2][:, 0:256], lhsT=tri_b[:], rhs=maskb[:], start=True, stop=True)
                return e.matmul(PS[2][:, 256:512], lhsT=ones_b[:], rhs=maskb[:], start=True, stop=True)
            S.pe(frk, reads=["tri_b", "ones_b", "maskb"], writes=["ps2"])
            S.dve(lambda e, i=i: e.tensor_tensor(out=pos_all[:, i, :], in0=PS[2][:, 0:256], in1=carry[:], op=ALU.add), reads=["ps2", "carry"], writes=["pos_all%d" % i])
            S.dve(lambda e: e.tensor_tensor(out=carry[:], in0=PS[2][:, 256:512], in1=carry[:], op=ALU.add), reads=["ps2", "carry"], writes=["carry"])

            def fsd(e):
                ins = None
                for half in range(2):
                    for m in range(2):
                        ins = e.matmul(PS[4 + half][:, :], lhsT=actT[:, m, :], rhs=wsd[:, m, half * 512:(half + 1) * 512], start=(m == 0), stop=(m == 1))
                return ins
            S.pe(fsd, reads=["actT", "wsd"], writes=["ps4", "ps5"])
            for half in range(2):
                hs = slice(half * 512, (half + 1) * 512)
                S.dve(lambda e, j=j, half=half, hs=hs: e.tensor_tensor(out=yt[j][:, hs], in0=PS[4 + half][:, :], in1=gate2_bc[:, hs], op=ALU.mult),
                      reads=["ps%d" % (4 + half), "gate2_bc"], writes=["yt%d_%d" % (j, half)])
            S.dve(lambda e, j=j, i=i: e.tensor_tensor(out=yt[j][:], in0=yt[j][:], in1=x1t[i % NX1][:], op=ALU.add),
                  reads=["yt%d_0" % j, "yt%d_1" % j, "x1u%d" % (i % NX1)], writes=["yt%d_0" % j, "yt%d_1" % j])
            S.dma("sp", lambda e, i=i, j=j: e.dma_start(out=x2p_d[i * 128:(i + 1) * 128, :], in_=yt[j][:]), reads=["yt%d_0" % j, "yt%d_1" % j], writes=["x2p_d%d" % i])

        stP(0)
        for i in range(NT):
            if i + 1 < NT:
                stP(i + 1)
            stQ(i)
        A.release(m5)
        S.barrier()
        if stage <= 4:
            zt = A.alloc("zt", [128, D], F32)
            S.dve(lambda e: e.memset(zt[:], 0.0), writes=["zt"])
            S.dma("sp", lambda e: e.dma_start(out=out[0:128, :], in_=zt[:]), reads=["zt"])
            S.emit(st)
            return nc, S

        PST = {}

        def bcreg(e):
            if "bc" not in PST:
                PST["bc"] = e.alloc_register("bc")
                e.reg_mov(PST["bc"], NBLK * 128 - 1)
            return PST["bc"]

        A.release(route_mark)
        cntp = A.alloc("cntp", [128, 256], F32)
        nbf = A.alloc("nbf", [128, 256], F32)
        nbi = A.alloc("nbi", [128, 256], I32)
        tmpa = A.alloc("tmpa", [128, 256], F32)
        tmpb = A.alloc("tmpb", [128, 256], F32)
        padded = A.alloc("padded", [128, 256], F32)
        pend = A.alloc("pend", [128, 256], F32)
        pstart = A.alloc("pstart", [128, 256], F32)
        ones256 = A.alloc("ones256", [128, 256], F32)
        S.dve(lambda e: e.memset(ones256[:], 1.0), writes=["ones256"])
        S.dve(lambda e: e.tensor_scalar(out=cntp[:], in0=carry[:], scalar1=63.5, scalar2=1.0 / 128, op0=ALU.add, op1=ALU.mult), reads=["carry"], writes=["cntp"])
        S.dve(lambda e: e.tensor_copy(out=nbi[:], in_=cntp[:]), reads=["cntp"], writes=["nbi"])
        S.dve(lambda e: e.tensor_copy(out=nbf[:], in_=nbi[:]), reads=["nbi"], writes=["nbf"])
        S.dve(lambda e: e.scalar_tensor_tensor(out=tmpa[:], in0=nbf[:], scalar=128.0, in1=carry[:], op0=ALU.mult, op1=ALU.is_lt), reads=["nbf", "carry"], writes=["tmpa"])
        S.dve(lambda e: e.tensor_scalar(out=tmpb[:], in0=nbf[:], scalar1=128.0, scalar2=-128.0, op0=ALU.mult, op1=ALU.add), reads=["nbf"], writes=["tmpb"])
        S.dve(lambda e: e.tensor_tensor(out=tmpb[:], in0=tmpb[:], in1=carry[:], op=ALU.is_ge), reads=["tmpb", "carry"], writes=["tmpb"])
        S.dve(lambda e: e.tensor_tensor(out=nbf[:], in0=nbf[:], in1=tmpa[:], op=ALU.add), reads=["nbf", "tmpa"], writes=["nbf"])
        S.dve(lambda e: e.tensor_tensor(out=nbf[:], in0=nbf[:], in1=tmpb[:], op=ALU.subtract), reads=["nbf", "tmpb"], writes=["nbf"])
        S.dve(lambda e: e.tensor_scalar(out=padded[:], in0=nbf[:], scalar1=128.0, scalar2=None, op0=ALU.mult), reads=["nbf"], writes=["padded"])
        S.dve(lambda e: e.tensor_tensor_scan(out=pend[:], data0=ones256[:], data1=padded[:], initial=0.0, op0=ALU.mult, op1=ALU.add), reads=["ones256", "padded"], writes=["pend"])
        S.dve(lambda e: e.tensor_tensor(out=pstart[:], in0=pend[:], in1=padded[:], op=ALU.subtract), reads=["pend", "padded"], writes=["pstart"])
        bvi = A.alloc("bvi", [128, 4], I32)
        bvf = A.alloc("bvf", [128, 4], F32)
        blkf = A.alloc("blkf", [128, 8], F32)
        blki = A.alloc("blki", [128, 8], I32)
        S.pool(lambda e: e.iota(bvi[:], pattern=[[128 * 128, 4]], base=0, channel_multiplier=128), writes=["bvi"])
        S.dve(lambda e: e.tensor_copy(out=bvf[:], in_=bvi[:]), reads=["bvi"], writes=["bvf"])
        for jb in range(4):
            S.dve(lambda e, jb=jb: e.tensor_scalar(out=tmpa[:], in0=pend[:], scalar1=bvf[:, jb:jb + 1], scalar2=0.0, op0=ALU.is_le, op1=ALU.add, accum_out=blkf[:, jb:jb + 1]),
                  reads=["pend", "bvf", "tmpa"], writes=["tmpa", "blkf%d" % jb])
        S.dve(lambda e: e.tensor_scalar(out=blkf[:, 0:4], in0=blkf[:, 0:4], scalar1=255.0, scalar2=None, op0=ALU.min), reads=["blkf%d" % jb for jb in range(4)], writes=["blkfa"])
        S.dve(lambda e: e.tensor_tensor(out=blkf[:, 4:8], in0=bvf[:], in1=pend[:, 255:256].to_broadcast([128, 4]), op=ALU.is_lt), reads=["bvf", "pend"], writes=["blkfb"])
        S.dve(lambda e: e.tensor_copy(out=blki[:], in_=blkf[:]), reads=["blkfa", "blkfb"], writes=["blki"])
        for r_ in range(2):
            S.dma("sp", lambda e, r_=r_: e.dma_start(out=blk_d[r_].rearrange("(j p) -> p j", p=128), in_=blki[:, r_ * 4:(r_ + 1) * 4]), reads=["blki"], writes=["blk_d%d" % r_])
        blkrow = A.alloc("blkrow", [1, 2 * NBLK], I32)
        S.dma("sp", lambda e: e.dma_start(out=blkrow[:], in_=blk_d.rearrange("r b -> (r b)").unsqueeze(0)), reads=["blk_d0", "blk_d1"], writes=["blkrow"])
        d8f = A.alloc("d8f", [128, 8], F32)
        d8i_all = A.alloc("d8i_all", [128, NT, 8], I32)
        keyt = A.alloc("keyt", [128, 256], F32)
        tmpk = [A.alloc("tmpk%d" % i, [128, 256], F32) for i in range(2)]
        h2g = [A.alloc("h2g%d" % i, [128, D], BF16) for i in range(3)]
        plan_mark = A.mark()
        for i in range(NT):
            j = i % 3
            S.dma("sp", lambda e, i=i, j=j: e.dma_start(out=h2g[j][:], in_=h2_d[i * 128:(i + 1) * 128, :]), reads=["h2_d%d" % i], writes=["h2g%d" % j])
            S.dve(lambda e, i=i: e.tensor_tensor(out=keyt[:], in0=pos_all[:, i, :], in1=pstart[:], op=ALU.add), reads=["pos_all%d" % i, "pstart"], writes=["keyt"])
            for k in range(8):
                tk = tmpk[k % 2]
                S.dve(lambda e, i=i, k=k, tk=tk: e.scalar_tensor_tensor(out=tk[:], in0=sb_all[:, i, :], scalar=m8_all[:, i, k:k + 1], in1=keyt[:], op0=ALU.is_equal, op1=ALU.mult),
                      reads=["sb_all%d" % i, "m8_all%d" % i, "keyt"], writes=["tmpk%d" % (k % 2)])
                S.dve(lambda e, k=k, tk=tk: e.tensor_reduce(out=d8f[:, k:k + 1], in_=tk[:], axis=AX.X, op=ALU.max), reads=["tmpk%d" % (k % 2)], writes=["d8f%d" % k])
            S.dve(lambda e, i=i: e.tensor_copy(out=d8i_all[:, i, :], in_=d8f[:]), reads=["d8f%d" % k for k in range(8)], writes=["d8i%d" % i])
            for k in range(8):
                S.dma("pool", lambda e, i=i, j=j, k=k: e.indirect_dma_start(
                    out=xs_d, out_offset=bass.IndirectOffsetOnAxis(ap=d8i_all[:, i, k:k + 1], axis=0),
                    in_=h2g[j][:], in_offset=None, bounds_check=bcreg(e), oob_is_err=False),
                    reads=["h2g%d" % j, "d8i%d" % i], writes=["xs_sc_%d_%d" % (i, k)])
        if debug:
            S.dma("sp", lambda e: e.dma_start(out=dbg_d8, in_=d8i_all[:].rearrange("p a b -> p (a b)")), reads=["d8i%d" % i for i in range(NT)])
            S.dma("sp", lambda e: e.dma_start(out=dbg_w8, in_=w8_all[:].rearrange("p a b -> p (a b)")), reads=["w8_all%d" % i for i in range(NT)])
        S.barrier()
        if stage <= 5:
            zt = A.alloc("zt", [128, D], F32)
            S.dve(lambda e: e.memset(zt[:], 0.0), writes=["zt"])
            S.dma("sp", lambda e: e.dma_start(out=out[0:128, :], in_=zt[:]), reads=["zt"])
            S.emit(st)
            return nc, S

        A.release(plan_mark)
        blkbc_i = A.alloc("blkbc_i", [128, 2 * NBLK], I32)
        blkbc_f = A.alloc("blkbc_f", [128, 2 * NBLK], F32)
        idxw_f = A.alloc("idxw_f", [128, NBLK], F32)
        idxw = A.alloc("idxw", [128, NBLK], I32)
        pcol_i = A.alloc("pcol_i", [128, 1], I32)
        pcol = A.alloc("pcol", [128, 1], F32)
        S.dma("sp", lambda e: e.dma_start(out=blkbc_i[:], in_=blk_d.rearrange("r b -> (r b)").partition_broadcast(128)), reads=["blk_d0", "blk_d1"], writes=["blkbc_i"])
        S.dve(lambda e: e.tensor_copy(out=blkbc_f[:], in_=blkbc_i[:]), reads=["blkbc_i"], writes=["blkbc_f"])
        S.pool(lambda e: e.iota(pcol_i[:], pattern=[[0, 1]], base=0, channel_multiplier=1), writes=["pcol_i"])
        S.dve(lambda e: e.tensor_copy(out=pcol[:], in_=pcol_i[:]), reads=["pcol_i"], writes=["pcol"])
        S.dve(lambda e: e.tensor_scalar(out=idxw_f[:], in0=blkbc_f[:, 0:NBLK], scalar1=128.0, scalar2=pcol[:, 0:1], op0=ALU.mult, op1=ALU.add), reads=["blkbc_f", "pcol"], writes=["idxw_f"])
        S.dve(lambda e: e.tensor_scalar(out=blkbc_f[:, NBLK:2 * NBLK], in0=blkbc_f[:, NBLK:2 * NBLK], scalar1=-1.0e6, scalar2=1.0e6, op0=ALU.mult, op1=ALU.add), reads=["blkbc_f"], writes=["blkbc_f"])
        S.dve(lambda e: e.tensor_tensor(out=idxw_f[:], in0=idxw_f[:], in1=blkbc_f[:, NBLK:2 * NBLK], op=ALU.add), reads=["idxw_f", "blkbc_f"], writes=["idxw_f"])
        idxw2 = A.alloc("idxw2", [128, NBLK], I32)
        gef = A.alloc("gef", [128, NBLK], F32)
        S.dve(lambda e: e.tensor_scalar(out=gef[:], in0=idxw_f[:], scalar1=16384.0, scalar2=1.0e6, op0=ALU.is_ge, op1=ALU.mult), reads=["idxw_f"], writes=["gef"])
        S.dve(lambda e: e.tensor_tensor(out=blkbc_f[:, 0:NBLK], in0=idxw_f[:], in1=gef[:], op=ALU.add), reads=["idxw_f", "gef", "blkbc_f"], writes=["blkbc_f"])
        S.dve(lambda e: e.tensor_copy(out=idxw[:], in_=blkbc_f[:, 0:NBLK]), reads=["blkbc_f"], writes=["idxw"])
        S.dve(lambda e: e.tensor_scalar(out=idxw_f[:], in0=idxw_f[:], scalar1=1.0e6 - 16384.0, scalar2=None, op0=ALU.add), reads=["idxw_f", "blkbc_f"], writes=["idxw_f"])
        S.dve(lambda e: e.tensor_tensor(out=idxw_f[:], in0=idxw_f[:], in1=gef[:], op=ALU.subtract), reads=["idxw_f", "gef"], writes=["idxw_f"])
        S.dve(lambda e: e.tensor_copy(out=idxw2[:], in_=idxw_f[:]), reads=["idxw_f"], writes=["idxw2"])
        convert_experts(256)
        NWB = 6
        NXB = 6
        PFD = NWB - 1
        A2 = Arena(nc)
        A2.off, A2.limit, A2.n = off_sb, off_sb + 2 * NT * 256 * 4, 5000
        wbuf = [A2.alloc("wbuf%d" % i, [128, 6144], BF16) for i in range(5)] + [A.alloc("wbuf%d" % i, [128, 6144], BF16) for i in range(5, NWB)]
        for i in range(NWB):
            S.pool(lambda e, i=i: e.memset(wbuf[i][:], 0.0), writes=["wbuf%dh0" % i, "wbuf%dh1" % i])
        Xb = [A.alloc("Xb%d" % i, [128, D], BF16) for i in range(NXB)]
        XT = [A.alloc("XT%d" % i, [128, 8, 128], BF16) for i in range(2)]
        Yb = [A.alloc("Yb%d" % i, [128, D], BF16) for i in range(2)]
        actE = [A.alloc("actE%d" % i, [128, 2, 128], BF16) for i in range(2)]
        sglE = [A.alloc("sglE%d" % i, [128, 256], F32) for i in range(2)]
        wreg = {}
        W16N = ["w16_%d" % e_ for e_ in range(256)]

        def wbound(e):
            if "r" not in wreg:
                wreg["r"] = e.alloc_register("wbnd")
                e.reg_mov(wreg["r"], 128 * 128 - 1)
            return wreg["r"]
        nblk_run = int(os.environ.get("KNBLK", str(NBLK)))

        def st_wload(b):
            s_ = b % NWB
            for hf, ix in ((0, idxw), (1, idxw2)):
                S.dma("pool", lambda e, hf=hf, ix=ix: e.indirect_dma_start(out=wbuf[s_][:], out_offset=None, in_=w16_d[hf],
                                                                         in_offset=bass.IndirectOffsetOnAxis(ap=ix[:, b:b + 1], axis=0), bounds_check=wbound(e), oob_is_err=False),
                      reads=["idxw", "idxw2"] + W16N, writes=["wbuf%dh%d" % (s_, hf)])

        def st_xload(b):
            jx = b % NXB
            if os.environ.get("KNOX", "0") == "1" and b > 8:
                return
            S.dma("sp", lambda e: e.dma_start(out=Xb[jx][:], in_=xs_d[b * 128:(b + 1) * 128, :]), writes=["Xb%d" % jx])

        def st_A(b):
            j, jx = b % 2, b % NXB

            def fxt(e):
                ins = None
                for kc in range(8):
                    ins = e.transpose(out=PSB[j][:, kc * 128:(kc + 1) * 128], in_=Xb[jx][:, kc * 128:(kc + 1) * 128], identity=ident_b[:])
                return ins
            S.pe(fxt, reads=["Xb%d" % jx, "ident_b"], writes=["ps%d" % j])
            S.act(lambda e: e.activation(out=XT[j][:].rearrange("p k t -> p (k t)"), in_=PSB[j][:, 0:1024], func=AF.Copy), reads=["ps%d" % j], writes=["XT%d" % j])

        def st_B(b):
            j, s_ = b % 2, b % NWB
            gb_ = 2 + j

            def fgu(e):
                ins = None
                for m in range(4):
                    base = (0 if m < 2 else 2048) + (m % 2) * 128
                    for kc in range(8):
                        ins = e.matmul(PS[gb_][:, m * 128:(m + 1) * 128], lhsT=wbuf[s_][:, base + kc * 256:base + kc * 256 + 128], rhs=XT[j][:, kc, :], start=(kc == 0), stop=(kc == 7))
                return ins
            S.pe(fgu, reads=["XT%d" % j, "wbuf%dh0" % s_, "wbuf%dh1" % s_], writes=["ps%d" % gb_])
            S.act(lambda e: e.activation(out=sglE[j][:], in_=PS[gb_][:, 0:256], func=AF.Silu), reads=["ps%d" % gb_], writes=["sglE%d" % j])
            S.dve(lambda e: e.tensor_tensor(out=actE[j][:].rearrange("p m t -> p (m t)"), in0=PS[gb_][:, 256:512], in1=sglE[j][:], op=ALU.mult), reads=["ps%d" % gb_, "sglE%d" % j], writes=["actE%d" % j])

        def st_C(b):
            j, s_ = b % 2, b % NWB
            yb_ = 4 + 2 * j

            def fy(e):
                ins = None
                for half in range(2):
                    for m in range(2):
                        ins = e.matmul(PS[yb_ + half][:, :], lhsT=actE[j][:, m, :], rhs=wbuf[s_][:, 4096 + m * 1024 + half * 512:4096 + m * 1024 + (half + 1) * 512], start=(m == 0), stop=(m == 1))
                return ins
            S.pe(fy, reads=["actE%d" % j, "wbuf%dh0" % s_, "wbuf%dh1" % s_], writes=["ps%d" % yb_, "ps%d" % (yb_ + 1)])
            S.act(lambda e: e.activation(out=Yb[j][:, 0:512], in_=PS[yb_][:, :], func=AF.Copy), reads=["ps%d" % yb_], writes=["Yb%d_0" % j])
            S.dve(lambda e: e.tensor_copy(out=Yb[j][:, 512:1024], in_=PS[yb_ + 1][:, :]), reads=["ps%d" % (yb_ + 1)], writes=["Yb%d_1" % j])
            if os.environ.get("KNOX", "0") == "1" and b > 8:
                return
            S.dma("sp", lambda e: e.dma_start(out=ys_d[b * 128:(b + 1) * 128, :], in_=Yb[j][:]), reads=["Yb%d_0" % j, "Yb%d_1" % j], writes=["ys_st_%d" % b])

        for b in range(min(PFD, nblk_run)):
            st_wload(b)
        for b in range(min(PFD, nblk_run)):
            st_xload(b)
        for t in range(-1, nblk_run + 1):
            if 0 <= t + 1 < nblk_run:
                st_A(t + 1)
            if 0 <= t < nblk_run:
                st_B(t)
            if 0 <= t - 1 < nblk_run:
                st_C(t - 1)
            if t >= 0 and t + PFD < nblk_run:
                st_wload(t + PFD)
                st_xload(t + PFD)
        S.barrier()
        A.release(plan_mark)
        fg_bc = A.alloc("fg_bc", [128, D], F32)
        S.dma("sp", lambda e: e.dma_start(out=fg_bc[:], in_=final_g.partition_broadcast(128)), writes=["fg_bc"])
        Gk = [[A.alloc("G%d_%d" % (jj, k), [128, D], BF16) for k in range(8)] for jj in range(2)]
        accA = [A.alloc("accA%d" % i, [128, D], F32) for i in range(2)]
        xpt = [A.alloc("xpt%d" % i, [128, D], F32) for i in range(2)]
        junkf = A.alloc("junkf", [128, D], F32)
        ssf = [A.alloc("ssf%d" % i, [128, 4], F32) for i in range(2)]
        for jj in range(2):
            for k in range(8):
                S.pool(lambda e, jj=jj, k=k: e.memset(Gk[jj][k][:], 0.0), writes=["G%d_%d" % (jj, k)])
        for i in range(NT):
            jj = i % 2
            S.dma("sp", lambda e, i=i, jj=jj: e.dma_start(out=xpt[jj][:], in_=x2p_d[i * 128:(i + 1) * 128, :]), reads=["x2p_d%d" % i], writes=["xpt%d" % jj])
            for k in range(8):
                S.dma("pool", lambda e, i=i, jj=jj, k=k: e.indirect_dma_start(out=Gk[jj][k][:], out_offset=None, in_=ys_d,
                                                                            in_offset=bass.IndirectOffsetOnAxis(ap=d8i_all[:, i, k:k + 1], axis=0), bounds_check=bcreg(e), oob_is_err=False),
                      reads=["d8i%d" % i, "G%d_%d" % (jj, k)], writes=["G%d_%d" % (jj, k)])
            an = "accA%d" % jj
            S.dve(lambda e, i=i, jj=jj: e.tensor_scalar(out=accA[jj][:], in0=Gk[jj][0][:], scalar1=w8_all[:, i, 0:1], scalar2=None, op0=ALU.mult), reads=["G%d_0" % jj, "w8_all%d" % i], writes=[an])
            for k in range(1, 8):
                S.dve(lambda e, i=i, jj=jj, k=k: e.scalar_tensor_tensor(out=accA[jj][:], in0=Gk[jj][k][:], scalar=w8_all[:, i, k:k + 1], in1=accA[jj][:], op0=ALU.mult, op1=ALU.add),
                      reads=["G%d_%d" % (jj, k), "w8_all%d" % i, an], writes=[an])
            S.dve(lambda e, jj=jj: e.tensor_tensor(out=accA[jj][:], in0=accA[jj][:], in1=gate2_bc[:], op=ALU.mult), reads=[an, "gate2_bc"], writes=[an])
            S.dve(lambda e, jj=jj: e.tensor_tensor(out=accA[jj][:], in0=accA[jj][:], in1=xpt[jj][:], op=ALU.add), reads=[an, "xpt%d" % jj], writes=[an])
            sn = "ssf%d" % jj
            S.act(lambda e, jj=jj: e.activation(out=junkf[:], in_=accA[jj][:], func=AF.Square, accum_out=ssf[jj][:, 0:1]), reads=[an], writes=["junkf", sn])
            S.dve(lambda e, jj=jj: e.tensor_scalar(out=ssf[jj][:, 1:2], in0=ssf[jj][:, 0:1], scalar1=1.0 / D, scalar2=EPS, op0=ALU.mult, op1=ALU.add), reads=[sn], writes=[sn + "b"])
            S.act(lambda e, jj=jj: e.activation(out=ssf[jj][:, 2:3], in_=ssf[jj][:, 1:2], func=AF.Sqrt), reads=[sn + "b"], writes=[sn + "c"])
            S.dve(lambda e, jj=jj: e.reciprocal(out=ssf[jj][:, 3:4], in_=ssf[jj][:, 2:3]), reads=[sn + "c"], writes=[sn + "d"])
            S.dve(lambda e, jj=jj: e.scalar_tensor_tensor(out=accA[jj][:], in0=accA[jj][:], scalar=ssf[jj][:, 3:4], in1=fg_bc[:], op0=ALU.mult, op1=ALU.mult),
                  reads=[an, sn + "d", "fg_bc"], writes=[an])
            S.dma("sp", lambda e, i=i, jj=jj: e.dma_start(out=out[i * 128:(i + 1) * 128, :], in_=accA[jj][:]), reads=[an], writes=["out%d" % i])
        S.emit(st)
    return nc, S


_CACHE = {}


def pack_expert_weights(wg, wu, wd):
    o = np.empty((256, 128, 6144), np.float32)
    o[:, :, 0:2048] = wg.reshape(256, 8, 128, 256).transpose(0, 2, 1, 3).reshape(256, 128, 2048)
    o[:, :, 2048:4096] = wu.reshape(256, 8, 128, 256).transpose(0, 2, 1, 3).reshape(256, 128, 2048)
    o[:, :, 4096:6144] = wd.reshape(256, 2, 128, 1024).transpose(0, 2, 1, 3).reshape(256, 128, 2048)
    return o.reshape(256 * 128, 6144)


def kernel(**inputs):
    n = 8
    if "nc" not in _CACHE:
        _CACHE["nc"] = build(stage=int(os.environ.get("KSTAGE", "99")))[0]
    nc = _CACHE["nc"]
    shared = {k: np.ascontiguousarray(v[0]) for k, v in inputs.items()
              if k not in ("x", "c", "lb_logits", "final_g", "w_exp_gate", "w_exp_up", "w_exp_down")}
    shared["w_exp_all"] = pack_expert_weights(inputs["w_exp_gate"][0], inputs["w_exp_up"][0], inputs["w_exp_down"][0])
    shared["lb_logits"] = np.ascontiguousarray(inputs["lb_logits"])
    shared["final_g"] = np.ascontiguousarray(inputs["final_g"])
    in_maps = []
    for b in range(n):
        m = dict(shared)
        m["x"] = np.ascontiguousarray(inputs["x"][b])
        m["c"] = np.ascontiguousarray(inputs["c"][b])
        in_maps.append(m)
    res = run_bass_kernel_spmd(nc, in_maps, core_ids=list(range(n)))
    return np.stack([r["out"] for r in res.results], axis=0)
```
